# Optimizing a Trainium2 kernel written in Bass

```python
import math
import jax, jax.numpy as jnp
from jax import lax
import numpy as np

D_MODEL = 2048
BATCH = 2
SEQ = 8192
DEPTH = 4

N_BRANCH = 4
BRANCH_WIDTH = 512

GLA_HEADS = 4
GLA_DK = 64
GLA_DV = 128
GLA_LOWRANK = 16
GLA_NORMALIZER = 16.0
GLA_CHUNK = 64

LRU_WIDTH = 512
LRU_BLOCKS = 4
LRU_BLOCK_DIM = LRU_WIDTH // LRU_BLOCKS
LRU_CONV = 4
LRU_C = 8.0

NSA_HEADS = 4
NSA_DH = 128
NSA_CMP_LEN = 32
NSA_CMP_STRIDE = 16
NSA_SEL_BLOCK = 64
NSA_SEL_TOPK = 16
NSA_WINDOW = 512
NSA_QBLOCK = 128

RWKV_HEADS = 8
RWKV_DH = 64
RWKV_WIDTH = RWKV_HEADS * RWKV_DH
RWKV_W_LORA = 96
RWKV_A_LORA = 96
RWKV_G_LORA = 256
RWKV_SPLITS = (RWKV_WIDTH, RWKV_WIDTH, RWKV_WIDTH, RWKV_W_LORA, RWKV_A_LORA, RWKV_G_LORA)
RWKV_IN = sum(RWKV_SPLITS)

NUM_BUCKETS = 32
MAX_DISTANCE = 128

D_FF = 5632
FFN_CONV = 3

IN_SPLITS = (
    GLA_HEADS * GLA_DK, GLA_HEADS * GLA_DK, GLA_HEADS * GLA_DV, GLA_HEADS * GLA_DV, GLA_LOWRANK,
    LRU_WIDTH, LRU_WIDTH,
    NSA_HEADS * NSA_DH, 6 * NSA_DH, 3 * NSA_HEADS,
    RWKV_IN,
    N_BRANCH * D_MODEL,
)
IN_TOTAL = sum(IN_SPLITS)
NEG_BIG = 1e9

kernel_name = 'hybrid_gla_rglru_nsa_rwkv7_convffn'


def rms_norm(x, g, eps=1e-6):
    xf = x.astype(jnp.float32)
    y = xf * lax.rsqrt(jnp.mean(xf * xf, axis=-1, keepdims=True) + eps)
    return (y * g.astype(jnp.float32)).astype(x.dtype)


def causal_dwconv(x, w, b):
    k, c = w.shape
    y = lax.conv_general_dilated(x, w[:, None, :].astype(x.dtype), window_strides=(1,), padding=[(k - 1, 0)],
                                 dimension_numbers=('NWC', 'WIO', 'NWC'), feature_group_count=c)
    return y + b.astype(x.dtype)


def split_cols(t, widths):
    parts, start = [], 0
    for w in widths:
        parts.append(t[..., start:start + w])
        start += w
    return parts


def masked_softmax(logits, mask):
    logits = jnp.where(mask, logits.astype(jnp.float32), -NEG_BIG)
    m = jnp.max(logits, axis=-1, keepdims=True)
    p = jnp.where(mask, jnp.exp(logits - m), 0.0)
    return p / jnp.maximum(jnp.sum(p, axis=-1, keepdims=True), 1e-30)


def t5_bucket(dist):
    n = jnp.maximum(dist, 0)
    max_exact = NUM_BUCKETS // 2
    nf = jnp.maximum(n, 1).astype(jnp.float32)
    large = max_exact + (jnp.log(nf / max_exact) / math.log(MAX_DISTANCE / max_exact)
                         * (NUM_BUCKETS - max_exact)).astype(jnp.int32)
    large = jnp.minimum(large, NUM_BUCKETS - 1)
    return jnp.where(n < max_exact, n, large)


def gla_mixer(q, k, v, g, lr, w_gk, b_gk, out_gain):
    B, S, _ = q.shape
    H, dk, dv, C = GLA_HEADS, GLA_DK, GLA_DV, GLA_CHUNK
    nc = S // C
    f32 = jnp.float32
    log_a = jax.nn.log_sigmoid((lr @ w_gk + b_gk).astype(f32)) / GLA_NORMALIZER

    def chunks(t, d):
        return t.astype(f32).reshape(B, nc, C, H, d).transpose(1, 0, 3, 2, 4)

    qc = chunks(q, dk) * dk ** -0.5
    kc = chunks(k, dk)
    vc = chunks(v, dv)
    bc = jnp.cumsum(chunks(log_a, dk), axis=3)
    causal = jnp.tril(jnp.ones((C, C), dtype=bool))[:, :, None]

    def step(state, inp):
        qi, ki, vi, bi = inp
        o_inter = jnp.einsum('bhtd,bhdv->bhtv', qi * jnp.exp(bi), state)
        decay = jnp.exp(jnp.where(causal, bi[:, :, :, None, :] - bi[:, :, None, :, :], -jnp.inf))
        scores = jnp.einsum('bhtd,bhsd,bhtsd->bhts', qi, ki, decay)
        o = o_inter + jnp.einsum('bhts,bhsv->bhtv', scores, vi)
        b_last = bi[:, :, -1:, :]
        state = jnp.exp(b_last[:, :, 0, :, None]) * state + jnp.einsum('bhsd,bhsv->bhdv', ki * jnp.exp(b_last - bi), vi)
        return state, o

    s0 = jnp.zeros((B, H, dk, dv), f32)
    _, o = lax.scan(step, s0, (qc, kc, vc, bc))
    o = o.transpose(1, 0, 3, 2, 4).reshape(B, S, H, dv)
    o = rms_norm(o, out_gain) * jax.nn.silu(g.astype(f32)).reshape(B, S, H, dv)
    return o.reshape(B, S, H * dv).astype(q.dtype)


def rglru_mixer(xb, gb, conv_w, conv_b, w_a, b_a, w_i, b_i, lam):
    B, S, _ = xb.shape
    f32 = jnp.float32
    xc = causal_dwconv(xb, conv_w, conv_b)
    xh = xc.reshape(B, S, LRU_BLOCKS, LRU_BLOCK_DIM)
    r = jax.nn.sigmoid(jnp.einsum('bsni,nij->bsnj', xh, w_a).reshape(B, S, LRU_WIDTH) + b_a).astype(f32)
    i = jax.nn.sigmoid(jnp.einsum('bsni,nij->bsnj', xh, w_i).reshape(B, S, LRU_WIDTH) + b_i).astype(f32)
    log_a = LRU_C * r * jax.nn.log_sigmoid(lam.astype(f32))
    a = jnp.exp(log_a)
    u = jnp.sqrt(-jnp.expm1(2.0 * log_a)) * (i * xc.astype(f32))
    _, h = lax.associative_scan(lambda e, l: (e[0] * l[0], l[0] * e[1] + l[1]), (a, u), axis=1)
    return (h * jax.nn.gelu(gb.astype(f32))).astype(xb.dtype)


def nsa_mixer(q, kv, gate, rel_bias, cmp_pos, cmp_k1, cmp_k2, cmp_v1, cmp_v2, q_gain, k_gain):
    B, S, _ = q.shape
    H, dh = NSA_HEADS, NSA_DH
    L, D, SB, QB, W = NSA_CMP_LEN, NSA_CMP_STRIDE, NSA_SEL_BLOCK, NSA_QBLOCK, NSA_WINDOW
    f32 = jnp.float32
    scale = dh ** -0.5
    pos = jnp.arange(S)
    qh = rms_norm(q.reshape(B, S, H, dh), q_gain).transpose(0, 2, 1, 3)
    k_cmp, v_cmp, k_slc, v_slc, k_win, v_win = split_cols(kv, (dh,) * 6)

    n_cmp = (S - L) // D + 1
    cmp_start = jnp.arange(n_cmp) * D
    idx = cmp_start[:, None] + jnp.arange(L)[None, :]

    def compress(t, w1, w2):
        blocks = (t[:, idx, :] + cmp_pos).reshape(B, n_cmp, L * dh)
        return jax.nn.gelu(blocks @ w1) @ w2

    kc = rms_norm(compress(k_cmp, cmp_k1, cmp_k2), k_gain)
    vc = compress(v_cmp, cmp_v1, cmp_v2)
    dist_c = pos[:, None] - (cmp_start + L - 1)[None, :]
    bias_c = rel_bias[t5_bucket(dist_c)].transpose(2, 0, 1).astype(f32)
    logit_c = jnp.einsum('bhsd,bnd->bhsn', qh, kc).astype(f32) * scale + bias_c
    p_c = masked_softmax(logit_c, dist_c >= 0)
    o_cmp = jnp.einsum('bhsn,bnd->bhsd', p_c, vc.astype(f32))

    n_sel = S // SB
    top_k = min(NSA_SEL_TOPK, n_sel)
    sel_start = jnp.arange(n_sel) * SB
    overlap = ((cmp_start[:, None] < sel_start[None, :] + SB) &
               (cmp_start[:, None] + L > sel_start[None, :])).astype(f32)
    score = jnp.einsum('bhsn,nj->bsj', p_c, overlap)
    blk = jnp.arange(n_sel)[None, :]
    cur = (pos // SB)[:, None]
    forced = (blk == 0) | (blk == cur) | (blk == cur - 1)
    score = jnp.where(forced, NEG_BIG, jnp.where(sel_start[None, :] <= pos[:, None], score, -NEG_BIG))
    _, sel_idx = lax.top_k(score, top_k)

    k_blocks = rms_norm(k_slc, k_gain).reshape(B, n_sel, SB, dh)
    v_blocks = v_slc.reshape(B, n_sel, SB, dh)
    nq = S // QB
    q_blocks = qh.reshape(B, H, nq, QB, dh).transpose(2, 0, 1, 3, 4)
    idx_blocks = sel_idx.reshape(B, nq, QB, top_k).transpose(1, 0, 2, 3)
    pos_blocks = pos.reshape(nq, QB)
    gather = jax.vmap(lambda blocks, ids: blocks[ids])

    def select_block(args):
        qb, ib, pb = args
        kg = gather(k_blocks, ib).reshape(B, QB, top_k * SB, dh)
        vg = gather(v_blocks, ib).reshape(B, QB, top_k * SB, dh)
        kpos = (ib[..., None] * SB + jnp.arange(SB)).reshape(B, QB, top_k * SB)
        dist = pb[None, :, None] - kpos
        bias = rel_bias[t5_bucket(dist)].transpose(0, 3, 1, 2).astype(f32)
        logit = jnp.einsum('bhtd,btkd->bhtk', qb, kg).astype(f32) * scale + bias
        p = masked_softmax(logit, (dist >= 0)[:, None])
        return jnp.einsum('bhtk,btkd->bhtd', p, vg.astype(f32))

    o_sel = lax.map(select_block, (q_blocks, idx_blocks, pos_blocks))
    o_sel = o_sel.transpose(1, 2, 0, 3, 4).reshape(B, H, S, dh)

    nw = W // QB

    def band(t):
        tp = jnp.pad(t, ((0, 0), (W, 0), (0, 0))).reshape(B, nq + nw, QB, dh)
        return jnp.concatenate([tp[:, j:j + nq] for j in range(nw + 1)], axis=2)

    kw = band(rms_norm(k_win, k_gain))
    vw = band(v_win)
    kpos_w = jnp.arange(nq)[:, None] * QB - W + jnp.arange((nw + 1) * QB)[None, :]
    dist_w = pos_blocks[:, :, None] - kpos_w[:, None, :]
    mask_w = (dist_w >= 0) & (dist_w < W) & (kpos_w[:, None, :] >= 0)
    bias_w = rel_bias[t5_bucket(dist_w)].transpose(3, 0, 1, 2).astype(f32)
    logit_w = jnp.einsum('bhnqd,bnkd->bhnqk', qh.reshape(B, H, nq, QB, dh), kw).astype(f32) * scale + bias_w
    p_w = masked_softmax(logit_w, mask_w)
    o_win = jnp.einsum('bhnqk,bnkd->bhnqd', p_w, vw.astype(f32)).reshape(B, H, S, dh)

    g = jax.nn.sigmoid(gate.astype(f32)).reshape(B, S, 3, H).transpose(2, 0, 3, 1)[..., None]
    o = g[0] * o_cmp + g[1] * o_sel + g[2] * o_win
    return o.transpose(0, 2, 1, 3).reshape(B, S, H * dh).astype(q.dtype)


def rwkv7_mixer(feat, mu, w0, w_lora, a0, a_lora, g_lora, k_k, k_a, r_k, ln_w, ln_b):
    B, S, _ = feat.shape
    H, N = RWKV_HEADS, RWKV_DH
    f32 = jnp.float32
    out_dtype = feat.dtype
    feat = feat.astype(f32)
    prev = jnp.pad(feat, ((0, 0), (1, 0), (0, 0)))[:, :-1]
    xm = feat + (prev - feat) * mu
    r, k, v, xw, xa, xg = split_cols(xm, RWKV_SPLITS)
    log_w = -math.exp(-0.5) * jax.nn.sigmoid(w0 + jnp.tanh(xw) @ w_lora)
    a = jax.nn.sigmoid(a0 + xa @ a_lora)
    g = jax.nn.sigmoid(xg) @ g_lora
    heads = lambda t: t.reshape(B, S, H, N)
    kk = heads(k * k_k)
    kk = kk / jnp.maximum(jnp.linalg.norm(kk, axis=-1, keepdims=True), 1e-12)
    k = k * (1.0 + (a - 1.0) * k_a)
    tm = lambda t: t.transpose(1, 0, 2, 3)

    def step(state, inp):
        r_t, w_t, k_t, v_t, kk_t, a_t = inp
        sa = jnp.einsum('bhij,bhj->bhi', state, -kk_t)
        state = (state * w_t[:, :, None, :] + sa[..., None] * (kk_t * a_t)[:, :, None, :]
                 + v_t[..., None] * k_t[:, :, None, :])
        return state, jnp.einsum('bhij,bhj->bhi', state, r_t)

    s0 = jnp.zeros((B, H, N, N), f32)
    xs = (tm(heads(r)), tm(heads(jnp.exp(log_w))), tm(heads(k)), tm(heads(v)), tm(kk), tm(heads(a)))
    _, y = lax.scan(step, s0, xs)
    y = y.transpose(1, 0, 2, 3)
    mean = jnp.mean(y, axis=-1, keepdims=True)
    var = jnp.mean(jnp.square(y - mean), axis=-1, keepdims=True)
    y = (y - mean) * lax.rsqrt(var + 64e-5) * ln_w.reshape(H, N) + ln_b.reshape(H, N)
    y = y + jnp.sum(heads(r) * heads(k) * r_k, axis=-1, keepdims=True) * heads(v)
    return (y.reshape(B, S, H * N) * g).astype(out_dtype)


def setup_inputs(seed: int = 0) -> dict:
    key = jax.random.key(seed)
    ks = iter(jax.random.split(key, 40))
    f32 = jnp.float32

    def nrm(shape, scale):
        return jax.random.normal(next(ks), shape, f32) * scale

    def gain(shape):
        return 1.0 + nrm(shape, 0.05)

    Lh = DEPTH
    res_scale = (2 * DEPTH) ** -0.5
    x = nrm((BATCH, SEQ, D_MODEL), 1.0)
    rel_bias = nrm((NUM_BUCKETS, NSA_HEADS), 0.5)
    attn_norm = gain((Lh, D_MODEL))
    ffn_norm = gain((Lh, D_MODEL))
    w_in = nrm((Lh, D_MODEL, IN_TOTAL), D_MODEL ** -0.5)
    gla_w_gk = nrm((Lh, GLA_LOWRANK, GLA_HEADS * GLA_DK), GLA_LOWRANK ** -0.5)
    gla_b_gk = nrm((Lh, GLA_HEADS * GLA_DK), 0.5)
    gla_out_norm = gain((Lh, GLA_DV))
    lru_conv_w = nrm((Lh, LRU_CONV, LRU_WIDTH), LRU_CONV ** -0.5)
    lru_conv_b = nrm((Lh, LRU_WIDTH), 0.02)
    lru_w_a = nrm((Lh, LRU_BLOCKS, LRU_BLOCK_DIM, LRU_BLOCK_DIM), LRU_BLOCK_DIM ** -0.5)
    lru_b_a = nrm((Lh, LRU_WIDTH), 0.1)
    lru_w_i = nrm((Lh, LRU_BLOCKS, LRU_BLOCK_DIM, LRU_BLOCK_DIM), LRU_BLOCK_DIM ** -0.5)
    lru_b_i = nrm((Lh, LRU_WIDTH), 0.1)
    a_base = jax.random.uniform(next(ks), (Lh, LRU_WIDTH), f32, minval=0.9, maxval=0.999)
    s = a_base ** (1.0 / LRU_C)
    lru_lambda = jnp.log(s) - jnp.log1p(-s)
    nsa_cmp_pos = nrm((Lh, NSA_CMP_LEN, NSA_DH), 0.1)
    nsa_cmp_k1 = nrm((Lh, NSA_CMP_LEN * NSA_DH, NSA_DH), (NSA_CMP_LEN * NSA_DH) ** -0.5)
    nsa_cmp_k2 = nrm((Lh, NSA_DH, NSA_DH), NSA_DH ** -0.5)
    nsa_cmp_v1 = nrm((Lh, NSA_CMP_LEN * NSA_DH, NSA_DH), (NSA_CMP_LEN * NSA_DH) ** -0.5)
    nsa_cmp_v2 = nrm((Lh, NSA_DH, NSA_DH), NSA_DH ** -0.5)
    nsa_q_norm = gain((Lh, NSA_DH))
    nsa_k_norm = gain((Lh, NSA_DH))
    rwkv_mu = jax.random.uniform(next(ks), (Lh, RWKV_IN), f32)
    rwkv_w0 = nrm((Lh, RWKV_WIDTH), 1.0)
    rwkv_w_lora = nrm((Lh, RWKV_W_LORA, RWKV_WIDTH), RWKV_W_LORA ** -0.5)
    rwkv_a0 = nrm((Lh, RWKV_WIDTH), 0.5)
    rwkv_a_lora = nrm((Lh, RWKV_A_LORA, RWKV_WIDTH), RWKV_A_LORA ** -0.5)
    rwkv_g_lora = nrm((Lh, RWKV_G_LORA, RWKV_WIDTH), RWKV_G_LORA ** -0.5)
    rwkv_k_k = 0.85 + nrm((Lh, RWKV_WIDTH), 0.05)
    rwkv_k_a = gain((Lh, RWKV_WIDTH))
    rwkv_r_k = nrm((Lh, RWKV_HEADS, RWKV_DH), 0.1)
    rwkv_ln_w = gain((Lh, RWKV_WIDTH))
    rwkv_ln_b = nrm((Lh, RWKV_WIDTH), 0.02)
    w_branch = nrm((Lh, N_BRANCH, BRANCH_WIDTH, D_MODEL), BRANCH_WIDTH ** -0.5)
    w_out = nrm((Lh, D_MODEL, D_MODEL), D_MODEL ** -0.5 * res_scale)
    ffn_up = nrm((Lh, D_MODEL, 2 * D_FF), D_MODEL ** -0.5)
    ffn_conv_w = nrm((Lh, FFN_CONV, 2 * D_FF), FFN_CONV ** -0.5)
    ffn_conv_b = nrm((Lh, 2 * D_FF), 0.02)
    ffn_down = nrm((Lh, D_FF, D_MODEL), D_FF ** -0.5 * res_scale)
    return {'x': x, 'rel_bias': rel_bias, 'attn_norm': attn_norm, 'ffn_norm': ffn_norm, 'w_in': w_in,
            'gla_w_gk': gla_w_gk, 'gla_b_gk': gla_b_gk, 'gla_out_norm': gla_out_norm,
            'lru_conv_w': lru_conv_w, 'lru_conv_b': lru_conv_b, 'lru_w_a': lru_w_a, 'lru_b_a': lru_b_a,
            'lru_w_i': lru_w_i, 'lru_b_i': lru_b_i, 'lru_lambda': lru_lambda,
            'nsa_cmp_pos': nsa_cmp_pos, 'nsa_cmp_k1': nsa_cmp_k1, 'nsa_cmp_k2': nsa_cmp_k2,
            'nsa_cmp_v1': nsa_cmp_v1, 'nsa_cmp_v2': nsa_cmp_v2, 'nsa_q_norm': nsa_q_norm, 'nsa_k_norm': nsa_k_norm,
            'rwkv_mu': rwkv_mu, 'rwkv_w0': rwkv_w0, 'rwkv_w_lora': rwkv_w_lora, 'rwkv_a0': rwkv_a0,
            'rwkv_a_lora': rwkv_a_lora, 'rwkv_g_lora': rwkv_g_lora, 'rwkv_k_k': rwkv_k_k, 'rwkv_k_a': rwkv_k_a,
            'rwkv_r_k': rwkv_r_k, 'rwkv_ln_w': rwkv_ln_w, 'rwkv_ln_b': rwkv_ln_b,
            'w_branch': w_branch, 'w_out': w_out, 'ffn_up': ffn_up, 'ffn_conv_w': ffn_conv_w,
            'ffn_conv_b': ffn_conv_b, 'ffn_down': ffn_down}


def reference(x, rel_bias, attn_norm, ffn_norm, w_in, gla_w_gk, gla_b_gk, gla_out_norm,
              lru_conv_w, lru_conv_b, lru_w_a, lru_b_a, lru_w_i, lru_b_i, lru_lambda,
              nsa_cmp_pos, nsa_cmp_k1, nsa_cmp_k2, nsa_cmp_v1, nsa_cmp_v2, nsa_q_norm, nsa_k_norm,
              rwkv_mu, rwkv_w0, rwkv_w_lora, rwkv_a0, rwkv_a_lora, rwkv_g_lora, rwkv_k_k, rwkv_k_a,
              rwkv_r_k, rwkv_ln_w, rwkv_ln_b, w_branch, w_out, ffn_up, ffn_conv_w, ffn_conv_b, ffn_down):
    B, S, _ = x.shape
    for l in range(DEPTH):
        h = rms_norm(x, attn_norm[l])
        proj = h @ w_in[l]
        (gla_q, gla_k, gla_v, gla_g, gla_lr, lru_x, lru_g,
         nsa_q, nsa_kv, nsa_gate, rwkv_feat, merge_gate) = split_cols(proj, IN_SPLITS)
        y_a = gla_mixer(gla_q, gla_k, gla_v, gla_g, gla_lr, gla_w_gk[l], gla_b_gk[l], gla_out_norm[l])
        y_b = rglru_mixer(lru_x, lru_g, lru_conv_w[l], lru_conv_b[l], lru_w_a[l], lru_b_a[l],
                          lru_w_i[l], lru_b_i[l], lru_lambda[l])
        y_c = nsa_mixer(nsa_q, nsa_kv, nsa_gate, rel_bias, nsa_cmp_pos[l], nsa_cmp_k1[l], nsa_cmp_k2[l],
                        nsa_cmp_v1[l], nsa_cmp_v2[l], nsa_q_norm[l], nsa_k_norm[l])
        y_d = rwkv7_mixer(rwkv_feat, rwkv_mu[l], rwkv_w0[l], rwkv_w_lora[l], rwkv_a0[l], rwkv_a_lora[l],
                          rwkv_g_lora[l], rwkv_k_k[l], rwkv_k_a[l], rwkv_r_k[l], rwkv_ln_w[l], rwkv_ln_b[l])
        gates = jax.nn.sigmoid(merge_gate).reshape(B, S, N_BRANCH, D_MODEL)
        merged = jnp.zeros_like(x)
        for n, y in enumerate((y_a, y_b, y_c, y_d)):
            merged = merged + gates[:, :, n] * (y @ w_branch[l, n])
        x = x + merged @ w_out[l]
        h = rms_norm(x, ffn_norm[l])
        u = causal_dwconv(h @ ffn_up[l], ffn_conv_w[l], ffn_conv_b[l])
        u_gate, u_val = jnp.split(u, 2, axis=-1)
        x = x + (jax.nn.silu(u_gate) * u_val) @ ffn_down[l]
    return x
```

```python
import numpy as np
import concourse.bass as bass
import concourse.mybir as mybir
from concourse.bass_utils import run_bass_kernel_spmd

F32 = mybir.dt.float32
BF16 = mybir.dt.bfloat16
AF = mybir.ActivationFunctionType
ALU = mybir.AluOpType
AX = mybir.AxisListType

D = 2048
S = 8192
NB = 2
DEPTH = 4
NCORE = 8
NTOK = 2048
HALO = 2
DFF = 5632
NMIX = 5852
NIN = 14044
KC = D // 128
import os as _os
SAME_ENGINE_SYNC = not bool(_os.environ.get("MK_NOSAME"))


class T:
    __slots__ = ("h", "lw", "rd", "name")

    def __init__(self, h, name):
        self.h = h
        self.name = name
        self.lw = None
        self.rd = {}

    def __getitem__(self, idx):
        return self.h[idx]


class MK:
    NDMA = 24

    def __init__(self, nc):
        self.nc = nc
        self.same = SAME_ENGINE_SYNC
        self.eng = {"pe": nc.tensor, "act": nc.scalar, "dve": nc.vector, "pool": nc.gpsimd, "sp": nc.sync}
        self.sem = {k: nc.alloc_semaphore("s_" + k) for k in self.eng}
        self.cnt = {k: 0 for k in self.eng}
        self.waited = {k: {} for k in self.eng}
        self.dsem, self.dval, self.dnext = {}, {}, {}
        for q in ("sp", "act", "pool"):
            self.dsem[q] = [nc.alloc_semaphore("d_%s_%d" % (q, i)) for i in range(self.NDMA)]
            self.dval[q] = [0] * self.NDMA
            self.dnext[q] = 0
        self.ntile = 0
        self.ninst = 0

    def sb(self, shape, dtype=F32, name=None):
        self.ntile += 1
        name = name or ("t%d" % self.ntile)
        return T(self.nc.alloc_sbuf_tensor(name, list(shape), dtype), name)

    def ps(self, shape, dtype=F32, name=None):
        self.ntile += 1
        name = name or ("p%d" % self.ntile)
        return T(self.nc.alloc_psum_tensor(name, list(shape), dtype), name)

    def dram(self, name, shape, dtype=F32, kind="Internal"):
        return T(self.nc.dram_tensor(name, list(shape), dtype, kind=kind), name)

    def _wait(self, e, key, val):
        if val <= 0:
            return
        w = self.waited[e]
        if w.get(key, 0) >= val:
            return
        w[key] = val
        if isinstance(key, str):
            self.eng[e].wait_ge(self.sem[key], val)
        else:
            q, i = key
            self.eng[e].wait_ge(self.dsem[q][i], val)

    def _deps(self, e, reads, writes):
        same = self.same and e != "pe"
        for t in reads:
            if t.lw is not None and (t.lw[0] != e or same):
                self._wait(e, *t.lw)
        for t in writes:
            if t.lw is not None and (t.lw[0] != e or same):
                self._wait(e, *t.lw)
            for k, v in t.rd.items():
                if k != e or same:
                    self._wait(e, k, v)

    def op(self, e, fn, reads=(), writes=()):
        self._deps(e, reads, writes)
        inst = fn(self.eng[e])
        self.cnt[e] += 1
        inst.then_inc(self.sem[e], 1)
        c = self.cnt[e]
        for t in reads:
            t.rd[e] = c
        for t in writes:
            t.lw = (e, c)
            t.rd = {}
        self.ninst += 1
        return inst

    def dma(self, q, out, in_, reads=(), writes=(), **kw):
        i = self.dnext[q]
        self.dnext[q] = (i + 1) % self.NDMA
        key = (q, i)
        self._wait(q, key, self.dval[q][i])
        self._deps(q, reads, writes)
        inst = self.eng[q].dma_start(out=out, in_=in_, **kw)
        self.dval[q][i] += 16
        inst.then_inc(self.dsem[q][i], 16)
        v = self.dval[q][i]
        for t in reads:
            t.rd[key] = v
        for t in writes:
            t.lw = (key, v)
            t.rd = {}
        self.ninst += 1
        return inst

    def barrier(self):
        for e in ("pe", "act", "dve", "pool", "sp"):
            for o in ("pe", "act", "dve", "pool", "sp"):
                if o != e:
                    self._wait(e, o, self.cnt[o])
            for q in ("sp", "act", "pool"):
                for i in range(self.NDMA):
                    self._wait(e, (q, i), self.dval[q][i])

    def finish(self):
        for e in ("pe", "act", "dve", "pool"):
            self._wait("sp", e, self.cnt[e])
        for q in ("sp", "act", "pool"):
            for i in range(self.NDMA):
                self._wait("sp", (q, i), self.dval[q][i])


class Arena:
    def __init__(self, m, nbytes):
        self.m = m
        nc = m.nc
        top = 229344
        self.base = ((top - nc.sbuf_bytes_remaining) + 63) // 64 * 64
        self.slab = nc.alloc_sbuf_tensor("arena_slab", [128, nbytes + 64], mybir.dt.uint8)
        self.size = nbytes
        self.off = 0

    def reset(self):
        self.m.barrier()
        self.off = 0

    def sb(self, shape, dtype=F32, name=None):
        esz = 4 if dtype == F32 else 2
        n = 1
        for d_ in shape[1:]:
            n *= d_
        nb = (n * esz + 63) // 64 * 64
        assert self.off + nb <= self.size, "arena overflow %s %d+%d>%d" % (name, self.off, nb, self.size)
        h = self.m.nc.alloc_sbuf_tensor_at(name or "ar", list(shape), dtype, offset=self.base + self.off)
        self.off += nb
        return T(h, name)


class Rot:
    def __init__(self, items):
        self.items = items
        self.i = 0

    def next(self):
        t = self.items[self.i]
        self.i = (self.i + 1) % len(self.items)
        return t


DP_ATTN = 0
DP_FFN = 16
DP_CW = 32
DP_CB = DP_CW + 264
DP_N = DP_CB + 88


def pack_dense_params(attn_norm, ffn_norm, conv_w, conv_b):
    out = np.zeros((128, DP_N), np.float32)
    out[:, DP_ATTN:DP_ATTN + 16] = attn_norm.reshape(16, 128).T
    out[:, DP_FFN:DP_FFN + 16] = ffn_norm.reshape(16, 128).T
    out[:, DP_CW:DP_CW + 264] = conv_w.reshape(3, 88, 128).transpose(2, 0, 1).reshape(128, 264)
    out[:, DP_CB:DP_CB + 88] = conv_b.reshape(88, 128).T
    return out


class DenseCtx:
    def __init__(self, m):
        self.m = m
        self.banks = Rot([m.ps([128, 512], F32, "bank%d" % i) for i in range(6)])
        self.sbank = m.ps([128, 512], F32, "sbank")
        self.ones = m.sb([128, 128], F32, "ones")
        m.op("pool", lambda e: e.memset(self.ones[:], 1.0), [], [self.ones])
        self.buf1 = m.sb([128, KC, NTOK + HALO], BF16, "buf1")
        self.buf2 = m.sb([128, 44 * 512], BF16, "buf2")
        self.xs = Rot([m.sb([128, 512], F32, "xs%d" % i) for i in range(4)])
        self.sq = Rot([m.sb([128, 512], F32, "sq%d" % i) for i in range(2)])
        self.rstd = m.sb([128, 512], F32, "rstd")
        self.wb = Rot([m.sb([128, 5632], BF16, "wb%d" % i) for i in range(2)])
        self.ev = Rot([m.sb([128, 512], F32, "ev%d" % i) for i in range(3)])
        self.evb = Rot([m.sb([128, 512], BF16, "evb%d" % i) for i in range(4)])
        self.scr = Rot([m.sb([128, 516], F32, "scr%d" % i) for i in range(7)])
        self.dq = Rot(["sp", "act"])


def rmsnorm_T(cx, x_dram, gain, tiles, dst, dst_off=0):
    m = cx.m
    for (t0, n) in tiles:
        for kc in range(KC):
            xs = cx.xs.next()
            m.dma("sp", xs[:, 0:n], x_dram[kc * 128:(kc + 1) * 128, t0:t0 + n], reads=[x_dram], writes=[xs])
            sq = cx.sq.next()
            m.op("act", lambda e: e.activation(out=sq[:, 0:n], in_=xs[:, 0:n], func=AF.Square), [xs], [sq])
            m.op("pe", lambda e: e.matmul(cx.sbank[:, 0:n], lhsT=cx.ones[:], rhs=sq[:, 0:n],
                                          start=(kc == 0), stop=(kc == KC - 1)), [cx.ones, sq], [cx.sbank])
        m.op("act", lambda e: e.activation(out=cx.rstd[:, 0:n], in_=cx.sbank[:, 0:n], func=AF.Sqrt,
                                           scale=1.0 / D, bias=1e-6), [cx.sbank], [cx.rstd])
        m.op("dve", lambda e: e.reciprocal(out=cx.rstd[:, 0:n], in_=cx.rstd[:, 0:n]), [cx.rstd], [cx.rstd])
        for kc in range(KC):
            xs = cx.xs.next()
            m.dma("sp", xs[:, 0:n], x_dram[kc * 128:(kc + 1) * 128, t0:t0 + n], reads=[x_dram], writes=[xs])
            m.op("dve", lambda e: e.scalar_tensor_tensor(
                out=dst[:, kc, dst_off + t0:dst_off + t0 + n], in0=xs[:, 0:n], scalar=gain[:, kc:kc + 1],
                in1=cx.rstd[:, 0:n], op0=ALU.mult, op1=ALU.mult), [xs, gain_t(gain), cx.rstd], [dst_t(dst)])


def gain_t(g):
    return g.t if hasattr(g, "t") else g


def dst_t(d):
    return d.t if hasattr(d, "t") else d


class V:
    def __init__(self, t, ap):
        self.t = t
        self.ap = ap

    def __getitem__(self, idx):
        return self.ap[idx]


def linear_T(cx, w_dram, row0, K, col_groups, src, src_off, tiles, epilogue, wq="pool"):
    m = cx.m
    kcn = K // 128
    for gi, grp in enumerate(col_groups):
        wb = cx.wb.next()
        gw = sum(nc_ for (_, nc_) in grp)
        wv = wb[:, 0:kcn * gw].rearrange("p (k c) -> p k c", k=kcn)
        off = 0
        offs = []
        for (c0, ncols) in grp:
            for k0 in range(0, kcn, 16):
                k1 = min(kcn, k0 + 16)
                src_ap = w_dram[row0 + k0 * 128:row0 + k1 * 128, c0:c0 + ncols].rearrange("(kc p) c -> p kc c", p=128)
                m.dma(wq, wv[:, k0:k1, off:off + ncols], src_ap, reads=[w_dram], writes=[wb])
            offs.append(off)
            off += ncols
        for si, (c0, ncols) in enumerate(grp):
            for ti, (t0, n) in enumerate(tiles):
                ps = cx.banks.next()
                for kc in range(kcn):
                    m.op("pe", lambda e: e.matmul(ps[0:ncols, 0:n], lhsT=wv[:, kc, offs[si]:offs[si] + ncols],
                                                  rhs=src[:, kc, src_off + t0:src_off + t0 + n],
                                                  start=(kc == 0), stop=(kc == kcn - 1)), [wb, dst_t(src)], [ps])
                epilogue(ps, gi, si, c0, ncols, ti, t0, n)


def groups_of(c_start, c_end, gw=256):
    out = []
    c = c_start
    while c < c_end:
        g = []
        ge = min(c + gw, c_end)
        while c < ge:
            n = min(128, ge - c)
            g.append((c, n))
            c += n
        out.append(g)
    return out


def phase_A(cx, x_dram, x_tiles, dp, w_in, proj_out, gates_out):
    m = cx.m
    gain = V(dp, dp[:, DP_ATTN:DP_ATTN + 16])
    tb = x_tiles[0][0]
    rmsnorm_T(cx, x_dram, gain, x_tiles, cx.buf1, dst_off=-tb)
    tiles = [(t0 - tb, n) for (t0, n) in x_tiles]

    def ep_mix(ps, gi, si, c0, ncols, ti, t0, n):
        ev = cx.ev.next()
        if (ti + si) % 2 == 0:
            m.op("act", lambda e: e.activation(out=ev[0:ncols, 0:n], in_=ps[0:ncols, 0:n], func=AF.Copy), [ps], [ev])
        else:
            m.op("dve", lambda e: e.tensor_copy(out=ev[0:ncols, 0:n], in_=ps[0:ncols, 0:n]), [ps], [ev])
        m.dma(cx.dq.next(), proj_out[c0:c0 + ncols, t0:t0 + n], ev[0:ncols, 0:n], reads=[ev], writes=[proj_out])

    def ep_gate(ps, gi, si, c0, ncols, ti, t0, n):
        ev = cx.evb.next()
        m.op("act", lambda e: e.activation(out=ev[0:ncols, 0:n], in_=ps[0:ncols, 0:n], func=AF.Sigmoid), [ps], [ev])
        m.dma(cx.dq.next(), gates_out[c0 - NMIX:c0 - NMIX + ncols, t0:t0 + n], ev[0:ncols, 0:n], reads=[ev],
              writes=[gates_out])

    linear_T(cx, w_in, 0, D, groups_of(0, NMIX), cx.buf1, 0, tiles, ep_mix)
    linear_T(cx, w_in, 0, D, groups_of(NMIX, NIN), cx.buf1, 0, tiles, ep_gate)


def phase_C(cx, x_dram, y_dram, gates_dram, dp, w_in, w_branch, w_out, ffn_up, ffn_down, xmid, x_out):
    m = cx.m
    NT = NTOK + HALO
    tiles = [(0, HALO)] + [(HALO + i * 512, 512) for i in range(NTOK // 512)]
    gain = V(dp, dp[:, DP_ATTN:DP_ATTN + 16])
    hh = m.sb([128, KC, HALO], BF16, "hh")
    rmsnorm_T(cx, x_dram, gain, [(0, HALO)], hh)
    gh = m.sb([128, 64, HALO], F32, "gh")

    def ep_gh(ps, gi, si, c0, ncols, ti, t0, n):
        j = (c0 - NMIX) // 128
        m.op("act", lambda e: e.activation(out=gh[:, j, :], in_=ps[:, 0:HALO], func=AF.Sigmoid), [ps], [gh])

    linear_T(cx, w_in, 0, D, groups_of(NMIX, NIN), hh, 0, [(0, HALO)], ep_gh)
    CSTOP = int(_os.environ.get("C_STOP", "9"))
    if CSTOP <= 1:
        return
    for r in range(16):
        m.dma("pool", cx.buf1[:, r, :], y_dram[r * 128:(r + 1) * 128, :], reads=[y_dram], writes=[cx.buf1])
    if CSTOP <= 2:
        return
    mview = V(cx.buf2, cx.buf2[:, 0:KC * 512].rearrange("p (k t) -> p k t", k=KC))
    for ti, (t0, n) in enumerate(tiles):
        for dt_ in range(KC):
            wb = cx.wb.next()
            wv = wb[:, 0:16 * 128].rearrange("p (k c) -> p k c", k=16)
            m.dma("pool", wv, w_branch[:, dt_ * 128:(dt_ + 1) * 128].rearrange("(r p) c -> p r c", p=128),
                  reads=[w_branch], writes=[wb])
            tmps = []
            for nb in range(4):
                ps = cx.banks.next()
                for kc in range(4):
                    r = nb * 4 + kc
                    m.op("pe", lambda e: e.matmul(ps[:, 0:n], lhsT=wv[:, r, :], rhs=cx.buf1[:, r, t0:t0 + n],
                                                  start=(kc == 0), stop=(kc == 3)), [wb, cx.buf1], [ps])
                tp = cx.scr.next()
                if ti == 0:
                    m.op("dve", lambda e: e.tensor_tensor(out=tp[:, 0:n], in0=ps[:, 0:n], in1=gh[:, nb * 16 + dt_, :],
                                                          op=ALU.mult), [ps, gh], [tp])
                else:
                    g = cx.evb.next()
                    m.dma(cx.dq.next(), g[:, 0:n], gates_dram[nb * D + dt_ * 128:nb * D + (dt_ + 1) * 128,
                                                              t0 - HALO:t0 - HALO + n], reads=[gates_dram], writes=[g])
                    m.op("dve", lambda e: e.tensor_tensor(out=tp[:, 0:n], in0=ps[:, 0:n], in1=g[:, 0:n], op=ALU.mult),
                         [ps, g], [tp])
                tmps.append(tp)
            m.op("pool", lambda e: e.tensor_tensor(out=tmps[0][:, 0:n], in0=tmps[0][:, 0:n], in1=tmps[1][:, 0:n], op=ALU.add),
                 [tmps[0], tmps[1]], [tmps[0]])
            m.op("pool", lambda e: e.tensor_tensor(out=tmps[2][:, 0:n], in0=tmps[2][:, 0:n], in1=tmps[3][:, 0:n], op=ALU.add),
                 [tmps[2], tmps[3]], [tmps[2]])
            m.op("pool", lambda e: e.tensor_tensor(out=mview[:, dt_, 0:n], in0=tmps[0][:, 0:n],
                                                   in1=tmps[2][:, 0:n], op=ALU.add), [tmps[0], tmps[2]], [cx.buf2])

        def ep_out(ps, gi, si, c0, ncols, ti_, t0_, n_, t0=t0):
            xs = cx.xs.next()
            m.dma("sp", xs[:, 0:n_], x_dram[c0:c0 + 128, t0:t0 + n_], reads=[x_dram], writes=[xs])
            ev = cx.ev.next()
            m.op("dve", lambda e: e.tensor_tensor(out=ev[:, 0:n_], in0=ps[:, 0:n_], in1=xs[:, 0:n_], op=ALU.add), [ps, xs], [ev])
            m.dma(cx.dq.next(), xmid[c0:c0 + 128, t0:t0 + n_], ev[:, 0:n_], reads=[ev], writes=[xmid])

        linear_T(cx, w_out, 0, D, groups_of(0, D), mview, 0, [(0, n)], ep_out)
    if CSTOP <= 3:
        return
    gain2 = V(dp, dp[:, DP_FFN:DP_FFN + 16])
    rmsnorm_T(cx, xmid, gain2, tiles, cx.buf1)
    if CSTOP <= 4:
        return
    carry = m.sb([128, 88, 2], F32, "carry")
    m.op("dve", lambda e: e.memset(carry[:], 0.0), [], [carry])
    G = V(cx.buf2, cx.buf2[:, 0:44 * 512].rearrange("p (k t) -> p k t", k=44))
    U = cx.scr
    CV = cx.scr
    passes = [[tiles[0], tiles[1]], [tiles[2]], [tiles[3]], [tiles[4]]]
    for pi, ptiles in enumerate(passes):
        conv_out = {}

        def ep_up(ps, gi, si, c0, ncols, ti, t0, n):
            ct = c0 // 128
            u = U.next()
            m.op("act", lambda e: e.activation(out=u[:, 2:2 + n], in_=ps[:, 0:n], func=AF.Copy), [ps], [u])
            m.op("dve", lambda e: e.tensor_copy(out=u[:, 0:2], in_=carry[:, ct, :]), [carry], [u])
            m.op("dve", lambda e: e.tensor_copy(out=carry[:, ct, :], in_=u[:, n:n + 2]), [u], [carry])
            if n == HALO:
                return
            cv = CV.next()
            w = lambda k: dp[:, DP_CW + k * 88 + ct:DP_CW + k * 88 + ct + 1]
            m.op("dve", lambda e: e.tensor_scalar(out=cv[:, 0:n], in0=u[:, 2:2 + n], scalar1=w(2),
                                                  scalar2=dp[:, DP_CB + ct:DP_CB + ct + 1], op0=ALU.mult, op1=ALU.add),
                 [u, dp], [cv])
            m.op("dve", lambda e: e.scalar_tensor_tensor(out=cv[:, 0:n], in0=u[:, 1:1 + n], scalar=w(1), in1=cv[:, 0:n],
                                                         op0=ALU.mult, op1=ALU.add), [u, dp, cv], [cv])
            m.op("dve", lambda e: e.scalar_tensor_tensor(out=cv[:, 0:n], in0=u[:, 0:n], scalar=w(0), in1=cv[:, 0:n],
                                                         op0=ALU.mult, op1=ALU.add), [u, dp, cv], [cv])
            if si == 0:
                sg = CV.next()
                m.op("act", lambda e: e.activation(out=sg[:, 0:n], in_=cv[:, 0:n], func=AF.Silu), [cv], [sg])
                conv_out["g"] = sg
            else:
                sg = conv_out["g"]
                m.op("dve", lambda e: e.tensor_tensor(out=G[:, gi, 0:n], in0=sg[:, 0:n], in1=cv[:, 0:n], op=ALU.mult),
                     [sg, cv], [cx.buf2])

        grps = [[(j * 128, 128), (DFF + j * 128, 128)] for j in range(44)]
        linear_T(cx, ffn_up, 0, D, grps, cx.buf1, 0, ptiles, ep_up)
        (t0r, nr) = ptiles[-1]
        if CSTOP == 5:
            continue

        def ep_down(ps, gi, si, c0, ncols, ti, t0, n):
            xs = cx.xs.next()
            m.dma("sp", xs[:, 0:n], xmid[c0:c0 + 128, t0r:t0r + n], reads=[xmid], writes=[xs])
            ev = cx.ev.next()
            m.op("dve", lambda e: e.tensor_tensor(out=ev[:, 0:n], in0=ps[:, 0:n], in1=xs[:, 0:n], op=ALU.add), [ps, xs], [ev])
            m.dma(cx.dq.next(), x_out[c0:c0 + 128, t0r - HALO:t0r - HALO + n], ev[:, 0:n], reads=[ev], writes=[x_out])

        linear_T(cx, ffn_down, 0, DFF, [[(j * 128, 128)] for j in range(KC)], G, 0, [(0, nr)], ep_down)


def build_dense(do_C, do_A):
    nc = bass.Bass("TRN2", target_bir_lowering=False)
    m = MK(nc)
    cx = DenseCtx(m)
    NT = NTOK + HALO
    ei = lambda name, shape, dt=F32: m.dram(name, shape, dt, kind="ExternalInput")
    eo = lambda name, shape, dt=F32: m.dram(name, shape, dt, kind="ExternalOutput")
    outs = []
    if do_C:
        x_in = ei("x_in", [D, NT])
        y_in = ei("y_in", [4 * 512, NT])
        g_in = ei("g_in", [4 * D, NTOK], BF16)
        dpc = ei("dpc", [128, DP_N])
        w_in_c = ei("w_in_c", [D, NIN])
        w_branch = ei("w_branch", [4 * 512, D])
        w_out = ei("w_out", [D, D])
        ffn_up = ei("ffn_up", [D, 2 * DFF])
        ffn_down = ei("ffn_down", [DFF, D])
        xmid = m.dram("xmid", [D, NT])
        x_out = eo("x_out", [D, NTOK])
        dpc_s = m.sb([128, DP_N], F32, "dpc_s")
        m.dma("sp", dpc_s[:], dpc[:], reads=[dpc], writes=[dpc_s])
        phase_C(cx, x_in, y_in, g_in, dpc_s, w_in_c, w_branch, w_out, ffn_up, ffn_down, xmid, x_out)
        xa, xa_tiles = x_out, [(i * 512, 512) for i in range(NTOK // 512)]
    else:
        xa = ei("x_in", [D, NTOK])
        xa_tiles = [(i * 512, 512) for i in range(NTOK // 512)]
    if do_A:
        dpa = ei("dpa", [128, DP_N])
        w_in_a = ei("w_in_a", [D, NIN])
        proj_out = eo("proj_out", [NMIX, NTOK])
        gates_out = eo("gates_out", [4 * D, NTOK], BF16)
        dpa_s = m.sb([128, DP_N], F32, "dpa_s")
        m.dma("sp", dpa_s[:], dpa[:], reads=[dpa], writes=[dpa_s])
        phase_A(cx, xa, xa_tiles, dpa_s, w_in_a, proj_out, gates_out)
    m.finish()
    return nc, m


R_GQ, R_GK, R_GV, R_GG, R_GLR = 0, 64, 128, 256, 384
R_LX, R_LG = 512, 640
R_NQ, R_NKV, R_NG = 768, 1280, 2048
R_RR, R_RK, R_RV, R_XW, R_XA, R_XG = 2176, 2304, 2432, 2560, 2688, 2816
R_NQO = 3072
MIX_ROWS = 3200
MP_GB, MP_GGAIN = 0, 1
MP_LCW, MP_LCB, MP_LBA, MP_LBI, MP_LLAM = 2, 6, 7, 8, 9
MP_NQG, MP_NKG = 10, 11
MP_MUR, MP_MUK, MP_MUV, MP_MUW, MP_MUA, MP_MUG = 12, 13, 14, 15, 16, 17
MP_W0, MP_A0, MP_KK, MP_KA, MP_RK, MP_LNW, MP_LNB = 19, 20, 21, 22, 23, 24, 25
MP_POS = 26
MP_N = 26 + 32
C_ID, C_BONES, C_CAUS, C_DELTA, C_CMASK = 0, 128, 256, 320, 384
C_REV = 896
CN = 896 + 128


def make_consts():
    c = np.zeros((128, CN), np.float32)
    c[:, C_ID:C_ID + 128] = np.eye(128)
    p = np.arange(128)
    c[:, C_BONES:C_BONES + 128] = (p[:, None] // 64 == p[None, :] // 64)
    s = np.arange(64)
    c[0:64, C_CAUS:C_CAUS + 64] = (s[:, None] <= s[None, :])
    c[:, C_DELTA:C_DELTA + 64] = ((p[:, None] % 64) == s[None, :])
    cm = np.ones((128, 512), np.float32)
    cm[:, 0::64] = 0.0
    c[:, C_CMASK:C_CMASK + 512] = cm
    c[:, C_REV:C_REV + 128] = np.eye(128)[::-1]
    return c


class MixCtx:
    def __init__(self, m, mix_in, y_out, mp, cst):
        self.m = m
        self.mix_in = mix_in
        self.y_out = y_out
        self.banks = Rot([m.ps([128, 512], F32, "mbank%d" % i) for i in range(8)])
        self.mp = m.sb([128, MP_N], F32, "mp_s")
        m.dma("sp", self.mp[:], mp[:], reads=[mp], writes=[self.mp])
        self.cst = m.sb([128, CN], F32, "cst_s")
        m.dma("sp", self.cst[:], cst[:], reads=[cst], writes=[self.cst])
        self.dq = Rot(["sp", "act"])
        self.arena = Arena(m, m.nc.sbuf_bytes_remaining - 1024)

    def col(self, c, rows=128):
        return self.mp[0:rows, c:c + 1]


def bcast_mid(base, reps):
    a = base.ap
    return bass.AP(tensor=base.tensor, offset=base.offset, ap=[[a[0][0], a[0][1]], [a[1][0], a[1][1]], [0, reps]])


def bcast_outer(t_ap, reps):
    a = t_ap.ap
    return bass.AP(tensor=t_ap.tensor, offset=t_ap.offset, ap=[[a[0][0], a[0][1]], [0, reps], [a[1][0], a[1][1]]])


def mixer_lru(mx, wa_d, wi_d, ntiles=S // 512):
    m, mp = mx.m, mx.mp
    ar = mx.arena
    ar.reset()
    c8 = ar.sb([128, 2], F32, "lru_c8")
    m.op("act", lambda e: e.activation(out=c8[:, 0:1], in_=mx.col(MP_LLAM), func=AF.Exp, scale=-1.0), [mp], [c8])
    m.op("act", lambda e: e.activation(out=c8[:, 0:1], in_=c8[:, 0:1], func=AF.Ln, bias=1.0), [c8], [c8])
    m.op("dve", lambda e: e.tensor_scalar(out=c8[:, 1:2], in0=c8[:, 0:1], scalar1=-16.0, scalar2=None, op0=ALU.mult), [c8], [c8])
    m.op("dve", lambda e: e.tensor_scalar(out=c8[:, 0:1], in0=c8[:, 0:1], scalar1=-8.0, scalar2=None, op0=ALU.mult), [c8], [c8])
    wa = ar.sb([128, 128], BF16, "lru_wa_s")
    wi = ar.sb([128, 128], BF16, "lru_wi_s")
    m.dma("pool", wa[:], wa_d[:], reads=[wa_d], writes=[wa])
    m.dma("pool", wi[:], wi_d[:], reads=[wi_d], writes=[wi])
    xt = Rot([ar.sb([128, 515], F32, "lru_xt%d" % i) for i in range(2)])
    F = Rot([ar.sb([128, 512], F32, "lru_f%d" % i) for i in range(10)])
    hb = Rot([ar.sb([128, 512], F32, "lru_h%d" % i) for i in range(2)])
    xcb = ar.sb([128, 512], BF16, "lru_xcb")
    prev_x, prev_h = None, None
    for ti in range(ntiles):
        t0 = ti * 512
        x = xt.next()
        m.dma("sp", x[:, 3:515], mx.mix_in[R_LX:R_LX + 128, t0:t0 + 512], reads=[mx.mix_in], writes=[x])
        if prev_x is None:
            m.op("dve", lambda e: e.memset(x[:, 0:3], 0.0), [], [x])
        else:
            m.op("dve", lambda e: e.tensor_copy(out=x[:, 0:3], in_=prev_x[:, 512:515]), [prev_x], [x])
        prev_x = x
        xc = F.next()
        m.op("dve", lambda e: e.tensor_scalar(out=xc[:], in0=x[:, 3:515], scalar1=mx.col(MP_LCW + 3), scalar2=mx.col(MP_LCB),
                                              op0=ALU.mult, op1=ALU.add), [x, mp], [xc])
        for k in range(3):
            m.op("dve", lambda e: e.scalar_tensor_tensor(out=xc[:], in0=x[:, k:k + 512], scalar=mx.col(MP_LCW + k), in1=xc[:],
                                                         op0=ALU.mult, op1=ALU.add), [x, mp, xc], [xc])
        m.op("act", lambda e: e.activation(out=xcb[:], in_=xc[:], func=AF.Copy), [xc], [xcb])
        pa, pi = mx.banks.next(), mx.banks.next()
        m.op("pe", lambda e: e.matmul(pa[:], lhsT=wa[:], rhs=xcb[:], start=True, stop=True), [wa, xcb], [pa])
        m.op("pe", lambda e: e.matmul(pi[:], lhsT=wi[:], rhs=xcb[:], start=True, stop=True), [wi, xcb], [pi])
        r, ig, a, a2 = F.next(), F.next(), F.next(), F.next()
        m.op("act", lambda e: e.activation(out=r[:], in_=pa[:], func=AF.Sigmoid, bias=mx.col(MP_LBA)), [pa, mp], [r])
        m.op("act", lambda e: e.activation(out=ig[:], in_=pi[:], func=AF.Sigmoid, bias=mx.col(MP_LBI)), [pi, mp], [ig])
        m.op("act", lambda e: e.activation(out=a[:], in_=r[:], func=AF.Exp, scale=c8[:, 0:1]), [r, c8], [a])
        m.op("act", lambda e: e.activation(out=a2[:], in_=r[:], func=AF.Exp, scale=c8[:, 1:2]), [r, c8], [a2])
        m.op("dve", lambda e: e.tensor_scalar(out=a2[:], in0=a2[:], scalar1=-1.0, scalar2=1.0, op0=ALU.mult, op1=ALU.add), [a2], [a2])
        m.op("act", lambda e: e.activation(out=a2[:], in_=a2[:], func=AF.Sqrt), [a2], [a2])
        m.op("dve", lambda e: e.tensor_tensor(out=ig[:], in0=ig[:], in1=xc[:], op=ALU.mult), [ig, xc], [ig])
        m.op("dve", lambda e: e.tensor_tensor(out=ig[:], in0=ig[:], in1=a2[:], op=ALU.mult), [ig, a2], [ig])
        h = hb.next()
        init = 0.0 if prev_h is None else prev_h[:, 511:512]
        m.op("dve", lambda e: e.tensor_tensor_scan(out=h[:], data0=a[:], data1=ig[:], initial=init, op0=ALU.mult, op1=ALU.add),
             [a, ig] + ([prev_h] if prev_h is not None else []), [h])
        prev_h = h
        g = F.next()
        m.dma("sp", g[:], mx.mix_in[R_LG:R_LG + 128, t0:t0 + 512], reads=[mx.mix_in], writes=[g])
        gl = F.next()
        m.op("act", lambda e: e.activation(out=gl[:], in_=g[:], func=AF.Gelu_apprx_tanh), [g], [gl])
        m.op("dve", lambda e: e.tensor_tensor(out=gl[:], in0=gl[:], in1=h[:], op=ALU.mult), [gl, h], [gl])
        m.dma(mx.dq.next(), mx.y_out[128:256, t0:t0 + 512], gl[:], reads=[gl], writes=[mx.y_out])


def mixer_gla(mx, wgk_d, ntiles=S // 512):
    m, mp, cst = mx.m, mx.mp, mx.cst
    ar = mx.arena
    ar.reset()
    wgk = ar.sb([16, 64], F32, "gla_wgk_s")
    m.dma("sp", wgk[:], wgk_d[:], reads=[wgk_d], writes=[wgk])
    nb = ar.sb([64, 1], F32, "gla_nb")
    m.op("dve", lambda e: e.tensor_scalar(out=nb[:], in0=mx.col(MP_GB, 64), scalar1=-1.0, scalar2=None, op0=ALU.mult), [mp], [nb])
    St = ar.sb([64, 128], F32, "gla_S")
    Sb = ar.sb([64, 128], BF16, "gla_Sb")
    m.op("dve", lambda e: e.memset(St[:], 0.0), [], [St])
    m.op("dve", lambda e: e.memset(Sb[:], 0.0), [], [Sb])
    ones = ar.sb([128, 128], F32, "gla_ones")
    m.op("dve", lambda e: e.memset(ones[:], 1.0), [], [ones])
    F64 = Rot([ar.sb([64, 512], F32, "gla_f%d" % i) for i in range(12)])
    F128 = Rot([ar.sb([128, 512], F32, "gla_F%d" % i) for i in range(8)])
    B16 = Rot([ar.sb([64, 512], BF16, "gla_b%d" % i) for i in range(4)])
    lrp = Rot([ar.sb([16, 512], F32, "gla_lr%d" % i) for i in range(2)])
    khT = Rot([ar.sb([64, 64], BF16, "gla_khT%d" % i) for i in range(3)])
    vT = Rot([ar.sb([64, 128], BF16, "gla_vT%d" % i) for i in range(3)])
    Am = Rot([ar.sb([64, 64], BF16, "gla_A%d" % i) for i in range(3)])
    for ti in range(ntiles):
        t0 = ti * 512
        q, k, lr = F64.next(), F64.next(), lrp.next()
        v, g = F128.next(), F128.next()
        m.dma("sp", q[:], mx.mix_in[R_GQ:R_GQ + 64, t0:t0 + 512], reads=[mx.mix_in], writes=[q])
        m.dma("sp", k[:], mx.mix_in[R_GK:R_GK + 64, t0:t0 + 512], reads=[mx.mix_in], writes=[k])
        m.dma("sp", v[:], mx.mix_in[R_GV:R_GV + 128, t0:t0 + 512], reads=[mx.mix_in], writes=[v])
        m.dma("sp", g[:], mx.mix_in[R_GG:R_GG + 128, t0:t0 + 512], reads=[mx.mix_in], writes=[g])
        m.dma("sp", lr[:], mx.mix_in[R_GLR:R_GLR + 16, t0:t0 + 512], reads=[mx.mix_in], writes=[lr])
        pz = mx.banks.next()
        m.op("pe", lambda e: e.matmul(pz[0:64, :], lhsT=wgk[:], rhs=lr[:], start=True, stop=True), [wgk, lr], [pz])
        la, Bc, E, Ei, kh = F64.next(), F64.next(), F64.next(), F64.next(), F64.next()
        m.op("act", lambda e: e.activation(out=la[:], in_=pz[0:64, :], func=AF.Exp, scale=-1.0, bias=nb[:]), [pz, nb], [la])
        m.op("act", lambda e: e.activation(out=la[:], in_=la[:], func=AF.Ln, bias=1.0), [la], [la])
        m.op("dve", lambda e: e.tensor_scalar(out=la[:], in0=la[:], scalar1=-1.0 / 16.0, scalar2=None, op0=ALU.mult), [la], [la])
        m.op("dve", lambda e: e.tensor_tensor_scan(out=Bc[:], data0=cst[0:64, C_CMASK:C_CMASK + 512], data1=la[:], initial=0.0,
                                                   op0=ALU.mult, op1=ALU.add), [cst, la], [Bc])
        m.op("act", lambda e: e.activation(out=E[:], in_=Bc[:], func=AF.Exp), [Bc], [E])
        m.op("act", lambda e: e.activation(out=Ei[:], in_=Bc[:], func=AF.Exp, scale=-1.0), [Bc], [Ei])
        qt, kt = B16.next(), B16.next()
        m.op("dve", lambda e: e.scalar_tensor_tensor(out=qt[:], in0=q[:], scalar=0.125, in1=E[:], op0=ALU.mult, op1=ALU.mult), [q, E], [qt])
        m.op("dve", lambda e: e.tensor_tensor(out=kt[:], in0=k[:], in1=Ei[:], op=ALU.mult), [k, Ei], [kt])
        for c in range(8):
            cs = slice(c * 64, (c + 1) * 64)
            m.op("dve", lambda e: e.scalar_tensor_tensor(out=kh[:, cs], in0=k[:, cs], scalar=E[:, c * 64 + 63:c * 64 + 64], in1=Ei[:, cs],
                                                         op0=ALU.mult, op1=ALU.mult), [k, E, Ei], [kh])
        o = F128.next()
        for c in range(8):
            cs = slice(c * 64, (c + 1) * 64)
            pT = mx.banks.next()
            m.op("pe", lambda e: e.transpose(out=pT[0:64, 0:64], in_=kh[:, cs], identity=cst[0:64, C_ID:C_ID + 64]), [kh, cst], [pT])
            m.op("pe", lambda e: e.transpose(out=pT[0:64, 64:192], in_=v[:, cs], identity=cst[:, C_ID:C_ID + 128]), [v, cst], [pT])
            kT_, vT_ = khT.next(), vT.next()
            m.op("act", lambda e: e.activation(out=kT_[:], in_=pT[0:64, 0:64], func=AF.Copy), [pT], [kT_])
            m.op("act", lambda e: e.activation(out=vT_[:], in_=pT[0:64, 64:192], func=AF.Copy), [pT], [vT_])
            pS = mx.banks.next()
            m.op("pe", lambda e: e.matmul(pS[0:64, 0:64], lhsT=kt[:, cs], rhs=qt[:, cs], start=True, stop=True), [kt, qt], [pS])
            A = Am.next()
            m.op("dve", lambda e: e.tensor_tensor(out=A[:], in0=pS[0:64, 0:64], in1=cst[0:64, C_CAUS:C_CAUS + 64], op=ALU.mult), [pS, cst], [A])
            pO = mx.banks.next()
            m.op("pe", lambda e: e.matmul(pO[:, 0:64], lhsT=vT_[:], rhs=A[:], start=True, stop=False), [vT_, A], [pO])
            m.op("pe", lambda e: e.matmul(pO[:, 0:64], lhsT=Sb[:], rhs=qt[:, cs], start=False, stop=True), [Sb, qt], [pO])
            m.op("act", lambda e: e.activation(out=o[:, cs], in_=pO[:, 0:64], func=AF.Copy), [pO], [o])
            pD = mx.banks.next()
            m.op("pe", lambda e: e.matmul(pD[0:64, 0:128], lhsT=kT_[:], rhs=vT_[:], start=True, stop=True), [kT_, vT_], [pD])
            m.op("dve", lambda e: e.scalar_tensor_tensor(out=St[:], in0=St[:], scalar=E[:, c * 64 + 63:c * 64 + 64], in1=pD[0:64, 0:128],
                                                         op0=ALU.mult, op1=ALU.add), [St, E, pD], [St])
            m.op("act", lambda e: e.activation(out=Sb[:], in_=St[:], func=AF.Copy), [St], [Sb])
        sq = F128.next()
        m.op("act", lambda e: e.activation(out=sq[:], in_=o[:], func=AF.Square), [o], [sq])
        pn = mx.banks.next()
        m.op("pe", lambda e: e.matmul(pn[:], lhsT=ones[:], rhs=sq[:], start=True, stop=True), [ones, sq], [pn])
        m.op("act", lambda e: e.activation(out=sq[:], in_=pn[:], func=AF.Sqrt, scale=1.0 / 128, bias=1e-6), [pn], [sq])
        m.op("dve", lambda e: e.reciprocal(out=sq[:], in_=sq[:]), [sq], [sq])
        m.op("dve", lambda e: e.scalar_tensor_tensor(out=o[:], in0=o[:], scalar=mx.col(MP_GGAIN), in1=sq[:], op0=ALU.mult, op1=ALU.mult),
             [o, mp, sq], [o])
        m.op("act", lambda e: e.activation(out=g[:], in_=g[:], func=AF.Silu), [g], [g])
        m.op("dve", lambda e: e.tensor_tensor(out=o[:], in0=o[:], in1=g[:], op=ALU.mult), [o, g], [o])
        m.dma(mx.dq.next(), mx.y_out[0:128, t0:t0 + 512], o[:], reads=[o], writes=[mx.y_out])


def mixer_rwkv(mx, wl_d, al_d, gl_d, ntiles=S // 512):
    m, mp, cst = mx.m, mx.mp, mx.cst
    ar = mx.arena
    ar.reset()
    wl = ar.sb([96, 128], F32, "rw_wl_s")
    al = ar.sb([96, 128], F32, "rw_al_s")
    gl = ar.sb([128, 2, 128], F32, "rw_gl_s")
    m.dma("sp", wl[:], wl_d[:], reads=[wl_d], writes=[wl])
    m.dma("sp", al[:], al_d[:], reads=[al_d], writes=[al])
    m.dma("sp", gl[:], gl_d[:, :].rearrange("(k p) c -> p k c", p=128), reads=[gl_d], writes=[gl])
    bones = cst[:, C_BONES:C_BONES + 128]
    St = ar.sb([128, 64], F32, "rw_S")
    m.op("dve", lambda e: e.memset(St[:], 0.0), [], [St])
    S2 = bass.AP(tensor=St.h, offset=0, ap=[[64, 128], [0, 2], [1, 64]])
    P2 = ar.sb([128, 2, 64], F32, "rw_P2")
    ft = Rot([ar.sb([128, 513], F32, "rw_ft%d" % i) for i in range(8)])
    prev = {}
    F = Rot([ar.sb([128, 512], F32, "rw_f%d" % i) for i in range(14)])
    XQ = Rot([ar.sb([128, 5, 512], F32, "rw_xq%d" % i) for i in range(2)])
    VM = Rot([ar.sb([128, 512], F32, "rw_vm%d" % i) for i in range(2)])
    SAZ = Rot([ar.sb([128, 512, 2], F32, "rw_saz%d" % i) for i in range(2)])
    Dt = Rot([ar.sb([128, 512], F32, "rw_D%d" % i) for i in range(4)])
    BC = Rot([ar.sb([128, 5, 512], F32, "rw_bc%d" % i) for i in range(2)])
    delta = cst[:, C_DELTA:C_DELTA + 64]

    def shifted(name, row0, rows, mucol, t0):
        f = ft.next()
        m.dma("sp", f[0:rows, 1:513], mx.mix_in[row0:row0 + rows, t0:t0 + 512], reads=[mx.mix_in], writes=[f])
        if name not in prev:
            m.op("dve", lambda e: e.memset(f[0:rows, 0:1], 0.0), [], [f])
        else:
            p = prev[name]
            m.op("dve", lambda e: e.tensor_copy(out=f[0:rows, 0:1], in_=p[0:rows, 512:513]), [p], [f])
        prev[name] = f
        d = F.next()
        m.op("dve", lambda e: e.tensor_tensor(out=d[0:rows, :], in0=f[0:rows, 0:512], in1=f[0:rows, 1:513], op=ALU.subtract), [f], [d])
        m.op("dve", lambda e: e.scalar_tensor_tensor(out=d[0:rows, :], in0=d[0:rows, :], scalar=mp[0:rows, mucol:mucol + 1],
                                                     in1=f[0:rows, 1:513], op0=ALU.mult, op1=ALU.add), [d, mp, f], [d])
        return d

    def bsum(src, scale=None):
        ps = mx.banks.next()
        m.op("pe", lambda e: e.matmul(ps[:], lhsT=bones, rhs=src[:], start=True, stop=True), [cst, src], [ps])
        return ps

    for ti in range(ntiles):
        t0 = ti * 512
        r = shifted("r", R_RR, 128, MP_MUR, t0)
        k = shifted("k", R_RK, 128, MP_MUK, t0)
        v = shifted("v", R_RV, 128, MP_MUV, t0)
        vm = VM.next()
        m.op("act", lambda e: e.activation(out=vm[:], in_=v[:], func=AF.Copy), [v], [vm])
        xw = shifted("xw", R_XW, 96, MP_MUW, t0)
        xa = shifted("xa", R_XA, 96, MP_MUA, t0)
        xg0 = shifted("xg0", R_XG, 128, MP_MUG, t0)
        xg1 = shifted("xg1", R_XG + 128, 128, MP_MUG + 1, t0)
        xq = XQ.next()
        kk_, wr_, w_, nkka_, kp_ = (xq[:, i, :] for i in range(5))
        m.op("act", lambda e: e.activation(out=xw[0:96, :], in_=xw[0:96, :], func=AF.Tanh), [xw], [xw])
        pw = mx.banks.next()
        m.op("pe", lambda e: e.matmul(pw[:], lhsT=wl[:], rhs=xw[0:96, :], start=True, stop=True), [wl, xw], [pw])
        m.op("act", lambda e: e.activation(out=w_, in_=pw[:], func=AF.Sigmoid, bias=mx.col(MP_W0)), [pw, mp], [xq])
        m.op("act", lambda e: e.activation(out=w_, in_=w_, func=AF.Exp, scale=-0.6065306597126334), [xq], [xq])
        pa = mx.banks.next()
        m.op("pe", lambda e: e.matmul(pa[:], lhsT=al[:], rhs=xa[0:96, :], start=True, stop=True), [al, xa], [pa])
        a = F.next()
        m.op("act", lambda e: e.activation(out=a[:], in_=pa[:], func=AF.Sigmoid, bias=mx.col(MP_A0)), [pa, mp], [a])
        m.op("act", lambda e: e.activation(out=xg0[:], in_=xg0[:], func=AF.Sigmoid), [xg0], [xg0])
        m.op("act", lambda e: e.activation(out=xg1[:], in_=xg1[:], func=AF.Sigmoid), [xg1], [xg1])
        pg = mx.banks.next()
        m.op("pe", lambda e: e.matmul(pg[:], lhsT=gl[:, 0, :], rhs=xg0[:], start=True, stop=False), [gl, xg0], [pg])
        m.op("pe", lambda e: e.matmul(pg[:], lhsT=gl[:, 1, :], rhs=xg1[:], start=False, stop=True), [gl, xg1], [pg])
        gg = F.next()
        m.op("act", lambda e: e.activation(out=gg[:], in_=pg[:], func=AF.Copy), [pg], [gg])
        m.op("dve", lambda e: e.tensor_scalar(out=kk_, in0=k[:], scalar1=mx.col(MP_KK), scalar2=None, op0=ALU.mult), [k, mp], [xq])
        sq = F.next()
        m.op("act", lambda e: e.activation(out=sq[:], in_=kk_, func=AF.Square), [xq], [sq])
        pn = bsum(sq)
        m.op("act", lambda e: e.activation(out=sq[:], in_=pn[:], func=AF.Sqrt), [pn], [sq])
        m.op("dve", lambda e: e.tensor_scalar(out=sq[:], in0=sq[:], scalar1=1e-12, scalar2=None, op0=ALU.max), [sq], [sq])
        m.op("dve", lambda e: e.reciprocal(out=sq[:], in_=sq[:]), [sq], [sq])
        m.op("dve", lambda e: e.tensor_tensor(out=kk_, in0=kk_, in1=sq[:], op=ALU.mult), [xq, sq], [xq])
        m.op("dve", lambda e: e.tensor_scalar(out=kp_, in0=a[:], scalar1=-1.0, scalar2=mx.col(MP_KA), op0=ALU.add, op1=ALU.mult), [a, mp], [xq])
        m.op("dve", lambda e: e.scalar_tensor_tensor(out=kp_, in0=kp_, scalar=1.0, in1=k[:], op0=ALU.add, op1=ALU.mult), [xq, k], [xq])
        m.op("dve", lambda e: e.scalar_tensor_tensor(out=nkka_, in0=kk_, scalar=-1.0, in1=a[:], op0=ALU.mult, op1=ALU.mult), [xq, a], [xq])
        m.op("dve", lambda e: e.tensor_tensor(out=wr_, in0=w_, in1=r[:], op=ALU.mult), [xq, r], [xq])
        p1, p2, p3 = F.next(), F.next(), F.next()
        m.op("dve", lambda e: e.tensor_tensor(out=p1[:], in0=nkka_, in1=r[:], op=ALU.mult), [xq, r], [p1])
        m.op("dve", lambda e: e.tensor_tensor(out=p2[:], in0=kp_, in1=r[:], op=ALU.mult), [xq, r], [p2])
        m.op("dve", lambda e: e.tensor_scalar(out=p3[:], in0=p2[:], scalar1=mx.col(MP_RK), scalar2=None, op0=ALU.mult), [p2, mp], [p3])
        c1, c2, c3 = bsum(p1), bsum(p2), bsum(p3)
        m.op("act", lambda e: e.activation(out=p1[:], in_=c1[:], func=AF.Copy), [c1], [p1])
        m.op("act", lambda e: e.activation(out=p2[:], in_=c2[:], func=AF.Copy), [c2], [p2])
        m.op("act", lambda e: e.activation(out=p3[:], in_=c3[:], func=AF.Copy), [c3], [p3])
        saz = SAZ.next()
        for gi in range(64):
            bc = BC.next()
            for qi in range(5):
                dt_ = Dt.next()
                m.op("pool", lambda e: e.tensor_tensor(out=dt_[:, :].rearrange("p (t j) -> p t j", j=64),
                                                       in0=bcast_mid(xq[:, qi, gi * 8:gi * 8 + 8], 64),
                                                       in1=bcast_outer(delta, 8), op=ALU.mult), [xq, cst], [dt_])
                pb = mx.banks.next()
                m.op("pe", lambda e: e.matmul(pb[:], lhsT=bones, rhs=dt_[:], start=True, stop=True), [cst, dt_], [pb])
                m.op("act", lambda e: e.activation(out=bc[:, qi, :], in_=pb[:], func=AF.Copy), [pb], [bc])
            for tt in range(8):
                t = gi * 8 + tt
                js = slice(tt * 64, (tt + 1) * 64)
                m.op("dve", lambda e: e.tensor_tensor(out=P2[:], in0=S2, in1=bc[:, 0:2, js], op=ALU.mult), [St, bc], [P2])
                m.op("dve", lambda e: e.tensor_reduce(out=saz[:, t, :], in_=P2[:], axis=AX.X, op=ALU.add), [P2], [saz])
                m.op("dve", lambda e: e.tensor_tensor(out=St[:], in0=St[:], in1=bc[:, 2, js], op=ALU.mult), [St, bc], [St])
                m.op("dve", lambda e: e.scalar_tensor_tensor(out=St[:], in0=bc[:, 3, js], scalar=saz[:, t, 0:1], in1=St[:],
                                                             op0=ALU.mult, op1=ALU.add), [bc, saz, St], [St])
                m.op("dve", lambda e: e.scalar_tensor_tensor(out=St[:], in0=bc[:, 4, js], scalar=vm[:, t:t + 1], in1=St[:],
                                                             op0=ALU.mult, op1=ALU.add), [bc, vm, St], [St])
        y = F.next()
        m.op("pool", lambda e: e.tensor_tensor(out=y[:], in0=saz[:, :, 0], in1=p1[:], op=ALU.mult), [saz, p1], [y])
        m.op("pool", lambda e: e.tensor_tensor(out=y[:], in0=y[:], in1=saz[:, :, 1], op=ALU.add), [y, saz], [y])
        m.op("pool", lambda e: e.tensor_tensor(out=p2[:], in0=p2[:], in1=vm[:], op=ALU.mult), [p2, vm], [p2])
        m.op("pool", lambda e: e.tensor_tensor(out=y[:], in0=y[:], in1=p2[:], op=ALU.add), [y, p2], [y])
        pm_ = bsum(y)
        m.op("pool", lambda e: e.tensor_copy(out=p1[:], in_=y[:]), [y], [p1])
        m.op("dve", lambda e: e.scalar_tensor_tensor(out=y[:], in0=pm_[:], scalar=-1.0 / 64, in1=p1[:], op0=ALU.mult, op1=ALU.add),
             [pm_, p1], [y])
        m.op("act", lambda e: e.activation(out=p1[:], in_=y[:], func=AF.Square), [y], [p1])
        pv = bsum(p1)
        m.op("act", lambda e: e.activation(out=p1[:], in_=pv[:], func=AF.Sqrt, scale=1.0 / 64, bias=64e-5), [pv], [p1])
        m.op("dve", lambda e: e.reciprocal(out=p1[:], in_=p1[:]), [p1], [p1])
        m.op("dve", lambda e: e.tensor_tensor(out=y[:], in0=y[:], in1=p1[:], op=ALU.mult), [y, p1], [y])
        m.op("dve", lambda e: e.tensor_scalar(out=y[:], in0=y[:], scalar1=mx.col(MP_LNW), scalar2=mx.col(MP_LNB), op0=ALU.mult, op1=ALU.add),
             [y, mp], [y])
        m.op("dve", lambda e: e.tensor_tensor(out=p3[:], in0=p3[:], in1=vm[:], op=ALU.mult), [p3, vm], [p3])
        m.op("dve", lambda e: e.tensor_tensor(out=y[:], in0=y[:], in1=p3[:], op=ALU.add), [y, p3], [y])
        m.op("dve", lambda e: e.tensor_tensor(out=y[:], in0=y[:], in1=gg[:], op=ALU.mult), [y, gg], [y])
        m.dma(mx.dq.next(), mx.y_out[384:512, t0:t0 + 512], y[:], reads=[y], writes=[mx.y_out])


def build_mixer(which=("gla", "lru", "nsa", "rwkv"), ntiles=S // 512):
    nc = bass.Bass("TRN2", target_bir_lowering=False)
    m = MK(nc)
    ei = lambda name, shape, dt=F32: m.dram(name, shape, dt, kind="ExternalInput")
    mix_in = ei("mix_in", [MIX_ROWS, S])
    mp = ei("mp", [128, MP_N])
    cst = ei("cst", [128, CN])
    y_out = m.dram("y_out", [512, S], F32, kind="ExternalOutput")
    mx = MixCtx(m, mix_in, y_out, mp, cst)
    if "lru" in which:
        mixer_lru(mx, ei("lru_wa", [128, 128]), ei("lru_wi", [128, 128]), ntiles)
    if "gla" in which:
        mixer_gla(mx, ei("gla_wgk", [16, 64]), ntiles)
    if "nsa" in which:
        d = {"k1": ei("nsa_k1", [4096, 128]), "k2": ei("nsa_k2", [128, 128]), "v1": ei("nsa_v1", [4096, 128]),
             "v2": ei("nsa_v2", [128, 128]), "rb": ei("nsa_rb", [32, 5]), "oh": ei("nsa_oh", [33, LV]),
             "ovl": ei("nsa_ovl", [128, 4 * 129]), "selmask": ei("nsa_selmask", [64 * 128, 128]), "gsel": ei("nsa_gsel", [3, 384])}
        mixer_nsa(mx, d, ntiles)
    if "rwkv" in which:
        mixer_rwkv(mx, ei("rw_wl", [96, 128]), ei("rw_al", [96, 128]), ei("rw_gl", [256, 128]), ntiles)
    m.finish()
    return nc, m


def pack_mixer_inputs(l, b, j, projT, inp):
    mi = np.zeros((MIX_ROWS, S), np.float32)
    mi[R_GQ:R_GQ + 64] = projT[64 * j:64 * j + 64]
    mi[R_GK:R_GK + 64] = projT[256 + 64 * j:256 + 64 * j + 64]
    mi[R_GV:R_GV + 128] = projT[512 + 128 * j:512 + 128 * j + 128]
    mi[R_GG:R_GG + 128] = projT[1024 + 128 * j:1024 + 128 * j + 128]
    mi[R_GLR:R_GLR + 16] = projT[1536:1552]
    mi[R_LX:R_LX + 128] = projT[1552 + 128 * j:1552 + 128 * j + 128]
    mi[R_LG:R_LG + 128] = projT[2064 + 128 * j:2064 + 128 * j + 128]
    mi[R_NQ:R_NQ + 512] = projT[2576:3088]
    mi[R_NKV:R_NKV + 768] = projT[3088:3856]
    mi[R_NQO:R_NQO + 128] = projT[2576 + 128 * j:2576 + 128 * j + 128]
    for gi_ in range(3):
        mi[R_NG + gi_] = projT[3856 + gi_ * 4 + j]
    f0 = 3868
    mi[R_RR:R_RR + 128] = projT[f0 + 128 * j:f0 + 128 * j + 128]
    mi[R_RK:R_RK + 128] = projT[f0 + 512 + 128 * j:f0 + 512 + 128 * j + 128]
    mi[R_RV:R_RV + 128] = projT[f0 + 1024 + 128 * j:f0 + 1024 + 128 * j + 128]
    mi[R_XW:R_XW + 96] = projT[f0 + 1536:f0 + 1632]
    mi[R_XA:R_XA + 96] = projT[f0 + 1632:f0 + 1728]
    mi[R_XG:R_XG + 256] = projT[f0 + 1728:f0 + 1984]
    mp = np.zeros((128, MP_N), np.float32)
    sl = slice(128 * j, 128 * j + 128)
    mp[0:64, MP_GB] = inp["gla_b_gk"][l][64 * j:64 * j + 64]
    mp[:, MP_GGAIN] = inp["gla_out_norm"][l]
    for k in range(4):
        mp[:, MP_LCW + k] = inp["lru_conv_w"][l][k, sl]
    mp[:, MP_LCB] = inp["lru_conv_b"][l][sl]
    mp[:, MP_LBA] = inp["lru_b_a"][l][sl]
    mp[:, MP_LBI] = inp["lru_b_i"][l][sl]
    mp[:, MP_LLAM] = inp["lru_lambda"][l][sl]
    mp[:, MP_NQG] = inp["nsa_q_norm"][l]
    mp[:, MP_NKG] = inp["nsa_k_norm"][l]
    mu = inp["rwkv_mu"][l]
    mp[:, MP_MUR] = mu[128 * j:128 * j + 128]
    mp[:, MP_MUK] = mu[512 + 128 * j:512 + 128 * j + 128]
    mp[:, MP_MUV] = mu[1024 + 128 * j:1024 + 128 * j + 128]
    mp[0:96, MP_MUW] = mu[1536:1632]
    mp[0:96, MP_MUA] = mu[1632:1728]
    mp[:, MP_MUG] = mu[1728:1856]
    mp[:, MP_MUG + 1] = mu[1856:1984]
    mp[:, MP_W0] = inp["rwkv_w0"][l][sl]
    mp[:, MP_A0] = inp["rwkv_a0"][l][sl]
    mp[:, MP_KK] = inp["rwkv_k_k"][l][sl]
    mp[:, MP_KA] = inp["rwkv_k_a"][l][sl]
    mp[:, MP_RK] = inp["rwkv_r_k"][l].reshape(512)[sl]
    mp[:, MP_LNW] = inp["rwkv_ln_w"][l][sl]
    mp[:, MP_LNB] = inp["rwkv_ln_b"][l][sl]
    mp[:, MP_POS:MP_POS + 32] = inp["nsa_cmp_pos"][l].T
    rb = inp["rel_bias"]
    d = {"mix_in": mi, "mp": mp, "nsa_k1": inp["nsa_cmp_k1"][l], "nsa_k2": inp["nsa_cmp_k2"][l], "nsa_v1": inp["nsa_cmp_v1"][l],
         "nsa_v2": inp["nsa_cmp_v2"][l], "nsa_rb": np.ascontiguousarray(np.concatenate([rb, rb[:, j:j + 1]], axis=1)),
         "gla_wgk": np.ascontiguousarray(inp["gla_w_gk"][l][:, 64 * j:64 * j + 64]),
         "lru_wa": np.ascontiguousarray(inp["lru_w_a"][l][j]), "lru_wi": np.ascontiguousarray(inp["lru_w_i"][l][j]),
         "rw_wl": np.ascontiguousarray(inp["rwkv_w_lora"][l][:, sl]), "rw_al": np.ascontiguousarray(inp["rwkv_a_lora"][l][:, sl]),
         "rw_gl": np.ascontiguousarray(inp["rwkv_g_lora"][l][:, sl])}
    return d


NEG = -30000.0
LV_SEL = 4592
LV_WIN = 1536
LV = LV_SEL + LV_WIN
NCMP = 511


def _t5_bucket(n):
    n = np.asarray(n)
    nf = np.maximum(n, 1).astype(np.float32)
    large = 16 + (np.log(nf / np.float32(16)) / np.float32(np.log(128 / 16)) * np.float32(16)).astype(np.int32)
    large = np.minimum(large, 31)
    return np.where(n < 16, n, large)


def make_nsa_consts():
    oh = np.zeros((33, LV), np.float32)
    i = np.arange(LV_SEL)
    dist = i - 2063
    ok = dist >= 0
    b = _t5_bucket(np.maximum(dist, 0))
    oh[b[ok], i[ok]] += 1.0
    oh[31, i[ok]] -= 1.0
    oh[32, i[~ok]] = NEG
    i2 = np.arange(LV_WIN)
    dist = i2 - 511
    ok = (dist >= 0) & (dist < 512)
    b = _t5_bucket(np.maximum(dist, 0))
    oh[b[ok], LV_SEL + i2[ok]] += 1.0
    oh[31, LV_SEL + i2[ok]] -= 1.0
    oh[32, LV_SEL + i2[~ok]] = NEG
    n = np.arange(512)
    j = np.arange(128)
    ov = ((16 * n[:, None] < 64 * j[None, :] + 64) & (16 * n[:, None] + 32 > 64 * j[None, :])).astype(np.float32)
    ov[511] = 0.0
    ovl = np.zeros((128, 4, 129), np.float32)
    ovl[:, :, 0:128] = ov.reshape(4, 128, 128).transpose(1, 0, 2)
    ovl[:, :, 128] = 1.0
    ovl[127, 3, :] = 0.0
    sm = np.zeros((64, 128, 128), np.float32)
    for st in range(64):
        pos = st * 128 + np.arange(128)
        cur = pos // 64
        blk = np.arange(128)[None, :]
        forced = (blk == 0) | (blk == cur[:, None]) | (blk == cur[:, None] - 1)
        fut = blk > cur[:, None]
        sm[st] = np.where(forced, 1e9, np.where(fut, -1e9, 0.0))
    gsel = np.zeros((3, 3, 128), np.float32)
    for g in range(3):
        gsel[g, g, :] = 1.0
    return {"nsa_oh": oh, "nsa_ovl": ovl.reshape(128, 4 * 129), "nsa_selmask": sm.reshape(64 * 128, 128),
            "nsa_gsel": gsel.reshape(3, 384)}


def mixer_nsa(mx, d, nqt=S // 512):
    m, mp, cst = mx.m, mx.mp, mx.cst
    ar = mx.arena
    ar.reset()
    F = Rot([ar.sb([128, 516], F32, "nsaF%d" % i) for i in range(12)])
    Bp = Rot([ar.sb([128, 512], BF16, "nsaB%d" % i) for i in range(6)])
    acc = [mx.banks.items[0], mx.banks.items[1], mx.banks.items[2]]
    rot = Rot(mx.banks.items[3:8])
    ident = cst[:, C_ID:C_ID + 128]
    onesf = ar.sb([128, 128], F32, "nsa_onesf")
    onesb = ar.sb([128, 128], BF16, "nsa_onesb")
    identb = ar.sb([128, 128], BF16, "nsa_identb")
    m.op("dve", lambda e: e.memset(onesf[:], 1.0), [], [onesf])
    m.op("dve", lambda e: e.memset(onesb[:], 1.0), [], [onesb])
    m.op("dve", lambda e: e.tensor_copy(out=identb[:], in_=ident), [cst], [identb])
    gq = ar.sb([128, 1], F32, "nsa_gq")
    m.op("dve", lambda e: e.tensor_scalar(out=gq[:], in0=mx.col(MP_NQG), scalar1=128 ** -0.5, scalar2=None, op0=ALU.mult), [mp], [gq])

    def rms_rows(src, gaincol, dst, n=512):
        sq = F.next()
        m.op("act", lambda e: e.activation(out=sq[:, 0:n], in_=src.ap, func=AF.Square), [src_t(src)], [sq])
        pn = rot.next()
        m.op("pe", lambda e: e.matmul(pn[:, 0:n], lhsT=onesf[:], rhs=sq[:, 0:n], start=True, stop=True), [onesf, sq], [pn])
        m.op("act", lambda e: e.activation(out=sq[:, 0:n], in_=pn[:, 0:n], func=AF.Sqrt, scale=1.0 / 128, bias=1e-6), [pn], [sq])
        m.op("dve", lambda e: e.reciprocal(out=sq[:, 0:n], in_=sq[:, 0:n]), [sq], [sq])
        m.op("dve", lambda e: e.scalar_tensor_tensor(out=dst.ap, in0=src.ap, scalar=gaincol, in1=sq[:, 0:n], op0=ALU.mult, op1=ALU.mult),
             [src_t(src), mp, gq, sq], [src_t(dst)])

    kcT = ar.sb([128, 512], BF16, "nsa_kcT")
    vc_tm = ar.sb([128, 4, 128], BF16, "nsa_vctm")
    m.op("dve", lambda e: e.memset(kcT[:], 0.0), [], [kcT])
    m.op("dve", lambda e: e.memset(vc_tm[:], 0.0), [], [vc_tm])
    w1 = [ar.sb([128, 32, 128], BF16, "nsa_w1%d" % i) for i in range(2)]
    w2 = [ar.sb([128, 128], BF16, "nsa_w2%d" % i) for i in range(2)]
    posb = ar.sb([128, 32], BF16, "nsa_posb")
    m.op("dve", lambda e: e.tensor_copy(out=posb[:], in_=mp[:, MP_POS:MP_POS + 32]), [mp], [posb])
    cb = ar.sb([128, 2], F32, "nsa_cb")
    for i, (k1n, k2n) in enumerate((("k1", "k2"), ("v1", "v2"))):
        for l0 in range(0, 32, 16):
            m.dma("pool", w1[i][:, l0:l0 + 16, :], d[k1n][l0 * 128:(l0 + 16) * 128, :].rearrange("(l p) h -> p l h", p=128),
                  reads=[d[k1n]], writes=[w1[i]])
        m.dma("pool", w2[i][:], d[k2n][:], reads=[d[k2n]], writes=[w2[i]])
        pb = rot.next()
        for l in range(32):
            m.op("pe", lambda e: e.matmul(pb[:, 0:1], lhsT=w1[i][:, l, :], rhs=posb[:, l:l + 1], start=(l == 0), stop=(l == 31)),
                 [w1[i], posb], [pb])
        m.op("act", lambda e: e.activation(out=cb[:, i:i + 1], in_=pb[:, 0:1], func=AF.Copy), [pb], [cb])
    chunk = Rot([ar.sb([128, 2080], BF16, "nsa_chunk%d" % i) for i in range(2)])
    for a in range(4):
        nb_ = 128 if a < 3 else 127
        tok0 = a * 2048
        ntok = 16 * (nb_ - 1) + 32
        for i in range(2):
            ch = chunk.next()
            row0 = R_NKV + (0 if i == 0 else 128)
            m.dma("pool", ch[:, 0:ntok], mx.mix_in[row0:row0 + 128, tok0:tok0 + ntok], reads=[mx.mix_in], writes=[ch])
            ph = rot.next()
            for l in range(32):
                rhs = bass.AP(tensor=ch.h, offset=l, ap=[[2080, 128], [16, nb_]])
                m.op("pe", lambda e: e.matmul(ph[:, 0:nb_], lhsT=w1[i][:, l, :], rhs=rhs, start=(l == 0), stop=(l == 31)), [w1[i], ch], [ph])
            hb_ = Bp.next()
            m.op("act", lambda e: e.activation(out=hb_[:, 0:nb_], in_=ph[:, 0:nb_], func=AF.Gelu_apprx_tanh, bias=cb[:, i:i + 1]), [ph, cb], [hb_])
            po = rot.next()
            m.op("pe", lambda e: e.matmul(po[:, 0:nb_], lhsT=w2[i][:], rhs=hb_[:, 0:nb_], start=True, stop=True), [w2[i], hb_], [po])
            of = F.next()
            m.op("act", lambda e: e.activation(out=of[:, 0:nb_], in_=po[:, 0:nb_], func=AF.Copy), [po], [of])
            if i == 0:
                rms_rows(V(of, of[:, 0:nb_]), mx.col(MP_NKG), V(kcT, kcT[:, a * 128:a * 128 + nb_]), nb_)
            else:
                pt = rot.next()
                m.op("pe", lambda e: e.transpose(out=pt[0:nb_, 0:128], in_=of[:, 0:nb_], identity=ident), [of, cst], [pt])
                m.op("act", lambda e: e.activation(out=vc_tm[0:nb_, a, :], in_=pt[0:nb_, 0:128], func=AF.Copy), [pt], [vc_tm])
    rbx = ar.sb([33, 5], F32, "nsa_rbx")
    m.op("dve", lambda e: e.memset(rbx[32:33, :], 1.0), [], [rbx])
    m.dma("sp", rbx[0:32, :], d["rb"][:], reads=[d["rb"]], writes=[rbx])
    fv_d = m.dram("nsa_fv", [5, LV], F32)
    ohs = Rot([ar.sb([33, 512], F32, "nsa_oh%d" % i) for i in range(2)])
    for c0 in range(0, LV, 512):
        n = min(512, LV - c0)
        o_ = ohs.next()
        m.dma("sp", o_[:, 0:n], d["oh"][:, c0:c0 + n], reads=[d["oh"]], writes=[o_])
        pf = rot.next()
        m.op("pe", lambda e: e.matmul(pf[0:5, 0:n], lhsT=rbx[:], rhs=o_[:, 0:n], start=True, stop=True), [rbx, o_], [pf])
        fs = F.next()
        m.op("act", lambda e: e.activation(out=fs[0:5, 0:n], in_=pf[0:5, 0:n], func=AF.Copy), [pf], [fs])
        m.dma("sp", fv_d[:, c0:c0 + n], fs[0:5, 0:n], reads=[fs], writes=[fv_d])
    MC = ar.sb([128, 5, 2560], BF16, "nsa_MC")
    MS = ar.sb([128, 1024], BF16, "nsa_MS")
    MW = ar.sb([128, 1408], BF16, "nsa_MW")
    jrev = cst[:, C_REV:C_REV + 128]
    tz = Rot([ar.sb([128, 512], F32, "nsa_tz%d" % i) for i in range(2)])

    def toeplitz(dst_fn, head_off, off, pstep, W):
        for c0 in range(0, W, 512):
            n = min(512, W - c0)
            t_ = tz.next()
            src = bass.AP(tensor=fv_d.h, offset=head_off + off + c0, ap=[[pstep, 128], [1, n]])
            m.dma("sp", t_[:, 0:n], src, reads=[fv_d], writes=[t_])
            pz = rot.next()
            m.op("pe", lambda e: e.matmul(pz[:, 0:n], lhsT=jrev, rhs=t_[:, 0:n], start=True, stop=True), [cst, t_], [pz])
            dst, dt_ = dst_fn(c0, n)
            m.op("act", lambda e: e.activation(out=dst, in_=pz[:, 0:n], func=AF.Copy), [pz], [dt_])

    for h in range(5):
        toeplitz(lambda c0, n, h=h: (MC[:, h, c0:c0 + n], MC), h * LV, 0, 16, 2560)
    toeplitz(lambda c0, n: (MS[:, c0:c0 + n], MS), 4 * LV, 1552, 1, 1024)
    toeplitz(lambda c0, n: (MW[:, c0:c0 + n], MW), 4 * LV, LV_SEL, 1, 1408)
    ovl = ar.sb([128, 4, 129], BF16, "nsa_ovl_s")
    m.dma("pool", ovl[:], d["ovl"][:, :].rearrange("p (a c) -> p a c", a=4), reads=[d["ovl"]], writes=[ovl])
    gsel = ar.sb([3, 3, 128], F32, "nsa_gsel_s")
    m.dma("sp", gsel[:], d["gsel"][:, :].rearrange("p (a c) -> p a c", a=3), reads=[d["gsel"]], writes=[gsel])
    stair = ar.sb([128, S], BF16, "nsa_stair")
    m.op("dve", lambda e: e.tensor_copy(out=stair[:, :].rearrange("p (j r) -> p j r", r=64), in_=bcast_mid(identb[:, 0:128], 64)),
         [identb], [stair])
    kslcT = ar.sb([128, S], BF16, "nsa_kslcT")
    vslc_tm = ar.sb([128, S // 128, 128], BF16, "nsa_vslc")
    kwin = Rot([ar.sb([128, 512], BF16, "nsa_kwin%d" % i) for i in range(2)])
    vwin = Rot([ar.sb([128, 4, 128], BF16, "nsa_vwin%d" % i) for i in range(2)])
    qn = ar.sb([128, 5, 512], BF16, "nsa_qn")
    Pc = [[ar.sb([128, 512], BF16, "nsa_pc%d_%d" % (h, a)) for a in range(4)] for h in range(5)]
    MT = ar.sb([128, 512], BF16, "nsa_MT")
    sc = ar.sb([128, 128], F32, "nsa_sc")
    sc2 = ar.sb([128, 128], F32, "nsa_sc2")
    top = ar.sb([128, 16], F32, "nsa_top")
    rsi = ar.sb([128, 4], F32, "nsa_rsi")
    smk = Rot([ar.sb([128, 128], F32, "nsa_smk%d" % i) for i in range(2)])
    gate3 = ar.sb([3, 512], F32, "nsa_gate3")
    prev_kw, prev_vw = None, None
    osum_b = ar.sb([128, 512], F32, "nsa_osum")
    for qt in range(nqt):
        q0 = qt * 512
        for (row, which_) in ((256, "ks"), (512, "kw")):
            kf = F.next()
            m.dma("sp", kf[:, 0:512], mx.mix_in[R_NKV + row:R_NKV + row + 128, q0:q0 + 512], reads=[mx.mix_in], writes=[kf])
            if which_ == "ks":
                rms_rows(V(kf, kf[:, 0:512]), mx.col(MP_NKG), V(kslcT, kslcT[:, q0:q0 + 512]))
            else:
                kw_cur = kwin.next()
                rms_rows(V(kf, kf[:, 0:512]), mx.col(MP_NKG), V(kw_cur, kw_cur[:, :]))
        vw_cur = vwin.next()
        for (row, which_) in ((384, "vs"), (640, "vw")):
            vf = F.next()
            m.dma("sp", vf[:, 0:512], mx.mix_in[R_NKV + row:R_NKV + row + 128, q0:q0 + 512], reads=[mx.mix_in], writes=[vf])
            pt = rot.next()
            for c in range(4):
                m.op("pe", lambda e: e.transpose(out=pt[:, c * 128:(c + 1) * 128], in_=vf[:, c * 128:(c + 1) * 128], identity=ident),
                     [vf, cst], [pt])
            if which_ == "vs":
                m.op("act", lambda e: e.activation(out=vslc_tm[:, qt * 4:qt * 4 + 4, :], in_=pt[:, :].rearrange("p (c v) -> p c v", c=4),
                                                   func=AF.Copy), [pt], [vslc_tm])
            else:
                m.op("act", lambda e: e.activation(out=vw_cur[:], in_=pt[:, :].rearrange("p (c v) -> p c v", c=4), func=AF.Copy),
                     [pt], [vw_cur])
        for h in range(5):
            qf = F.next()
            r0 = R_NQ + h * 128 if h < 4 else R_NQO
            m.dma("sp", qf[:, 0:512], mx.mix_in[r0:r0 + 128, q0:q0 + 512], reads=[mx.mix_in], writes=[qf])
            rms_rows(V(qf, qf[:, 0:512]), gq[:], V(qn, qn[:, h, :]))
        m.dma("sp", gate3[:], mx.mix_in[R_NG:R_NG + 3, q0:q0 + 512], reads=[mx.mix_in], writes=[gate3])
        m.op("act", lambda e: e.activation(out=gate3[:], in_=gate3[:], func=AF.Sigmoid), [gate3], [gate3])
        osum = osum_b

        def finish_branch(g, po, prs, first):
            pg = rot.next()
            m.op("pe", lambda e: e.matmul(pg[:], lhsT=gsel[:, g, :], rhs=gate3[:], start=True, stop=True), [gsel, gate3], [pg])
            wv_ = F.next()
            m.op("dve", lambda e: e.tensor_scalar(out=wv_[:, 0:512], in0=prs[:], scalar1=1e-30, scalar2=None, op0=ALU.max), [prs], [wv_])
            m.op("dve", lambda e: e.reciprocal(out=wv_[:, 0:512], in_=wv_[:, 0:512]), [wv_], [wv_])
            m.op("dve", lambda e: e.tensor_tensor(out=wv_[:, 0:512], in0=wv_[:, 0:512], in1=pg[:], op=ALU.mult), [wv_, pg], [wv_])
            if first:
                m.op("dve", lambda e: e.tensor_tensor(out=osum[:, 0:512], in0=wv_[:, 0:512], in1=po[:], op=ALU.mult), [wv_, po], [osum])
            else:
                m.op("dve", lambda e: e.tensor_tensor(out=wv_[:, 0:512], in0=wv_[:, 0:512], in1=po[:], op=ALU.mult), [wv_, po], [wv_])
                m.op("dve", lambda e: e.tensor_tensor(out=osum[:, 0:512], in0=osum[:, 0:512], in1=wv_[:, 0:512], op=ALU.add), [osum, wv_], [osum])

        na = min(4, qt // 4 + 1)
        for h in range(5):
            for a in range(na):
                kn = 128 if a < 3 else 127
                dc = q0 - 2048 * a
                ps = rot.next()
                need_add = dc <= 2048
                m.op("pe", lambda e: e.matmul(ps[0:kn, :], lhsT=kcT[:, a * 128:a * 128 + kn], rhs=qn[:, h, :], start=True, stop=not need_add),
                     [kcT, qn], [ps])
                if need_add:
                    m.op("pe", lambda e: e.matmul(ps[0:kn, :], lhsT=identb[0:kn, 0:kn], rhs=MC[0:kn, h, dc:dc + 512], start=False, stop=True),
                         [identb, MC], [ps])
                m.op("act", lambda e: e.activation(out=Pc[h][a][0:kn, :], in_=ps[0:kn, :], func=AF.Exp), [ps], [Pc[h][a]])
        po, prs = acc[0], acc[1]
        for a in range(na):
            kn = 128 if a < 3 else 127
            m.op("pe", lambda e: e.matmul(po[:], lhsT=vc_tm[0:kn, a, :], rhs=Pc[4][a][0:kn, :], start=(a == 0), stop=(a == na - 1)),
                 [vc_tm, Pc[4][a]], [po])
        for a in range(na):
            kn = 128 if a < 3 else 127
            m.op("pe", lambda e: e.matmul(prs[:], lhsT=onesb[0:kn, :], rhs=Pc[4][a][0:kn, :], start=(a == 0), stop=(a == na - 1)),
                 [onesb, Pc[4][a]], [prs])
        finish_branch(0, po, prs, True)
        for si in range(4):
            pss = [rot.next(), rot.next()]
            for h in range(4):
                pb_ = pss[h // 2]
                c0 = (h % 2) * 160
                for a in range(na):
                    kn = 128 if a < 3 else 127
                    m.op("pe", lambda e: e.matmul(pb_[:, c0:c0 + 129], lhsT=Pc[h][a][0:kn, si * 128:(si + 1) * 128], rhs=ovl[0:kn, a, :],
                                                  start=(a == 0), stop=(a == na - 1)), [Pc[h][a], ovl], [pb_])
            for h in range(4):
                pb_ = pss[h // 2]
                c0 = (h % 2) * 160
                m.op("dve", lambda e: e.tensor_scalar(out=rsi[:, h:h + 1], in0=pb_[:, c0 + 128:c0 + 129], scalar1=1e-30, scalar2=None, op0=ALU.max),
                     [pb_], [rsi])
            m.op("dve", lambda e: e.reciprocal(out=rsi[:], in_=rsi[:]), [rsi], [rsi])
            sk = smk.next()
            st_ = qt * 4 + si
            m.dma("sp", sk[:], d["selmask"][st_ * 128:(st_ + 1) * 128, :], reads=[d["selmask"]], writes=[sk])
            for h in range(4):
                pb_ = pss[h // 2]
                c0 = (h % 2) * 160
                m.op("dve", lambda e: e.scalar_tensor_tensor(out=sc[:], in0=pb_[:, c0:c0 + 128], scalar=rsi[:, h:h + 1],
                                                             in1=(sk[:] if h == 0 else sc[:]), op0=ALU.mult, op1=ALU.add),
                     [pb_, rsi, sk, sc], [sc])
            m.op("dve", lambda e: e.max(out=top[:, 0:8], in_=sc[:]), [sc], [top])
            m.op("dve", lambda e: e.match_replace(out=sc2[:], in_to_replace=top[:, 0:8], in_values=sc[:], imm_value=-3e38), [sc, top], [sc2])
            m.op("dve", lambda e: e.max(out=top[:, 8:16], in_=sc2[:]), [sc2], [top])
            m.op("dve", lambda e: e.tensor_scalar(out=sc2[:], in0=sc[:], scalar1=top[:, 15:16], scalar2=None, op0=ALU.is_ge), [sc, top], [sc2])
            m.op("dve", lambda e: e.tensor_scalar(out=sc2[:], in0=sc2[:], scalar1=-1.0, scalar2=-NEG, op0=ALU.add, op1=ALU.mult), [sc2], [sc2])
            ptm = rot.next()
            m.op("pe", lambda e: e.transpose(out=ptm[:, 0:128], in_=sc2[:], identity=ident), [sc2, cst], [ptm])
            m.op("act", lambda e: e.activation(out=MT[:, si * 128:(si + 1) * 128], in_=ptm[:, 0:128], func=AF.Copy), [ptm], [MT])
        po, prs = acc[0], acc[1]
        nkt = 4 * qt + 4
        for kt in range(nkt):
            dl = q0 - 128 * kt
            ps = rot.next()
            need_add = dl <= 128
            m.op("pe", lambda e: e.matmul(ps[:], lhsT=kslcT[:, kt * 128:(kt + 1) * 128], rhs=qn[:, 4, :], start=True, stop=False), [kslcT, qn], [ps])
            m.op("pe", lambda e: e.matmul(ps[:], lhsT=stair[:, kt * 128:(kt + 1) * 128], rhs=MT[:], start=False, stop=not need_add), [stair, MT], [ps])
            if need_add:
                m.op("pe", lambda e: e.matmul(ps[:], lhsT=identb[:], rhs=MS[:, dl + 384:dl + 384 + 512], start=False, stop=True), [identb, MS], [ps])
            pp = Bp.next()
            m.op("act", lambda e: e.activation(out=pp[:], in_=ps[:], func=AF.Exp), [ps], [pp])
            m.op("pe", lambda e: e.matmul(po[:], lhsT=vslc_tm[:, kt, :], rhs=pp[:], start=(kt == 0), stop=(kt == nkt - 1)), [vslc_tm, pp], [po])
            m.op("pe", lambda e: e.matmul(prs[:], lhsT=onesb[:], rhs=pp[:], start=(kt == 0), stop=(kt == nkt - 1)), [onesb, pp], [prs])
        finish_branch(1, po, prs, False)
        po, prs = acc[0], acc[1]
        wt = []
        if prev_kw is not None:
            wt += [(prev_kw, prev_vw, c, q0 - (q0 - 512 + 128 * c)) for c in range(4)]
        wt += [(kw_cur, vw_cur, c, q0 - (q0 + 128 * c)) for c in range(4)]
        for wi, (kw_, vw_, c, dl) in enumerate(wt):
            ps = rot.next()
            m.op("pe", lambda e: e.matmul(ps[:], lhsT=kw_[:, c * 128:(c + 1) * 128], rhs=qn[:, 4, :], start=True, stop=False), [kw_, qn], [ps])
            m.op("pe", lambda e: e.matmul(ps[:], lhsT=identb[:], rhs=MW[:, dl + 384:dl + 384 + 512], start=False, stop=True), [identb, MW], [ps])
            pp = Bp.next()
            m.op("act", lambda e: e.activation(out=pp[:], in_=ps[:], func=AF.Exp), [ps], [pp])
            m.op("pe", lambda e: e.matmul(po[:], lhsT=vw_[:, c, :], rhs=pp[:], start=(wi == 0), stop=(wi == len(wt) - 1)), [vw_, pp], [po])
            m.op("pe", lambda e: e.matmul(prs[:], lhsT=onesb[:], rhs=pp[:], start=(wi == 0), stop=(wi == len(wt) - 1)), [onesb, pp], [prs])
        finish_branch(2, po, prs, False)
        prev_kw, prev_vw = kw_cur, vw_cur
        m.dma(mx.dq.next(), mx.y_out[256:384, q0:q0 + 512], osum[:, 0:512], reads=[osum], writes=[mx.y_out])


def src_t(x):
    return x.t if hasattr(x, "t") else x


_PROGS = {}


def _prog(key):
    if key not in _PROGS:
        if key == "A":
            _PROGS[key] = build_dense(False, True)[0]
        elif key == "CA":
            _PROGS[key] = build_dense(True, True)[0]
        elif key == "C":
            _PROGS[key] = build_dense(True, False)[0]
        elif key == "M":
            _PROGS[key] = build_mixer()[0]
    return _PROGS[key]


def _halo_cols(aT, tb):
    if tb == 0:
        return np.ascontiguousarray(np.concatenate([np.zeros((aT.shape[0], HALO), aT.dtype), aT[:, 0:NTOK]], axis=1))
    return np.ascontiguousarray(aT[:, tb - HALO:tb + NTOK])


def kernel(**inp):
    inp = {k: np.asarray(v) for k, v in inp.items()}
    x = inp["x"]
    cores = list(range(NCORE))
    xT = [np.ascontiguousarray(x[b].T) for b in range(NB)]
    cst = make_consts()
    ncst = make_nsa_consts()
    dps = [pack_dense_params(inp["attn_norm"][l], inp["ffn_norm"][l], inp["ffn_conv_w"][l], inp["ffn_conv_b"][l])
           for l in range(DEPTH)]
    maps = []
    for c in cores:
        b, tb = c // 4, (c % 4) * NTOK
        maps.append({"x_in": np.ascontiguousarray(xT[b][:, tb:tb + NTOK]), "dpa": dps[0], "w_in_a": inp["w_in"][0]})
    res = run_bass_kernel_spmd(_prog("A"), maps, core_ids=cores).results
    proj = [r["proj_out"] for r in res]
    gates = [r["gates_out"] for r in res]
    for l in range(DEPTH):
        projT = [np.concatenate([proj[b * 4 + i] for i in range(4)], axis=1) for b in range(NB)]
        maps = []
        for c in cores:
            b, j = c // 4, c % 4
            d = pack_mixer_inputs(l, b, j, projT[b], inp)
            d["cst"] = cst
            d.update(ncst)
            maps.append(d)
        res = run_bass_kernel_spmd(_prog("M"), maps, core_ids=cores).results
        yT = []
        for b in range(NB):
            y = np.empty((4 * 512, S), np.float32)
            for j in range(4):
                yo = res[b * 4 + j]["y_out"]
                for n in range(4):
                    y[n * 512 + j * 128:n * 512 + (j + 1) * 128] = yo[n * 128:(n + 1) * 128]
            yT.append(y)
        del res, maps
        last = (l == DEPTH - 1)
        maps = []
        for c in cores:
            b, tb = c // 4, (c % 4) * NTOK
            d = {"x_in": _halo_cols(xT[b], tb), "y_in": _halo_cols(yT[b], tb), "g_in": gates[c], "dpc": dps[l],
                 "w_in_c": inp["w_in"][l], "w_branch": inp["w_branch"][l].reshape(4 * 512, D), "w_out": inp["w_out"][l],
                 "ffn_up": inp["ffn_up"][l], "ffn_down": inp["ffn_down"][l]}
            if not last:
                d["dpa"] = dps[l + 1]
                d["w_in_a"] = inp["w_in"][l + 1]
            maps.append(d)
        res = run_bass_kernel_spmd(_prog("C" if last else "CA"), maps, core_ids=cores).results
        for c in cores:
            b, tb = c // 4, (c % 4) * NTOK
            xT[b][:, tb:tb + NTOK] = res[c]["x_out"]
        if not last:
            proj = [r["proj_out"] for r in res]
            gates = [r["gates_out"] for r in res]
        del res, maps
    out = np.stack([xT[b].T for b in range(NB)], axis=0)
    return np.ascontiguousarray(out.astype(np.float32))
```

```python
import numpy as np
import concourse.bass as bass
import concourse.mybir as mybir
from concourse.bass_utils import run_bass_kernel_spmd

F32 = mybir.dt.float32
BF16 = mybir.dt.bfloat16
AF = mybir.ActivationFunctionType
ALU = mybir.AluOpType
AX = mybir.AxisListType

D = 2048
S = 8192
NB = 2
DEPTH = 4
NCORE = 8
NTOK = 2048
HALO = 2
DFF = 5632
NMIX = 5852
NIN = 14044
KC = D // 128
import os as _os
SAME_ENGINE_SYNC = not bool(_os.environ.get("MK_NOSAME"))
RW_NOSAME = not bool(_os.environ.get("RW_SAME"))
RW_NOINC = bool(_os.environ.get("RW_NOINC"))


class T:
    __slots__ = ("h", "lw", "rd", "name")

    def __init__(self, h, name):
        self.h = h
        self.name = name
        self.lw = None
        self.rd = {}

    def __getitem__(self, idx):
        return self.h[idx]


class MK:
    NDMA = 24

    def __init__(self, nc):
        self.nc = nc
        self.same = SAME_ENGINE_SYNC
        self.nosame_now = False
        self.eng = {"pe": nc.tensor, "act": nc.scalar, "dve": nc.vector, "pool": nc.gpsimd, "sp": nc.sync}
        self.sem = {k: nc.alloc_semaphore("s_" + k) for k in self.eng}
        self.cnt = {k: 0 for k in self.eng}
        self.waited = {k: {} for k in self.eng}
        self.dsem, self.dval, self.dnext = {}, {}, {}
        for q in ("sp", "act", "pool"):
            self.dsem[q] = [nc.alloc_semaphore("d_%s_%d" % (q, i)) for i in range(self.NDMA)]
            self.dval[q] = [0] * self.NDMA
            self.dnext[q] = 0
        self.ntile = 0
        self.ninst = 0

    def sb(self, shape, dtype=F32, name=None):
        self.ntile += 1
        name = name or ("t%d" % self.ntile)
        return T(self.nc.alloc_sbuf_tensor(name, list(shape), dtype), name)

    def ps(self, shape, dtype=F32, name=None):
        self.ntile += 1
        name = name or ("p%d" % self.ntile)
        return T(self.nc.alloc_psum_tensor(name, list(shape), dtype), name)

    def dram(self, name, shape, dtype=F32, kind="Internal"):
        return T(self.nc.dram_tensor(name, list(shape), dtype, kind=kind), name)

    def _wait(self, e, key, val):
        if val <= 0:
            return
        w = self.waited[e]
        if w.get(key, 0) >= val:
            return
        w[key] = val
        if isinstance(key, str):
            self.eng[e].wait_ge(self.sem[key], val)
        else:
            q, i = key
            self.eng[e].wait_ge(self.dsem[q][i], val)

    def _deps(self, e, reads, writes):
        same = self.same and e != "pe" and not self.nosame_now
        for t in reads:
            if t.lw is not None and (t.lw[0] != e or same):
                self._wait(e, *t.lw)
        for t in writes:
            if t.lw is not None and (t.lw[0] != e or same):
                self._wait(e, *t.lw)
            for k, v in t.rd.items():
                if k != e or same:
                    self._wait(e, k, v)

    def op(self, e, fn, reads=(), writes=(), inc=True):
        self._deps(e, reads, writes)
        inst = fn(self.eng[e])
        if inc:
            self.cnt[e] += 1
            inst.then_inc(self.sem[e], 1)
            c = self.cnt[e]
        else:
            c = self.cnt[e] + 1
        for t in reads:
            t.rd[e] = c
        for t in writes:
            t.lw = (e, c)
            t.rd = {}
        self.ninst += 1
        return inst

    def dma(self, q, out, in_, reads=(), writes=(), **kw):
        i = self.dnext[q]
        self.dnext[q] = (i + 1) % self.NDMA
        key = (q, i)
        self._wait(q, key, self.dval[q][i])
        self._deps(q, reads, writes)
        inst = self.eng[q].dma_start(out=out, in_=in_, **kw)
        self.dval[q][i] += 16
        inst.then_inc(self.dsem[q][i], 16)
        v = self.dval[q][i]
        for t in reads:
            t.rd[key] = v
        for t in writes:
            t.lw = (key, v)
            t.rd = {}
        self.ninst += 1
        return inst

    def barrier(self):
        for e in ("pe", "act", "dve", "pool", "sp"):
            for o in ("pe", "act", "dve", "pool", "sp"):
                if o != e:
                    self._wait(e, o, self.cnt[o])
            for q in ("sp", "act", "pool"):
                for i in range(self.NDMA):
                    self._wait(e, (q, i), self.dval[q][i])

    def finish(self):
        for e in ("pe", "act", "dve", "pool"):
            self._wait("sp", e, self.cnt[e])
        for q in ("sp", "act", "pool"):
            for i in range(self.NDMA):
                self._wait("sp", (q, i), self.dval[q][i])


class Arena:
    def __init__(self, m, nbytes):
        self.m = m
        nc = m.nc
        top = 229344
        self.base = ((top - nc.sbuf_bytes_remaining) + 63) // 64 * 64
        self.slab = nc.alloc_sbuf_tensor("arena_slab", [128, nbytes + 64], mybir.dt.uint8)
        self.size = nbytes
        self.off = 0

    def reset(self):
        self.m.barrier()
        self.off = 0

    def sb(self, shape, dtype=F32, name=None):
        esz = 4 if dtype == F32 else 2
        n = 1
        for d_ in shape[1:]:
            n *= d_
        nb = (n * esz + 63) // 64 * 64
        assert self.off + nb <= self.size, "arena overflow %s %d+%d>%d" % (name, self.off, nb, self.size)
        h = self.m.nc.alloc_sbuf_tensor_at(name or "ar", list(shape), dtype, offset=self.base + self.off)
        self.off += nb
        return T(h, name)


class Rot:
    def __init__(self, items):
        self.items = items
        self.i = 0

    def next(self):
        t = self.items[self.i]
        self.i = (self.i + 1) % len(self.items)
        return t


DP_ATTN = 0
DP_FFN = 16
DP_CW = 32
DP_CB = DP_CW + 264
DP_N = DP_CB + 88


def pack_dense_params(attn_norm, ffn_norm, conv_w, conv_b):
    out = np.zeros((128, DP_N), np.float32)
    out[:, DP_ATTN:DP_ATTN + 16] = attn_norm.reshape(16, 128).T
    out[:, DP_FFN:DP_FFN + 16] = ffn_norm.reshape(16, 128).T
    out[:, DP_CW:DP_CW + 264] = conv_w.reshape(3, 88, 128).transpose(2, 0, 1).reshape(128, 264)
    out[:, DP_CB:DP_CB + 88] = conv_b.reshape(88, 128).T
    return out


class DenseCtx:
    def __init__(self, m):
        self.m = m
        self.banks = Rot([m.ps([128, 512], F32, "bank%d" % i) for i in range(6)])
        self.sbank = m.ps([128, 512], F32, "sbank")
        self.ones = m.sb([128, 128], F32, "ones")
        m.op("pool", lambda e: e.memset(self.ones[:], 1.0), [], [self.ones])
        self.buf1 = m.sb([128, KC, NTOK + HALO], BF16, "buf1")
        self.buf2 = m.sb([128, 44 * 512], BF16, "buf2")
        self.xs = Rot([m.sb([128, 512], F32, "xs%d" % i) for i in range(4)])
        self.sq = Rot([m.sb([128, 512], F32, "sq%d" % i) for i in range(2)])
        self.rstd = m.sb([128, 512], F32, "rstd")
        self.wb = Rot([m.sb([128, 5632], BF16, "wb%d" % i) for i in range(2)])
        self.ev = Rot([m.sb([128, 512], F32, "ev%d" % i) for i in range(3)])
        self.evb = Rot([m.sb([128, 512], BF16, "evb%d" % i) for i in range(4)])
        self.scr = Rot([m.sb([128, 516], F32, "scr%d" % i) for i in range(7)])
        self.dq = Rot(["sp", "act"])


def rmsnorm_T(cx, x_dram, gain, tiles, dst, dst_off=0):
    m = cx.m
    for (t0, n) in tiles:
        for kc in range(KC):
            xs = cx.xs.next()
            m.dma("sp", xs[:, 0:n], x_dram[kc * 128:(kc + 1) * 128, t0:t0 + n], reads=[x_dram], writes=[xs])
            sq = cx.sq.next()
            m.op("act", lambda e: e.activation(out=sq[:, 0:n], in_=xs[:, 0:n], func=AF.Square), [xs], [sq])
            m.op("pe", lambda e: e.matmul(cx.sbank[:, 0:n], lhsT=cx.ones[:], rhs=sq[:, 0:n],
                                          start=(kc == 0), stop=(kc == KC - 1)), [cx.ones, sq], [cx.sbank])
        m.op("act", lambda e: e.activation(out=cx.rstd[:, 0:n], in_=cx.sbank[:, 0:n], func=AF.Sqrt,
                                           scale=1.0 / D, bias=1e-6), [cx.sbank], [cx.rstd])
        m.op("dve", lambda e: e.reciprocal(out=cx.rstd[:, 0:n], in_=cx.rstd[:, 0:n]), [cx.rstd], [cx.rstd])
        for kc in range(KC):
            xs = cx.xs.next()
            m.dma("sp", xs[:, 0:n], x_dram[kc * 128:(kc + 1) * 128, t0:t0 + n], reads=[x_dram], writes=[xs])
            m.op("dve", lambda e: e.scalar_tensor_tensor(
                out=dst[:, kc, dst_off + t0:dst_off + t0 + n], in0=xs[:, 0:n], scalar=gain[:, kc:kc + 1],
                in1=cx.rstd[:, 0:n], op0=ALU.mult, op1=ALU.mult), [xs, gain_t(gain), cx.rstd], [dst_t(dst)])


def gain_t(g):
    return g.t if hasattr(g, "t") else g


def dst_t(d):
    return d.t if hasattr(d, "t") else d


class V:
    def __init__(self, t, ap):
        self.t = t
        self.ap = ap

    def __getitem__(self, idx):
        return self.ap[idx]


def linear_T(cx, w_dram, row0, K, col_groups, src, src_off, tiles, epilogue, wq="pool"):
    m = cx.m
    kcn = K // 128
    for gi, grp in enumerate(col_groups):
        wb = cx.wb.next()
        gw = sum(nc_ for (_, nc_) in grp)
        wv = wb[:, 0:kcn * gw].rearrange("p (k c) -> p k c", k=kcn)
        off = 0
        offs = []
        for (c0, ncols) in grp:
            for k0 in range(0, kcn, 16):
                k1 = min(kcn, k0 + 16)
                src_ap = w_dram[row0 + k0 * 128:row0 + k1 * 128, c0:c0 + ncols].rearrange("(kc p) c -> p kc c", p=128)
                m.dma(wq, wv[:, k0:k1, off:off + ncols], src_ap, reads=[w_dram], writes=[wb])
            offs.append(off)
            off += ncols
        for si, (c0, ncols) in enumerate(grp):
            for ti, (t0, n) in enumerate(tiles):
                ps = cx.banks.next()
                for kc in range(kcn):
                    m.op("pe", lambda e: e.matmul(ps[0:ncols, 0:n], lhsT=wv[:, kc, offs[si]:offs[si] + ncols],
                                                  rhs=src[:, kc, src_off + t0:src_off + t0 + n],
                                                  start=(kc == 0), stop=(kc == kcn - 1)), [wb, dst_t(src)], [ps])
                epilogue(ps, gi, si, c0, ncols, ti, t0, n)


def groups_of(c_start, c_end, gw=256):
    out = []
    c = c_start
    while c < c_end:
        g = []
        ge = min(c + gw, c_end)
        while c < ge:
            n = min(128, ge - c)
            g.append((c, n))
            c += n
        out.append(g)
    return out


def phase_A(cx, x_dram, x_tiles, dp, w_in, proj_out, gates_out):
    m = cx.m
    gain = V(dp, dp[:, DP_ATTN:DP_ATTN + 16])
    tb = x_tiles[0][0]
    rmsnorm_T(cx, x_dram, gain, x_tiles, cx.buf1, dst_off=-tb)
    tiles = [(t0 - tb, n) for (t0, n) in x_tiles]

    def ep_mix(ps, gi, si, c0, ncols, ti, t0, n):
        ev = cx.ev.next()
        if (ti + si) % 2 == 0:
            m.op("act", lambda e: e.activation(out=ev[0:ncols, 0:n], in_=ps[0:ncols, 0:n], func=AF.Copy), [ps], [ev])
        else:
            m.op("dve", lambda e: e.tensor_copy(out=ev[0:ncols, 0:n], in_=ps[0:ncols, 0:n]), [ps], [ev])
        m.dma(cx.dq.next(), proj_out[c0:c0 + ncols, t0:t0 + n], ev[0:ncols, 0:n], reads=[ev], writes=[proj_out])

    def ep_gate(ps, gi, si, c0, ncols, ti, t0, n):
        ev = cx.evb.next()
        m.op("act", lambda e: e.activation(out=ev[0:ncols, 0:n], in_=ps[0:ncols, 0:n], func=AF.Sigmoid), [ps], [ev])
        m.dma(cx.dq.next(), gates_out[c0 - NMIX:c0 - NMIX + ncols, t0:t0 + n], ev[0:ncols, 0:n], reads=[ev],
              writes=[gates_out])

    linear_T(cx, w_in, 0, D, groups_of(0, NMIX), cx.buf1, 0, tiles, ep_mix)
    linear_T(cx, w_in, 0, D, groups_of(NMIX, NIN), cx.buf1, 0, tiles, ep_gate)


def phase_C(cx, x_dram, y_dram, gates_dram, dp, w_in, w_branch, w_out, ffn_up, ffn_down, xmid, x_out):
    m = cx.m
    NT = NTOK + HALO
    tiles = [(0, HALO)] + [(HALO + i * 512, 512) for i in range(NTOK // 512)]
    gain = V(dp, dp[:, DP_ATTN:DP_ATTN + 16])
    hh = m.sb([128, KC, HALO], BF16, "hh")
    rmsnorm_T(cx, x_dram, gain, [(0, HALO)], hh)
    gh = m.sb([128, 64, HALO], F32, "gh")

    def ep_gh(ps, gi, si, c0, ncols, ti, t0, n):
        j = (c0 - NMIX) // 128
        m.op("act", lambda e: e.activation(out=gh[:, j, :], in_=ps[:, 0:HALO], func=AF.Sigmoid), [ps], [gh])

    linear_T(cx, w_in, 0, D, groups_of(NMIX, NIN), hh, 0, [(0, HALO)], ep_gh)
    CSTOP = int(_os.environ.get("C_STOP", "9"))
    if CSTOP <= 1:
        return
    for r in range(16):
        m.dma("pool", cx.buf1[:, r, :], y_dram[r * 128:(r + 1) * 128, :], reads=[y_dram], writes=[cx.buf1])
    if CSTOP <= 2:
        return
    mview = V(cx.buf2, cx.buf2[:, 0:KC * 512].rearrange("p (k t) -> p k t", k=KC))
    for ti, (t0, n) in enumerate(tiles):
        for dt_ in range(KC):
            wb = cx.wb.next()
            wv = wb[:, 0:16 * 128].rearrange("p (k c) -> p k c", k=16)
            m.dma("pool", wv, w_branch[:, dt_ * 128:(dt_ + 1) * 128].rearrange("(r p) c -> p r c", p=128),
                  reads=[w_branch], writes=[wb])
            tmps = []
            for nb in range(4):
                ps = cx.banks.next()
                for kc in range(4):
                    r = nb * 4 + kc
                    m.op("pe", lambda e: e.matmul(ps[:, 0:n], lhsT=wv[:, r, :], rhs=cx.buf1[:, r, t0:t0 + n],
                                                  start=(kc == 0), stop=(kc == 3)), [wb, cx.buf1], [ps])
                tp = cx.scr.next()
                if ti == 0:
                    m.op("dve", lambda e: e.tensor_tensor(out=tp[:, 0:n], in0=ps[:, 0:n], in1=gh[:, nb * 16 + dt_, :],
                                                          op=ALU.mult), [ps, gh], [tp])
                else:
                    g = cx.evb.next()
                    m.dma(cx.dq.next(), g[:, 0:n], gates_dram[nb * D + dt_ * 128:nb * D + (dt_ + 1) * 128,
                                                              t0 - HALO:t0 - HALO + n], reads=[gates_dram], writes=[g])
                    m.op("dve", lambda e: e.tensor_tensor(out=tp[:, 0:n], in0=ps[:, 0:n], in1=g[:, 0:n], op=ALU.mult),
                         [ps, g], [tp])
                tmps.append(tp)
            m.op("pool", lambda e: e.tensor_tensor(out=tmps[0][:, 0:n], in0=tmps[0][:, 0:n], in1=tmps[1][:, 0:n], op=ALU.add),
                 [tmps[0], tmps[1]], [tmps[0]])
            m.op("pool", lambda e: e.tensor_tensor(out=tmps[2][:, 0:n], in0=tmps[2][:, 0:n], in1=tmps[3][:, 0:n], op=ALU.add),
                 [tmps[2], tmps[3]], [tmps[2]])
            m.op("pool", lambda e: e.tensor_tensor(out=mview[:, dt_, 0:n], in0=tmps[0][:, 0:n],
                                                   in1=tmps[2][:, 0:n], op=ALU.add), [tmps[0], tmps[2]], [cx.buf2])

        def ep_out(ps, gi, si, c0, ncols, ti_, t0_, n_, t0=t0):
            xs = cx.xs.next()
            m.dma("sp", xs[:, 0:n_], x_dram[c0:c0 + 128, t0:t0 + n_], reads=[x_dram], writes=[xs])
            ev = cx.ev.next()
            m.op("dve", lambda e: e.tensor_tensor(out=ev[:, 0:n_], in0=ps[:, 0:n_], in1=xs[:, 0:n_], op=ALU.add), [ps, xs], [ev])
            m.dma(cx.dq.next(), xmid[c0:c0 + 128, t0:t0 + n_], ev[:, 0:n_], reads=[ev], writes=[xmid])

        linear_T(cx, w_out, 0, D, groups_of(0, D), mview, 0, [(0, n)], ep_out)
    if CSTOP <= 3:
        return
    gain2 = V(dp, dp[:, DP_FFN:DP_FFN + 16])
    rmsnorm_T(cx, xmid, gain2, tiles, cx.buf1)
    if CSTOP <= 4:
        return
    carry = m.sb([128, 88, 2], F32, "carry")
    m.op("dve", lambda e: e.memset(carry[:], 0.0), [], [carry])
    G = V(cx.buf2, cx.buf2[:, 0:44 * 512].rearrange("p (k t) -> p k t", k=44))
    U = cx.scr
    CV = cx.scr
    passes = [[tiles[0], tiles[1]], [tiles[2]], [tiles[3]], [tiles[4]]]
    for pi, ptiles in enumerate(passes):
        conv_out = {}

        def ep_up(ps, gi, si, c0, ncols, ti, t0, n):
            ct = c0 // 128
            u = U.next()
            m.op("act", lambda e: e.activation(out=u[:, 2:2 + n], in_=ps[:, 0:n], func=AF.Copy), [ps], [u])
            m.op("dve", lambda e: e.tensor_copy(out=u[:, 0:2], in_=carry[:, ct, :]), [carry], [u])
            m.op("dve", lambda e: e.tensor_copy(out=carry[:, ct, :], in_=u[:, n:n + 2]), [u], [carry])
            if n == HALO:
                return
            cv = CV.next()
            w = lambda k: dp[:, DP_CW + k * 88 + ct:DP_CW + k * 88 + ct + 1]
            m.op("dve", lambda e: e.tensor_scalar(out=cv[:, 0:n], in0=u[:, 2:2 + n], scalar1=w(2),
                                                  scalar2=dp[:, DP_CB + ct:DP_CB + ct + 1], op0=ALU.mult, op1=ALU.add),
                 [u, dp], [cv])
            m.op("dve", lambda e: e.scalar_tensor_tensor(out=cv[:, 0:n], in0=u[:, 1:1 + n], scalar=w(1), in1=cv[:, 0:n],
                                                         op0=ALU.mult, op1=ALU.add), [u, dp, cv], [cv])
            m.op("dve", lambda e: e.scalar_tensor_tensor(out=cv[:, 0:n], in0=u[:, 0:n], scalar=w(0), in1=cv[:, 0:n],
                                                         op0=ALU.mult, op1=ALU.add), [u, dp, cv], [cv])
            if si == 0:
                sg = CV.next()
                m.op("act", lambda e: e.activation(out=sg[:, 0:n], in_=cv[:, 0:n], func=AF.Silu), [cv], [sg])
                conv_out["g"] = sg
            else:
                sg = conv_out["g"]
                m.op("dve", lambda e: e.tensor_tensor(out=G[:, gi, 0:n], in0=sg[:, 0:n], in1=cv[:, 0:n], op=ALU.mult),
                     [sg, cv], [cx.buf2])

        grps = [[(j * 128, 128), (DFF + j * 128, 128)] for j in range(44)]
        linear_T(cx, ffn_up, 0, D, grps, cx.buf1, 0, ptiles, ep_up)
        (t0r, nr) = ptiles[-1]
        if CSTOP == 5:
            continue

        def ep_down(ps, gi, si, c0, ncols, ti, t0, n):
            xs = cx.xs.next()
            m.dma("sp", xs[:, 0:n], xmid[c0:c0 + 128, t0r:t0r + n], reads=[xmid], writes=[xs])
            ev = cx.ev.next()
            m.op("dve", lambda e: e.tensor_tensor(out=ev[:, 0:n], in0=ps[:, 0:n], in1=xs[:, 0:n], op=ALU.add), [ps, xs], [ev])
            m.dma(cx.dq.next(), x_out[c0:c0 + 128, t0r - HALO:t0r - HALO + n], ev[:, 0:n], reads=[ev], writes=[x_out])

        linear_T(cx, ffn_down, 0, DFF, [[(j * 128, 128)] for j in range(KC)], G, 0, [(0, nr)], ep_down)


def build_dense(do_C, do_A):
    nc = bass.Bass("TRN2", target_bir_lowering=False)
    m = MK(nc)
    cx = DenseCtx(m)
    NT = NTOK + HALO
    ei = lambda name, shape, dt=F32: m.dram(name, shape, dt, kind="ExternalInput")
    eo = lambda name, shape, dt=F32: m.dram(name, shape, dt, kind="ExternalOutput")
    outs = []
    if do_C:
        x_in = ei("x_in", [D, NT])
        y_in = ei("y_in", [4 * 512, NT])
        g_in = ei("g_in", [4 * D, NTOK], BF16)
        dpc = ei("dpc", [128, DP_N])
        w_in_c = ei("w_in_c", [D, NIN])
        w_branch = ei("w_branch", [4 * 512, D])
        w_out = ei("w_out", [D, D])
        ffn_up = ei("ffn_up", [D, 2 * DFF])
        ffn_down = ei("ffn_down", [DFF, D])
        xmid = m.dram("xmid", [D, NT])
        x_out = eo("x_out", [D, NTOK])
        dpc_s = m.sb([128, DP_N], F32, "dpc_s")
        m.dma("sp", dpc_s[:], dpc[:], reads=[dpc], writes=[dpc_s])
        phase_C(cx, x_in, y_in, g_in, dpc_s, w_in_c, w_branch, w_out, ffn_up, ffn_down, xmid, x_out)
        xa, xa_tiles = x_out, [(i * 512, 512) for i in range(NTOK // 512)]
    else:
        xa = ei("x_in", [D, NTOK])
        xa_tiles = [(i * 512, 512) for i in range(NTOK // 512)]
    if do_A:
        dpa = ei("dpa", [128, DP_N])
        w_in_a = ei("w_in_a", [D, NIN])
        proj_out = eo("proj_out", [NMIX, NTOK])
        gates_out = eo("gates_out", [4 * D, NTOK], BF16)
        dpa_s = m.sb([128, DP_N], F32, "dpa_s")
        m.dma("sp", dpa_s[:], dpa[:], reads=[dpa], writes=[dpa_s])
        phase_A(cx, xa, xa_tiles, dpa_s, w_in_a, proj_out, gates_out)
    m.finish()
    return nc, m


R_GQ, R_GK, R_GV, R_GG, R_GLR = 0, 64, 128, 256, 384
R_LX, R_LG = 512, 640
R_NQ, R_NKV, R_NG = 768, 1280, 2048
R_RR, R_RK, R_RV, R_XW, R_XA, R_XG = 2176, 2304, 2432, 2560, 2688, 2816
R_NQO = 3072
MIX_ROWS = 3200
MP_GB, MP_GGAIN = 0, 1
MP_LCW, MP_LCB, MP_LBA, MP_LBI, MP_LLAM = 2, 6, 7, 8, 9
MP_NQG, MP_NKG = 10, 11
MP_MUR, MP_MUK, MP_MUV, MP_MUW, MP_MUA, MP_MUG = 12, 13, 14, 15, 16, 17
MP_W0, MP_A0, MP_KK, MP_KA, MP_RK, MP_LNW, MP_LNB = 19, 20, 21, 22, 23, 24, 25
MP_POS = 26
MP_N = 26 + 32
C_ID, C_BONES, C_CAUS, C_DELTA, C_CMASK = 0, 128, 256, 320, 384
C_REV = 896
CN = 896 + 128


def make_consts():
    c = np.zeros((128, CN), np.float32)
    c[:, C_ID:C_ID + 128] = np.eye(128)
    p = np.arange(128)
    c[:, C_BONES:C_BONES + 128] = (p[:, None] // 64 == p[None, :] // 64)
    s = np.arange(64)
    c[0:64, C_CAUS:C_CAUS + 64] = (s[:, None] <= s[None, :])
    c[:, C_DELTA:C_DELTA + 64] = ((p[:, None] % 64) == s[None, :])
    cm = np.ones((128, 512), np.float32)
    cm[:, 0::64] = 0.0
    c[:, C_CMASK:C_CMASK + 512] = cm
    c[:, C_REV:C_REV + 128] = np.eye(128)[::-1]
    return c


class MixCtx:
    def __init__(self, m, mix_in, y_out, mp, cst):
        self.m = m
        self.mix_in = mix_in
        self.y_out = y_out
        self.banks = Rot([m.ps([128, 512], F32, "mbank%d" % i) for i in range(8)])
        self.mp = m.sb([128, MP_N], F32, "mp_s")
        m.dma("sp", self.mp[:], mp[:], reads=[mp], writes=[self.mp])
        self.cst = m.sb([128, CN], F32, "cst_s")
        m.dma("sp", self.cst[:], cst[:], reads=[cst], writes=[self.cst])
        self.dq = Rot(["sp", "act"])
        self.arena = Arena(m, m.nc.sbuf_bytes_remaining - 1024)

    def col(self, c, rows=128):
        return self.mp[0:rows, c:c + 1]


def bcast_mid(base, reps):
    a = base.ap
    return bass.AP(tensor=base.tensor, offset=base.offset, ap=[[a[0][0], a[0][1]], [a[1][0], a[1][1]], [0, reps]])


def bcast_outer(t_ap, reps):
    a = t_ap.ap
    return bass.AP(tensor=t_ap.tensor, offset=t_ap.offset, ap=[[a[0][0], a[0][1]], [0, reps], [a[1][0], a[1][1]]])


def mixer_lru(mx, wa_d, wi_d, ntiles=S // 512):
    m, mp = mx.m, mx.mp
    ar = mx.arena
    ar.reset()
    c8 = ar.sb([128, 2], F32, "lru_c8")
    m.op("act", lambda e: e.activation(out=c8[:, 0:1], in_=mx.col(MP_LLAM), func=AF.Exp, scale=-1.0), [mp], [c8])
    m.op("act", lambda e: e.activation(out=c8[:, 0:1], in_=c8[:, 0:1], func=AF.Ln, bias=1.0), [c8], [c8])
    m.op("dve", lambda e: e.tensor_scalar(out=c8[:, 1:2], in0=c8[:, 0:1], scalar1=-16.0, scalar2=None, op0=ALU.mult), [c8], [c8])
    m.op("dve", lambda e: e.tensor_scalar(out=c8[:, 0:1], in0=c8[:, 0:1], scalar1=-8.0, scalar2=None, op0=ALU.mult), [c8], [c8])
    wa = ar.sb([128, 128], BF16, "lru_wa_s")
    wi = ar.sb([128, 128], BF16, "lru_wi_s")
    m.dma("pool", wa[:], wa_d[:], reads=[wa_d], writes=[wa])
    m.dma("pool", wi[:], wi_d[:], reads=[wi_d], writes=[wi])
    xt = Rot([ar.sb([128, 515], F32, "lru_xt%d" % i) for i in range(2)])
    F = Rot([ar.sb([128, 512], F32, "lru_f%d" % i) for i in range(10)])
    hb = Rot([ar.sb([128, 512], F32, "lru_h%d" % i) for i in range(2)])
    xcb = ar.sb([128, 512], BF16, "lru_xcb")
    prev_x, prev_h = None, None
    for ti in range(ntiles):
        t0 = ti * 512
        x = xt.next()
        m.dma("sp", x[:, 3:515], mx.mix_in[R_LX:R_LX + 128, t0:t0 + 512], reads=[mx.mix_in], writes=[x])
        if prev_x is None:
            m.op("dve", lambda e: e.memset(x[:, 0:3], 0.0), [], [x])
        else:
            m.op("dve", lambda e: e.tensor_copy(out=x[:, 0:3], in_=prev_x[:, 512:515]), [prev_x], [x])
        prev_x = x
        xc = F.next()
        m.op("dve", lambda e: e.tensor_scalar(out=xc[:], in0=x[:, 3:515], scalar1=mx.col(MP_LCW + 3), scalar2=mx.col(MP_LCB),
                                              op0=ALU.mult, op1=ALU.add), [x, mp], [xc])
        for k in range(3):
            m.op("dve", lambda e: e.scalar_tensor_tensor(out=xc[:], in0=x[:, k:k + 512], scalar=mx.col(MP_LCW + k), in1=xc[:],
                                                         op0=ALU.mult, op1=ALU.add), [x, mp, xc], [xc])
        m.op("act", lambda e: e.activation(out=xcb[:], in_=xc[:], func=AF.Copy), [xc], [xcb])
        pa, pi = mx.banks.next(), mx.banks.next()
        m.op("pe", lambda e: e.matmul(pa[:], lhsT=wa[:], rhs=xcb[:], start=True, stop=True), [wa, xcb], [pa])
        m.op("pe", lambda e: e.matmul(pi[:], lhsT=wi[:], rhs=xcb[:], start=True, stop=True), [wi, xcb], [pi])
        r, ig, a, a2 = F.next(), F.next(), F.next(), F.next()
        m.op("act", lambda e: e.activation(out=r[:], in_=pa[:], func=AF.Sigmoid, bias=mx.col(MP_LBA)), [pa, mp], [r])
        m.op("act", lambda e: e.activation(out=ig[:], in_=pi[:], func=AF.Sigmoid, bias=mx.col(MP_LBI)), [pi, mp], [ig])
        m.op("act", lambda e: e.activation(out=a[:], in_=r[:], func=AF.Exp, scale=c8[:, 0:1]), [r, c8], [a])
        m.op("act", lambda e: e.activation(out=a2[:], in_=r[:], func=AF.Exp, scale=c8[:, 1:2]), [r, c8], [a2])
        m.op("dve", lambda e: e.tensor_scalar(out=a2[:], in0=a2[:], scalar1=-1.0, scalar2=1.0, op0=ALU.mult, op1=ALU.add), [a2], [a2])
        m.op("act", lambda e: e.activation(out=a2[:], in_=a2[:], func=AF.Sqrt), [a2], [a2])
        m.op("dve", lambda e: e.tensor_tensor(out=ig[:], in0=ig[:], in1=xc[:], op=ALU.mult), [ig, xc], [ig])
        m.op("dve", lambda e: e.tensor_tensor(out=ig[:], in0=ig[:], in1=a2[:], op=ALU.mult), [ig, a2], [ig])
        h = hb.next()
        init = 0.0 if prev_h is None else prev_h[:, 511:512]
        m.op("dve", lambda e: e.tensor_tensor_scan(out=h[:], data0=a[:], data1=ig[:], initial=init, op0=ALU.mult, op1=ALU.add),
             [a, ig] + ([prev_h] if prev_h is not None else []), [h])
        prev_h = h
        g = F.next()
        m.dma("sp", g[:], mx.mix_in[R_LG:R_LG + 128, t0:t0 + 512], reads=[mx.mix_in], writes=[g])
        gl = F.next()
        m.op("act", lambda e: e.activation(out=gl[:], in_=g[:], func=AF.Gelu_apprx_tanh), [g], [gl])
        m.op("dve", lambda e: e.tensor_tensor(out=gl[:], in0=gl[:], in1=h[:], op=ALU.mult), [gl, h], [gl])
        m.dma(mx.dq.next(), mx.y_out[128:256, t0:t0 + 512], gl[:], reads=[gl], writes=[mx.y_out])


def mixer_gla(mx, wgk_d, ntiles=S // 512):
    m, mp, cst = mx.m, mx.mp, mx.cst
    ar = mx.arena
    ar.reset()
    wgk = ar.sb([16, 64], F32, "gla_wgk_s")
    m.dma("sp", wgk[:], wgk_d[:], reads=[wgk_d], writes=[wgk])
    nb = ar.sb([64, 1], F32, "gla_nb")
    m.op("dve", lambda e: e.tensor_scalar(out=nb[:], in0=mx.col(MP_GB, 64), scalar1=-1.0, scalar2=None, op0=ALU.mult), [mp], [nb])
    St = ar.sb([64, 128], F32, "gla_S")
    Sb = ar.sb([64, 128], BF16, "gla_Sb")
    m.op("dve", lambda e: e.memset(St[:], 0.0), [], [St])
    m.op("dve", lambda e: e.memset(Sb[:], 0.0), [], [Sb])
    ones = ar.sb([128, 128], F32, "gla_ones")
    m.op("dve", lambda e: e.memset(ones[:], 1.0), [], [ones])
    F64 = Rot([ar.sb([64, 512], F32, "gla_f%d" % i) for i in range(12)])
    F128 = Rot([ar.sb([128, 512], F32, "gla_F%d" % i) for i in range(8)])
    B16 = Rot([ar.sb([64, 512], BF16, "gla_b%d" % i) for i in range(4)])
    lrp = Rot([ar.sb([16, 512], F32, "gla_lr%d" % i) for i in range(2)])
    khT = Rot([ar.sb([64, 64], BF16, "gla_khT%d" % i) for i in range(3)])
    vT = Rot([ar.sb([64, 128], BF16, "gla_vT%d" % i) for i in range(3)])
    Am = Rot([ar.sb([64, 64], BF16, "gla_A%d" % i) for i in range(3)])
    for ti in range(ntiles):
        t0 = ti * 512
        q, k, lr = F64.next(), F64.next(), lrp.next()
        v, g = F128.next(), F128.next()
        m.dma("sp", q[:], mx.mix_in[R_GQ:R_GQ + 64, t0:t0 + 512], reads=[mx.mix_in], writes=[q])
        m.dma("sp", k[:], mx.mix_in[R_GK:R_GK + 64, t0:t0 + 512], reads=[mx.mix_in], writes=[k])
        m.dma("sp", v[:], mx.mix_in[R_GV:R_GV + 128, t0:t0 + 512], reads=[mx.mix_in], writes=[v])
        m.dma("sp", g[:], mx.mix_in[R_GG:R_GG + 128, t0:t0 + 512], reads=[mx.mix_in], writes=[g])
        m.dma("sp", lr[:], mx.mix_in[R_GLR:R_GLR + 16, t0:t0 + 512], reads=[mx.mix_in], writes=[lr])
        pz = mx.banks.next()
        m.op("pe", lambda e: e.matmul(pz[0:64, :], lhsT=wgk[:], rhs=lr[:], start=True, stop=True), [wgk, lr], [pz])
        la, Bc, E, Ei, kh = F64.next(), F64.next(), F64.next(), F64.next(), F64.next()
        m.op("act", lambda e: e.activation(out=la[:], in_=pz[0:64, :], func=AF.Exp, scale=-1.0, bias=nb[:]), [pz, nb], [la])
        m.op("act", lambda e: e.activation(out=la[:], in_=la[:], func=AF.Ln, bias=1.0), [la], [la])
        m.op("dve", lambda e: e.tensor_scalar(out=la[:], in0=la[:], scalar1=-1.0 / 16.0, scalar2=None, op0=ALU.mult), [la], [la])
        m.op("dve", lambda e: e.tensor_tensor_scan(out=Bc[:], data0=cst[0:64, C_CMASK:C_CMASK + 512], data1=la[:], initial=0.0,
                                                   op0=ALU.mult, op1=ALU.add), [cst, la], [Bc])
        m.op("act", lambda e: e.activation(out=E[:], in_=Bc[:], func=AF.Exp), [Bc], [E])
        m.op("act", lambda e: e.activation(out=Ei[:], in_=Bc[:], func=AF.Exp, scale=-1.0), [Bc], [Ei])
        qt, kt = B16.next(), B16.next()
        m.op("dve", lambda e: e.scalar_tensor_tensor(out=qt[:], in0=q[:], scalar=0.125, in1=E[:], op0=ALU.mult, op1=ALU.mult), [q, E], [qt])
        m.op("dve", lambda e: e.tensor_tensor(out=kt[:], in0=k[:], in1=Ei[:], op=ALU.mult), [k, Ei], [kt])
        for c in range(8):
            cs = slice(c * 64, (c + 1) * 64)
            m.op("dve", lambda e: e.scalar_tensor_tensor(out=kh[:, cs], in0=k[:, cs], scalar=E[:, c * 64 + 63:c * 64 + 64], in1=Ei[:, cs],
                                                         op0=ALU.mult, op1=ALU.mult), [k, E, Ei], [kh])
        o = F128.next()
        for c in range(8):
            cs = slice(c * 64, (c + 1) * 64)
            pT = mx.banks.next()
            m.op("pe", lambda e: e.transpose(out=pT[0:64, 0:64], in_=kh[:, cs], identity=cst[0:64, C_ID:C_ID + 64]), [kh, cst], [pT])
            m.op("pe", lambda e: e.transpose(out=pT[0:64, 64:192], in_=v[:, cs], identity=cst[:, C_ID:C_ID + 128]), [v, cst], [pT])
            kT_, vT_ = khT.next(), vT.next()
            m.op("act", lambda e: e.activation(out=kT_[:], in_=pT[0:64, 0:64], func=AF.Copy), [pT], [kT_])
            m.op("act", lambda e: e.activation(out=vT_[:], in_=pT[0:64, 64:192], func=AF.Copy), [pT], [vT_])
            pS = mx.banks.next()
            m.op("pe", lambda e: e.matmul(pS[0:64, 0:64], lhsT=kt[:, cs], rhs=qt[:, cs], start=True, stop=True), [kt, qt], [pS])
            A = Am.next()
            m.op("dve", lambda e: e.tensor_tensor(out=A[:], in0=pS[0:64, 0:64], in1=cst[0:64, C_CAUS:C_CAUS + 64], op=ALU.mult), [pS, cst], [A])
            pO = mx.banks.next()
            m.op("pe", lambda e: e.matmul(pO[:, 0:64], lhsT=vT_[:], rhs=A[:], start=True, stop=False), [vT_, A], [pO])
            m.op("pe", lambda e: e.matmul(pO[:, 0:64], lhsT=Sb[:], rhs=qt[:, cs], start=False, stop=True), [Sb, qt], [pO])
            m.op("act", lambda e: e.activation(out=o[:, cs], in_=pO[:, 0:64], func=AF.Copy), [pO], [o])
            pD = mx.banks.next()
            m.op("pe", lambda e: e.matmul(pD[0:64, 0:128], lhsT=kT_[:], rhs=vT_[:], start=True, stop=True), [kT_, vT_], [pD])
            m.op("dve", lambda e: e.scalar_tensor_tensor(out=St[:], in0=St[:], scalar=E[:, c * 64 + 63:c * 64 + 64], in1=pD[0:64, 0:128],
                                                         op0=ALU.mult, op1=ALU.add), [St, E, pD], [St])
            m.op("act", lambda e: e.activation(out=Sb[:], in_=St[:], func=AF.Copy), [St], [Sb])
        sq = F128.next()
        m.op("act", lambda e: e.activation(out=sq[:], in_=o[:], func=AF.Square), [o], [sq])
        pn = mx.banks.next()
        m.op("pe", lambda e: e.matmul(pn[:], lhsT=ones[:], rhs=sq[:], start=True, stop=True), [ones, sq], [pn])
        m.op("act", lambda e: e.activation(out=sq[:], in_=pn[:], func=AF.Sqrt, scale=1.0 / 128, bias=1e-6), [pn], [sq])
        m.op("dve", lambda e: e.reciprocal(out=sq[:], in_=sq[:]), [sq], [sq])
        m.op("dve", lambda e: e.scalar_tensor_tensor(out=o[:], in0=o[:], scalar=mx.col(MP_GGAIN), in1=sq[:], op0=ALU.mult, op1=ALU.mult),
             [o, mp, sq], [o])
        m.op("act", lambda e: e.activation(out=g[:], in_=g[:], func=AF.Silu), [g], [g])
        m.op("dve", lambda e: e.tensor_tensor(out=o[:], in0=o[:], in1=g[:], op=ALU.mult), [o, g], [o])
        m.dma(mx.dq.next(), mx.y_out[0:128, t0:t0 + 512], o[:], reads=[o], writes=[mx.y_out])


def mixer_rwkv(mx, wl_d, al_d, gl_d, ntiles=S // 512):
    m, mp, cst = mx.m, mx.mp, mx.cst
    ar = mx.arena
    ar.reset()
    wl = ar.sb([96, 128], F32, "rw_wl_s")
    al = ar.sb([96, 128], F32, "rw_al_s")
    gl = ar.sb([128, 2, 128], F32, "rw_gl_s")
    m.dma("sp", wl[:], wl_d[:], reads=[wl_d], writes=[wl])
    m.dma("sp", al[:], al_d[:], reads=[al_d], writes=[al])
    m.dma("sp", gl[:], gl_d[:, :].rearrange("(k p) c -> p k c", p=128), reads=[gl_d], writes=[gl])
    bones = cst[:, C_BONES:C_BONES + 128]
    St = ar.sb([128, 64], F32, "rw_S")
    m.op("dve", lambda e: e.memset(St[:], 0.0), [], [St])
    S2 = bass.AP(tensor=St.h, offset=0, ap=[[64, 128], [0, 2], [1, 64]])
    P2 = ar.sb([128, 2, 64], F32, "rw_P2")
    ft = Rot([ar.sb([128, 513], F32, "rw_ft%d" % i) for i in range(8)])
    prev = {}
    F = Rot([ar.sb([128, 512], F32, "rw_f%d" % i) for i in range(14)])
    XQ = Rot([ar.sb([128, 5, 512], F32, "rw_xq%d" % i) for i in range(2)])
    VM = Rot([ar.sb([128, 512], F32, "rw_vm%d" % i) for i in range(2)])
    SAZ = Rot([ar.sb([128, 512, 2], F32, "rw_saz%d" % i) for i in range(2)])
    Dt = Rot([ar.sb([128, 512], F32, "rw_D%d" % i) for i in range(int(_os.environ.get("RW_ND", "4")))])
    BC = Rot([ar.sb([128, 5, 512], F32, "rw_bc%d" % i) for i in range(int(_os.environ.get("RW_NBC", "2")))])
    delta = cst[:, C_DELTA:C_DELTA + 64]

    def shifted(name, row0, rows, mucol, t0):
        f = ft.next()
        m.dma("sp", f[0:rows, 1:513], mx.mix_in[row0:row0 + rows, t0:t0 + 512], reads=[mx.mix_in], writes=[f])
        if name not in prev:
            m.op("dve", lambda e: e.memset(f[0:rows, 0:1], 0.0), [], [f])
        else:
            p = prev[name]
            m.op("dve", lambda e: e.tensor_copy(out=f[0:rows, 0:1], in_=p[0:rows, 512:513]), [p], [f])
        prev[name] = f
        d = F.next()
        m.op("dve", lambda e: e.tensor_tensor(out=d[0:rows, :], in0=f[0:rows, 0:512], in1=f[0:rows, 1:513], op=ALU.subtract), [f], [d])
        m.op("dve", lambda e: e.scalar_tensor_tensor(out=d[0:rows, :], in0=d[0:rows, :], scalar=mp[0:rows, mucol:mucol + 1],
                                                     in1=f[0:rows, 1:513], op0=ALU.mult, op1=ALU.add), [d, mp, f], [d])
        return d

    def bsum(src, scale=None):
        ps = mx.banks.next()
        m.op("pe", lambda e: e.matmul(ps[:], lhsT=bones, rhs=src[:], start=True, stop=True), [cst, src], [ps])
        return ps

    for ti in range(ntiles):
        t0 = ti * 512
        r = shifted("r", R_RR, 128, MP_MUR, t0)
        k = shifted("k", R_RK, 128, MP_MUK, t0)
        v = shifted("v", R_RV, 128, MP_MUV, t0)
        vm = VM.next()
        m.op("act", lambda e: e.activation(out=vm[:], in_=v[:], func=AF.Copy), [v], [vm])
        xw = shifted("xw", R_XW, 96, MP_MUW, t0)
        xa = shifted("xa", R_XA, 96, MP_MUA, t0)
        xg0 = shifted("xg0", R_XG, 128, MP_MUG, t0)
        xg1 = shifted("xg1", R_XG + 128, 128, MP_MUG + 1, t0)
        xq = XQ.next()
        kk_, wr_, w_, nkka_, kp_ = (xq[:, i, :] for i in range(5))
        m.op("act", lambda e: e.activation(out=xw[0:96, :], in_=xw[0:96, :], func=AF.Tanh), [xw], [xw])
        pw = mx.banks.next()
        m.op("pe", lambda e: e.matmul(pw[:], lhsT=wl[:], rhs=xw[0:96, :], start=True, stop=True), [wl, xw], [pw])
        m.op("act", lambda e: e.activation(out=w_, in_=pw[:], func=AF.Sigmoid, bias=mx.col(MP_W0)), [pw, mp], [xq])
        m.op("act", lambda e: e.activation(out=w_, in_=w_, func=AF.Exp, scale=-0.6065306597126334), [xq], [xq])
        pa = mx.banks.next()
        m.op("pe", lambda e: e.matmul(pa[:], lhsT=al[:], rhs=xa[0:96, :], start=True, stop=True), [al, xa], [pa])
        a = F.next()
        m.op("act", lambda e: e.activation(out=a[:], in_=pa[:], func=AF.Sigmoid, bias=mx.col(MP_A0)), [pa, mp], [a])
        m.op("act", lambda e: e.activation(out=xg0[:], in_=xg0[:], func=AF.Sigmoid), [xg0], [xg0])
        m.op("act", lambda e: e.activation(out=xg1[:], in_=xg1[:], func=AF.Sigmoid), [xg1], [xg1])
        pg = mx.banks.next()
        m.op("pe", lambda e: e.matmul(pg[:], lhsT=gl[:, 0, :], rhs=xg0[:], start=True, stop=False), [gl, xg0], [pg])
        m.op("pe", lambda e: e.matmul(pg[:], lhsT=gl[:, 1, :], rhs=xg1[:], start=False, stop=True), [gl, xg1], [pg])
        gg = F.next()
        m.op("act", lambda e: e.activation(out=gg[:], in_=pg[:], func=AF.Copy), [pg], [gg])
        m.op("dve", lambda e: e.tensor_scalar(out=kk_, in0=k[:], scalar1=mx.col(MP_KK), scalar2=None, op0=ALU.mult), [k, mp], [xq])
        sq = F.next()
        m.op("act", lambda e: e.activation(out=sq[:], in_=kk_, func=AF.Square), [xq], [sq])
        pn = bsum(sq)
        m.op("act", lambda e: e.activation(out=sq[:], in_=pn[:], func=AF.Sqrt), [pn], [sq])
        m.op("dve", lambda e: e.tensor_scalar(out=sq[:], in0=sq[:], scalar1=1e-12, scalar2=None, op0=ALU.max), [sq], [sq])
        m.op("dve", lambda e: e.reciprocal(out=sq[:], in_=sq[:]), [sq], [sq])
        m.op("dve", lambda e: e.tensor_tensor(out=kk_, in0=kk_, in1=sq[:], op=ALU.mult), [xq, sq], [xq])
        m.op("dve", lambda e: e.tensor_scalar(out=kp_, in0=a[:], scalar1=-1.0, scalar2=mx.col(MP_KA), op0=ALU.add, op1=ALU.mult), [a, mp], [xq])
        m.op("dve", lambda e: e.scalar_tensor_tensor(out=kp_, in0=kp_, scalar=1.0, in1=k[:], op0=ALU.add, op1=ALU.mult), [xq, k], [xq])
        m.op("dve", lambda e: e.scalar_tensor_tensor(out=nkka_, in0=kk_, scalar=-1.0, in1=a[:], op0=ALU.mult, op1=ALU.mult), [xq, a], [xq])
        m.op("dve", lambda e: e.tensor_tensor(out=wr_, in0=w_, in1=r[:], op=ALU.mult), [xq, r], [xq])
        p1, p2, p3 = F.next(), F.next(), F.next()
        m.op("dve", lambda e: e.tensor_tensor(out=p1[:], in0=nkka_, in1=r[:], op=ALU.mult), [xq, r], [p1])
        m.op("dve", lambda e: e.tensor_tensor(out=p2[:], in0=kp_, in1=r[:], op=ALU.mult), [xq, r], [p2])
        m.op("dve", lambda e: e.tensor_scalar(out=p3[:], in0=p2[:], scalar1=mx.col(MP_RK), scalar2=None, op0=ALU.mult), [p2, mp], [p3])
        c1, c2, c3 = bsum(p1), bsum(p2), bsum(p3)
        m.op("act", lambda e: e.activation(out=p1[:], in_=c1[:], func=AF.Copy), [c1], [p1])
        m.op("act", lambda e: e.activation(out=p2[:], in_=c2[:], func=AF.Copy), [c2], [p2])
        m.op("act", lambda e: e.activation(out=p3[:], in_=c3[:], func=AF.Copy), [c3], [p3])
        saz = SAZ.next()
        for gi in range(64):
            bc = BC.next()
            for qi in range(5):
                dt_ = Dt.next()
                m.op("pool", lambda e: e.tensor_tensor(out=dt_[:, :].rearrange("p (t j) -> p t j", j=64),
                                                       in0=bcast_mid(xq[:, qi, gi * 8:gi * 8 + 8], 64),
                                                       in1=bcast_outer(delta, 8), op=ALU.mult), [xq, cst], [dt_])
                pb = mx.banks.next()
                m.op("pe", lambda e: e.matmul(pb[:], lhsT=bones, rhs=dt_[:], start=True, stop=True), [cst, dt_], [pb])
                m.op("act", lambda e: e.activation(out=bc[:, qi, :], in_=pb[:], func=AF.Copy), [pb], [bc])
            m.nosame_now = RW_NOSAME
            for tt in range(8):
                t = gi * 8 + tt
                js = slice(tt * 64, (tt + 1) * 64)
                li = (not RW_NOINC) or (tt == 7)
                m.op("dve", lambda e: e.tensor_tensor(out=P2[:], in0=S2, in1=bc[:, 0:2, js], op=ALU.mult), [St, bc], [P2], inc=not RW_NOINC)
                m.op("dve", lambda e: e.tensor_reduce(out=saz[:, t, :], in_=P2[:], axis=AX.X, op=ALU.add), [P2], [saz], inc=not RW_NOINC)
                m.op("dve", lambda e: e.tensor_tensor(out=St[:], in0=St[:], in1=bc[:, 2, js], op=ALU.mult), [St, bc], [St], inc=not RW_NOINC)
                m.op("dve", lambda e: e.scalar_tensor_tensor(out=St[:], in0=bc[:, 3, js], scalar=saz[:, t, 0:1], in1=St[:],
                                                             op0=ALU.mult, op1=ALU.add), [bc, saz, St], [St], inc=not RW_NOINC)
                m.op("dve", lambda e: e.scalar_tensor_tensor(out=St[:], in0=bc[:, 4, js], scalar=vm[:, t:t + 1], in1=St[:],
                                                             op0=ALU.mult, op1=ALU.add), [bc, vm, St], [St], inc=li)
            m.nosame_now = False
        y = F.next()
        m.op("pool", lambda e: e.tensor_tensor(out=y[:], in0=saz[:, :, 0], in1=p1[:], op=ALU.mult), [saz, p1], [y])
        m.op("pool", lambda e: e.tensor_tensor(out=y[:], in0=y[:], in1=saz[:, :, 1], op=ALU.add), [y, saz], [y])
        m.op("pool", lambda e: e.tensor_tensor(out=p2[:], in0=p2[:], in1=vm[:], op=ALU.mult), [p2, vm], [p2])
        m.op("pool", lambda e: e.tensor_tensor(out=y[:], in0=y[:], in1=p2[:], op=ALU.add), [y, p2], [y])
        pm_ = bsum(y)
        m.op("pool", lambda e: e.tensor_copy(out=p1[:], in_=y[:]), [y], [p1])
        m.op("dve", lambda e: e.scalar_tensor_tensor(out=y[:], in0=pm_[:], scalar=-1.0 / 64, in1=p1[:], op0=ALU.mult, op1=ALU.add),
             [pm_, p1], [y])
        m.op("act", lambda e: e.activation(out=p1[:], in_=y[:], func=AF.Square), [y], [p1])
        pv = bsum(p1)
        m.op("act", lambda e: e.activation(out=p1[:], in_=pv[:], func=AF.Sqrt, scale=1.0 / 64, bias=64e-5), [pv], [p1])
        m.op("dve", lambda e: e.reciprocal(out=p1[:], in_=p1[:]), [p1], [p1])
        m.op("dve", lambda e: e.tensor_tensor(out=y[:], in0=y[:], in1=p1[:], op=ALU.mult), [y, p1], [y])
        m.op("dve", lambda e: e.tensor_scalar(out=y[:], in0=y[:], scalar1=mx.col(MP_LNW), scalar2=mx.col(MP_LNB), op0=ALU.mult, op1=ALU.add),
             [y, mp], [y])
        m.op("dve", lambda e: e.tensor_tensor(out=p3[:], in0=p3[:], in1=vm[:], op=ALU.mult), [p3, vm], [p3])
        m.op("dve", lambda e: e.tensor_tensor(out=y[:], in0=y[:], in1=p3[:], op=ALU.add), [y, p3], [y])
        m.op("dve", lambda e: e.tensor_tensor(out=y[:], in0=y[:], in1=gg[:], op=ALU.mult), [y, gg], [y])
        m.dma(mx.dq.next(), mx.y_out[384:512, t0:t0 + 512], y[:], reads=[y], writes=[mx.y_out])


def build_mixer(which=("gla", "lru", "nsa", "rwkv"), ntiles=S // 512):
    nc = bass.Bass("TRN2", target_bir_lowering=False)
    m = MK(nc)
    ei = lambda name, shape, dt=F32: m.dram(name, shape, dt, kind="ExternalInput")
    mix_in = ei("mix_in", [MIX_ROWS, S])
    mp = ei("mp", [128, MP_N])
    cst = ei("cst", [128, CN])
    y_out = m.dram("y_out", [512, S], F32, kind="ExternalOutput")
    mx = MixCtx(m, mix_in, y_out, mp, cst)
    if "lru" in which:
        mixer_lru(mx, ei("lru_wa", [128, 128]), ei("lru_wi", [128, 128]), ntiles)
    if "gla" in which:
        mixer_gla(mx, ei("gla_wgk", [16, 64]), ntiles)
    if "nsa" in which:
        d = {"k1": ei("nsa_k1", [4096, 128]), "k2": ei("nsa_k2", [128, 128]), "v1": ei("nsa_v1", [4096, 128]),
             "v2": ei("nsa_v2", [128, 128]), "rb": ei("nsa_rb", [32, 5]), "oh": ei("nsa_oh", [33, LV]),
             "ovl": ei("nsa_ovl", [128, 4 * 129]), "selmask": ei("nsa_selmask", [64 * 128, 128]), "gsel": ei("nsa_gsel", [3, 384])}
        mixer_nsa(mx, d, ntiles)
    if "rwkv" in which:
        mixer_rwkv(mx, ei("rw_wl", [96, 128]), ei("rw_al", [96, 128]), ei("rw_gl", [256, 128]), ntiles)
    m.finish()
    return nc, m


def pack_mixer_inputs(l, b, j, projT, inp):
    mi = np.zeros((MIX_ROWS, S), np.float32)
    mi[R_GQ:R_GQ + 64] = projT[64 * j:64 * j + 64]
    mi[R_GK:R_GK + 64] = projT[256 + 64 * j:256 + 64 * j + 64]
    mi[R_GV:R_GV + 128] = projT[512 + 128 * j:512 + 128 * j + 128]
    mi[R_GG:R_GG + 128] = projT[1024 + 128 * j:1024 + 128 * j + 128]
    mi[R_GLR:R_GLR + 16] = projT[1536:1552]
    mi[R_LX:R_LX + 128] = projT[1552 + 128 * j:1552 + 128 * j + 128]
    mi[R_LG:R_LG + 128] = projT[2064 + 128 * j:2064 + 128 * j + 128]
    mi[R_NQ:R_NQ + 512] = projT[2576:3088]
    mi[R_NKV:R_NKV + 768] = projT[3088:3856]
    mi[R_NQO:R_NQO + 128] = projT[2576 + 128 * j:2576 + 128 * j + 128]
    for gi_ in range(3):
        mi[R_NG + gi_] = projT[3856 + gi_ * 4 + j]
    f0 = 3868
    mi[R_RR:R_RR + 128] = projT[f0 + 128 * j:f0 + 128 * j + 128]
    mi[R_RK:R_RK + 128] = projT[f0 + 512 + 128 * j:f0 + 512 + 128 * j + 128]
    mi[R_RV:R_RV + 128] = projT[f0 + 1024 + 128 * j:f0 + 1024 + 128 * j + 128]
    mi[R_XW:R_XW + 96] = projT[f0 + 1536:f0 + 1632]
    mi[R_XA:R_XA + 96] = projT[f0 + 1632:f0 + 1728]
    mi[R_XG:R_XG + 256] = projT[f0 + 1728:f0 + 1984]
    mp = np.zeros((128, MP_N), np.float32)
    sl = slice(128 * j, 128 * j + 128)
    mp[0:64, MP_GB] = inp["gla_b_gk"][l][64 * j:64 * j + 64]
    mp[:, MP_GGAIN] = inp["gla_out_norm"][l]
    for k in range(4):
        mp[:, MP_LCW + k] = inp["lru_conv_w"][l][k, sl]
    mp[:, MP_LCB] = inp["lru_conv_b"][l][sl]
    mp[:, MP_LBA] = inp["lru_b_a"][l][sl]
    mp[:, MP_LBI] = inp["lru_b_i"][l][sl]
    mp[:, MP_LLAM] = inp["lru_lambda"][l][sl]
    mp[:, MP_NQG] = inp["nsa_q_norm"][l]
    mp[:, MP_NKG] = inp["nsa_k_norm"][l]
    mu = inp["rwkv_mu"][l]
    mp[:, MP_MUR] = mu[128 * j:128 * j + 128]
    mp[:, MP_MUK] = mu[512 + 128 * j:512 + 128 * j + 128]
    mp[:, MP_MUV] = mu[1024 + 128 * j:1024 + 128 * j + 128]
    mp[0:96, MP_MUW] = mu[1536:1632]
    mp[0:96, MP_MUA] = mu[1632:1728]
    mp[:, MP_MUG] = mu[1728:1856]
    mp[:, MP_MUG + 1] = mu[1856:1984]
    mp[:, MP_W0] = inp["rwkv_w0"][l][sl]
    mp[:, MP_A0] = inp["rwkv_a0"][l][sl]
    mp[:, MP_KK] = inp["rwkv_k_k"][l][sl]
    mp[:, MP_KA] = inp["rwkv_k_a"][l][sl]
    mp[:, MP_RK] = inp["rwkv_r_k"][l].reshape(512)[sl]
    mp[:, MP_LNW] = inp["rwkv_ln_w"][l][sl]
    mp[:, MP_LNB] = inp["rwkv_ln_b"][l][sl]
    mp[:, MP_POS:MP_POS + 32] = inp["nsa_cmp_pos"][l].T
    rb = inp["rel_bias"]
    d = {"mix_in": mi, "mp": mp, "nsa_k1": inp["nsa_cmp_k1"][l], "nsa_k2": inp["nsa_cmp_k2"][l], "nsa_v1": inp["nsa_cmp_v1"][l],
         "nsa_v2": inp["nsa_cmp_v2"][l], "nsa_rb": np.ascontiguousarray(np.concatenate([rb, rb[:, j:j + 1]], axis=1)),
         "gla_wgk": np.ascontiguousarray(inp["gla_w_gk"][l][:, 64 * j:64 * j + 64]),
         "lru_wa": np.ascontiguousarray(inp["lru_w_a"][l][j]), "lru_wi": np.ascontiguousarray(inp["lru_w_i"][l][j]),
         "rw_wl": np.ascontiguousarray(inp["rwkv_w_lora"][l][:, sl]), "rw_al": np.ascontiguousarray(inp["rwkv_a_lora"][l][:, sl]),
         "rw_gl": np.ascontiguousarray(inp["rwkv_g_lora"][l][:, sl])}
    return d


NEG = -30000.0
LV_SEL = 4592
LV_WIN = 1536
LV = LV_SEL + LV_WIN
NCMP = 511


def _t5_bucket(n):
    n = np.asarray(n)
    nf = np.maximum(n, 1).astype(np.float32)
    large = 16 + (np.log(nf / np.float32(16)) / np.float32(np.log(128 / 16)) * np.float32(16)).astype(np.int32)
    large = np.minimum(large, 31)
    return np.where(n < 16, n, large)


def make_nsa_consts():
    oh = np.zeros((33, LV), np.float32)
    i = np.arange(LV_SEL)
    dist = i - 2063
    ok = dist >= 0
    b = _t5_bucket(np.maximum(dist, 0))
    oh[b[ok], i[ok]] += 1.0
    oh[31, i[ok]] -= 1.0
    oh[32, i[~ok]] = NEG
    i2 = np.arange(LV_WIN)
    dist = i2 - 511
    ok = (dist >= 0) & (dist < 512)
    b = _t5_bucket(np.maximum(dist, 0))
    oh[b[ok], LV_SEL + i2[ok]] += 1.0
    oh[31, LV_SEL + i2[ok]] -= 1.0
    oh[32, LV_SEL + i2[~ok]] = NEG
    n = np.arange(512)
    j = np.arange(128)
    ov = ((16 * n[:, None] < 64 * j[None, :] + 64) & (16 * n[:, None] + 32 > 64 * j[None, :])).astype(np.float32)
    ov[511] = 0.0
    ovl = np.zeros((128, 4, 129), np.float32)
    ovl[:, :, 0:128] = ov.reshape(4, 128, 128).transpose(1, 0, 2)
    ovl[:, :, 128] = 1.0
    ovl[127, 3, :] = 0.0
    sm = np.zeros((64, 128, 128), np.float32)
    for st in range(64):
        pos = st * 128 + np.arange(128)
        cur = pos // 64
        blk = np.arange(128)[None, :]
        forced = (blk == 0) | (blk == cur[:, None]) | (blk == cur[:, None] - 1)
        fut = blk > cur[:, None]
        sm[st] = np.where(forced, 1e9, np.where(fut, -1e9, 0.0))
    gsel = np.zeros((3, 3, 128), np.float32)
    for g in range(3):
        gsel[g, g, :] = 1.0
    return {"nsa_oh": oh, "nsa_ovl": ovl.reshape(128, 4 * 129), "nsa_selmask": sm.reshape(64 * 128, 128),
            "nsa_gsel": gsel.reshape(3, 384)}


def mixer_nsa(mx, d, nqt=S // 512):
    m, mp, cst = mx.m, mx.mp, mx.cst
    ar = mx.arena
    ar.reset()
    F = Rot([ar.sb([128, 516], F32, "nsaF%d" % i) for i in range(12)])
    Bp = Rot([ar.sb([128, 512], BF16, "nsaB%d" % i) for i in range(6)])
    acc = [mx.banks.items[0], mx.banks.items[1], mx.banks.items[2]]
    rot = Rot(mx.banks.items[3:8])
    ident = cst[:, C_ID:C_ID + 128]
    onesf = ar.sb([128, 128], F32, "nsa_onesf")
    onesb = ar.sb([128, 128], BF16, "nsa_onesb")
    identb = ar.sb([128, 128], BF16, "nsa_identb")
    m.op("dve", lambda e: e.memset(onesf[:], 1.0), [], [onesf])
    m.op("dve", lambda e: e.memset(onesb[:], 1.0), [], [onesb])
    m.op("dve", lambda e: e.tensor_copy(out=identb[:], in_=ident), [cst], [identb])
    gq = ar.sb([128, 1], F32, "nsa_gq")
    m.op("dve", lambda e: e.tensor_scalar(out=gq[:], in0=mx.col(MP_NQG), scalar1=128 ** -0.5, scalar2=None, op0=ALU.mult), [mp], [gq])

    def rms_rows(src, gaincol, dst, n=512):
        sq = F.next()
        m.op("act", lambda e: e.activation(out=sq[:, 0:n], in_=src.ap, func=AF.Square), [src_t(src)], [sq])
        pn = rot.next()
        m.op("pe", lambda e: e.matmul(pn[:, 0:n], lhsT=onesf[:], rhs=sq[:, 0:n], start=True, stop=True), [onesf, sq], [pn])
        m.op("act", lambda e: e.activation(out=sq[:, 0:n], in_=pn[:, 0:n], func=AF.Sqrt, scale=1.0 / 128, bias=1e-6), [pn], [sq])
        m.op("dve", lambda e: e.reciprocal(out=sq[:, 0:n], in_=sq[:, 0:n]), [sq], [sq])
        m.op("dve", lambda e: e.scalar_tensor_tensor(out=dst.ap, in0=src.ap, scalar=gaincol, in1=sq[:, 0:n], op0=ALU.mult, op1=ALU.mult),
             [src_t(src), mp, gq, sq], [src_t(dst)])

    kcT = ar.sb([128, 512], BF16, "nsa_kcT")
    vc_tm = ar.sb([128, 4, 128], BF16, "nsa_vctm")
    m.op("dve", lambda e: e.memset(kcT[:], 0.0), [], [kcT])
    m.op("dve", lambda e: e.memset(vc_tm[:], 0.0), [], [vc_tm])
    w1 = [ar.sb([128, 32, 128], BF16, "nsa_w1%d" % i) for i in range(2)]
    w2 = [ar.sb([128, 128], BF16, "nsa_w2%d" % i) for i in range(2)]
    posb = ar.sb([128, 32], BF16, "nsa_posb")
    m.op("dve", lambda e: e.tensor_copy(out=posb[:], in_=mp[:, MP_POS:MP_POS + 32]), [mp], [posb])
    cb = ar.sb([128, 2], F32, "nsa_cb")
    for i, (k1n, k2n) in enumerate((("k1", "k2"), ("v1", "v2"))):
        for l0 in range(0, 32, 16):
            m.dma("pool", w1[i][:, l0:l0 + 16, :], d[k1n][l0 * 128:(l0 + 16) * 128, :].rearrange("(l p) h -> p l h", p=128),
                  reads=[d[k1n]], writes=[w1[i]])
        m.dma("pool", w2[i][:], d[k2n][:], reads=[d[k2n]], writes=[w2[i]])
        pb = rot.next()
        for l in range(32):
            m.op("pe", lambda e: e.matmul(pb[:, 0:1], lhsT=w1[i][:, l, :], rhs=posb[:, l:l + 1], start=(l == 0), stop=(l == 31)),
                 [w1[i], posb], [pb])
        m.op("act", lambda e: e.activation(out=cb[:, i:i + 1], in_=pb[:, 0:1], func=AF.Copy), [pb], [cb])
    chunk = Rot([ar.sb([128, 2080], BF16, "nsa_chunk%d" % i) for i in range(2)])
    for a in range(4):
        nb_ = 128 if a < 3 else 127
        tok0 = a * 2048
        ntok = 16 * (nb_ - 1) + 32
        for i in range(2):
            ch = chunk.next()
            row0 = R_NKV + (0 if i == 0 else 128)
            m.dma("pool", ch[:, 0:ntok], mx.mix_in[row0:row0 + 128, tok0:tok0 + ntok], reads=[mx.mix_in], writes=[ch])
            ph = rot.next()
            for l in range(32):
                rhs = bass.AP(tensor=ch.h, offset=l, ap=[[2080, 128], [16, nb_]])
                m.op("pe", lambda e: e.matmul(ph[:, 0:nb_], lhsT=w1[i][:, l, :], rhs=rhs, start=(l == 0), stop=(l == 31)), [w1[i], ch], [ph])
            hb_ = Bp.next()
            m.op("act", lambda e: e.activation(out=hb_[:, 0:nb_], in_=ph[:, 0:nb_], func=AF.Gelu_apprx_tanh, bias=cb[:, i:i + 1]), [ph, cb], [hb_])
            po = rot.next()
            m.op("pe", lambda e: e.matmul(po[:, 0:nb_], lhsT=w2[i][:], rhs=hb_[:, 0:nb_], start=True, stop=True), [w2[i], hb_], [po])
            of = F.next()
            m.op("act", lambda e: e.activation(out=of[:, 0:nb_], in_=po[:, 0:nb_], func=AF.Copy), [po], [of])
            if i == 0:
                rms_rows(V(of, of[:, 0:nb_]), mx.col(MP_NKG), V(kcT, kcT[:, a * 128:a * 128 + nb_]), nb_)
            else:
                pt = rot.next()
                m.op("pe", lambda e: e.transpose(out=pt[0:nb_, 0:128], in_=of[:, 0:nb_], identity=ident), [of, cst], [pt])
                m.op("act", lambda e: e.activation(out=vc_tm[0:nb_, a, :], in_=pt[0:nb_, 0:128], func=AF.Copy), [pt], [vc_tm])
    rbx = ar.sb([33, 5], F32, "nsa_rbx")
    m.op("dve", lambda e: e.memset(rbx[32:33, :], 1.0), [], [rbx])
    m.dma("sp", rbx[0:32, :], d["rb"][:], reads=[d["rb"]], writes=[rbx])
    fv_d = m.dram("nsa_fv", [5, LV], F32)
    ohs = Rot([ar.sb([33, 512], F32, "nsa_oh%d" % i) for i in range(2)])
    for c0 in range(0, LV, 512):
        n = min(512, LV - c0)
        o_ = ohs.next()
        m.dma("sp", o_[:, 0:n], d["oh"][:, c0:c0 + n], reads=[d["oh"]], writes=[o_])
        pf = rot.next()
        m.op("pe", lambda e: e.matmul(pf[0:5, 0:n], lhsT=rbx[:], rhs=o_[:, 0:n], start=True, stop=True), [rbx, o_], [pf])
        fs = F.next()
        m.op("act", lambda e: e.activation(out=fs[0:5, 0:n], in_=pf[0:5, 0:n], func=AF.Copy), [pf], [fs])
        m.dma("sp", fv_d[:, c0:c0 + n], fs[0:5, 0:n], reads=[fs], writes=[fv_d])
    MC = ar.sb([128, 5, 2560], BF16, "nsa_MC")
    MS = ar.sb([128, 1024], BF16, "nsa_MS")
    MW = ar.sb([128, 1408], BF16, "nsa_MW")
    jrev = cst[:, C_REV:C_REV + 128]
    tz = Rot([ar.sb([128, 512], F32, "nsa_tz%d" % i) for i in range(2)])

    def toeplitz(dst_fn, head_off, off, pstep, W):
        for c0 in range(0, W, 512):
            n = min(512, W - c0)
            t_ = tz.next()
            src = bass.AP(tensor=fv_d.h, offset=head_off + off + c0, ap=[[pstep, 128], [1, n]])
            m.dma("sp", t_[:, 0:n], src, reads=[fv_d], writes=[t_])
            pz = rot.next()
            m.op("pe", lambda e: e.matmul(pz[:, 0:n], lhsT=jrev, rhs=t_[:, 0:n], start=True, stop=True), [cst, t_], [pz])
            dst, dt_ = dst_fn(c0, n)
            m.op("act", lambda e: e.activation(out=dst, in_=pz[:, 0:n], func=AF.Copy), [pz], [dt_])

    for h in range(5):
        toeplitz(lambda c0, n, h=h: (MC[:, h, c0:c0 + n], MC), h * LV, 0, 16, 2560)
    toeplitz(lambda c0, n: (MS[:, c0:c0 + n], MS), 4 * LV, 1552, 1, 1024)
    toeplitz(lambda c0, n: (MW[:, c0:c0 + n], MW), 4 * LV, LV_SEL, 1, 1408)
    ovl = ar.sb([128, 4, 129], BF16, "nsa_ovl_s")
    m.dma("pool", ovl[:], d["ovl"][:, :].rearrange("p (a c) -> p a c", a=4), reads=[d["ovl"]], writes=[ovl])
    gsel = ar.sb([3, 3, 128], F32, "nsa_gsel_s")
    m.dma("sp", gsel[:], d["gsel"][:, :].rearrange("p (a c) -> p a c", a=3), reads=[d["gsel"]], writes=[gsel])
    stair = ar.sb([128, S], BF16, "nsa_stair")
    m.op("dve", lambda e: e.tensor_copy(out=stair[:, :].rearrange("p (j r) -> p j r", r=64), in_=bcast_mid(identb[:, 0:128], 64)),
         [identb], [stair])
    kslcT = ar.sb([128, S], BF16, "nsa_kslcT")
    vslc_tm = ar.sb([128, S // 128, 128], BF16, "nsa_vslc")
    kwin = Rot([ar.sb([128, 512], BF16, "nsa_kwin%d" % i) for i in range(2)])
    vwin = Rot([ar.sb([128, 4, 128], BF16, "nsa_vwin%d" % i) for i in range(2)])
    qn = ar.sb([128, 5, 512], BF16, "nsa_qn")
    Pc = [[ar.sb([128, 512], BF16, "nsa_pc%d_%d" % (h, a)) for a in range(4)] for h in range(5)]
    MT = ar.sb([128, 512], BF16, "nsa_MT")
    sc = ar.sb([128, 128], F32, "nsa_sc")
    sc2 = ar.sb([128, 128], F32, "nsa_sc2")
    top = ar.sb([128, 16], F32, "nsa_top")
    rsi = ar.sb([128, 4], F32, "nsa_rsi")
    smk = Rot([ar.sb([128, 128], F32, "nsa_smk%d" % i) for i in range(2)])
    gate3 = ar.sb([3, 512], F32, "nsa_gate3")
    prev_kw, prev_vw = None, None
    osum_b = ar.sb([128, 512], F32, "nsa_osum")
    for qt in range(nqt):
        q0 = qt * 512
        for (row, which_) in ((256, "ks"), (512, "kw")):
            kf = F.next()
            m.dma("sp", kf[:, 0:512], mx.mix_in[R_NKV + row:R_NKV + row + 128, q0:q0 + 512], reads=[mx.mix_in], writes=[kf])
            if which_ == "ks":
                rms_rows(V(kf, kf[:, 0:512]), mx.col(MP_NKG), V(kslcT, kslcT[:, q0:q0 + 512]))
            else:
                kw_cur = kwin.next()
                rms_rows(V(kf, kf[:, 0:512]), mx.col(MP_NKG), V(kw_cur, kw_cur[:, :]))
        vw_cur = vwin.next()
        for (row, which_) in ((384, "vs"), (640, "vw")):
            vf = F.next()
            m.dma("sp", vf[:, 0:512], mx.mix_in[R_NKV + row:R_NKV + row + 128, q0:q0 + 512], reads=[mx.mix_in], writes=[vf])
            pt = rot.next()
            for c in range(4):
                m.op("pe", lambda e: e.transpose(out=pt[:, c * 128:(c + 1) * 128], in_=vf[:, c * 128:(c + 1) * 128], identity=ident),
                     [vf, cst], [pt])
            if which_ == "vs":
                m.op("act", lambda e: e.activation(out=vslc_tm[:, qt * 4:qt * 4 + 4, :], in_=pt[:, :].rearrange("p (c v) -> p c v", c=4),
                                                   func=AF.Copy), [pt], [vslc_tm])
            else:
                m.op("act", lambda e: e.activation(out=vw_cur[:], in_=pt[:, :].rearrange("p (c v) -> p c v", c=4), func=AF.Copy),
                     [pt], [vw_cur])
        for h in range(5):
            qf = F.next()
            r0 = R_NQ + h * 128 if h < 4 else R_NQO
            m.dma("sp", qf[:, 0:512], mx.mix_in[r0:r0 + 128, q0:q0 + 512], reads=[mx.mix_in], writes=[qf])
            rms_rows(V(qf, qf[:, 0:512]), gq[:], V(qn, qn[:, h, :]))
        m.dma("sp", gate3[:], mx.mix_in[R_NG:R_NG + 3, q0:q0 + 512], reads=[mx.mix_in], writes=[gate3])
        m.op("act", lambda e: e.activation(out=gate3[:], in_=gate3[:], func=AF.Sigmoid), [gate3], [gate3])
        osum = osum_b

        def finish_branch(g, po, prs, first):
            pg = rot.next()
            m.op("pe", lambda e: e.matmul(pg[:], lhsT=gsel[:, g, :], rhs=gate3[:], start=True, stop=True), [gsel, gate3], [pg])
            wv_ = F.next()
            m.op("dve", lambda e: e.tensor_scalar(out=wv_[:, 0:512], in0=prs[:], scalar1=1e-30, scalar2=None, op0=ALU.max), [prs], [wv_])
            m.op("dve", lambda e: e.reciprocal(out=wv_[:, 0:512], in_=wv_[:, 0:512]), [wv_], [wv_])
            m.op("dve", lambda e: e.tensor_tensor(out=wv_[:, 0:512], in0=wv_[:, 0:512], in1=pg[:], op=ALU.mult), [wv_, pg], [wv_])
            if first:
                m.op("dve", lambda e: e.tensor_tensor(out=osum[:, 0:512], in0=wv_[:, 0:512], in1=po[:], op=ALU.mult), [wv_, po], [osum])
            else:
                m.op("dve", lambda e: e.tensor_tensor(out=wv_[:, 0:512], in0=wv_[:, 0:512], in1=po[:], op=ALU.mult), [wv_, po], [wv_])
                m.op("dve", lambda e: e.tensor_tensor(out=osum[:, 0:512], in0=osum[:, 0:512], in1=wv_[:, 0:512], op=ALU.add), [osum, wv_], [osum])

        na = min(4, qt // 4 + 1)
        for h in range(5):
            for a in range(na):
                kn = 128 if a < 3 else 127
                dc = q0 - 2048 * a
                ps = rot.next()
                need_add = dc <= 2048
                m.op("pe", lambda e: e.matmul(ps[0:kn, :], lhsT=kcT[:, a * 128:a * 128 + kn], rhs=qn[:, h, :], start=True, stop=not need_add),
                     [kcT, qn], [ps])
                if need_add:
                    m.op("pe", lambda e: e.matmul(ps[0:kn, :], lhsT=identb[0:kn, 0:kn], rhs=MC[0:kn, h, dc:dc + 512], start=False, stop=True),
                         [identb, MC], [ps])
                m.op("act", lambda e: e.activation(out=Pc[h][a][0:kn, :], in_=ps[0:kn, :], func=AF.Exp), [ps], [Pc[h][a]])
        po, prs = acc[0], acc[1]
        for a in range(na):
            kn = 128 if a < 3 else 127
            m.op("pe", lambda e: e.matmul(po[:], lhsT=vc_tm[0:kn, a, :], rhs=Pc[4][a][0:kn, :], start=(a == 0), stop=(a == na - 1)),
                 [vc_tm, Pc[4][a]], [po])
        for a in range(na):
            kn = 128 if a < 3 else 127
            m.op("pe", lambda e: e.matmul(prs[:], lhsT=onesb[0:kn, :], rhs=Pc[4][a][0:kn, :], start=(a == 0), stop=(a == na - 1)),
                 [onesb, Pc[4][a]], [prs])
        finish_branch(0, po, prs, True)
        for si in range(4):
            pss = [rot.next(), rot.next()]
            for h in range(4):
                pb_ = pss[h // 2]
                c0 = (h % 2) * 160
                for a in range(na):
                    kn = 128 if a < 3 else 127
                    m.op("pe", lambda e: e.matmul(pb_[:, c0:c0 + 129], lhsT=Pc[h][a][0:kn, si * 128:(si + 1) * 128], rhs=ovl[0:kn, a, :],
                                                  start=(a == 0), stop=(a == na - 1)), [Pc[h][a], ovl], [pb_])
            for h in range(4):
                pb_ = pss[h // 2]
                c0 = (h % 2) * 160
                m.op("dve", lambda e: e.tensor_scalar(out=rsi[:, h:h + 1], in0=pb_[:, c0 + 128:c0 + 129], scalar1=1e-30, scalar2=None, op0=ALU.max),
                     [pb_], [rsi])
            m.op("dve", lambda e: e.reciprocal(out=rsi[:], in_=rsi[:]), [rsi], [rsi])
            sk = smk.next()
            st_ = qt * 4 + si
            m.dma("sp", sk[:], d["selmask"][st_ * 128:(st_ + 1) * 128, :], reads=[d["selmask"]], writes=[sk])
            for h in range(4):
                pb_ = pss[h // 2]
                c0 = (h % 2) * 160
                m.op("dve", lambda e: e.scalar_tensor_tensor(out=sc[:], in0=pb_[:, c0:c0 + 128], scalar=rsi[:, h:h + 1],
                                                             in1=(sk[:] if h == 0 else sc[:]), op0=ALU.mult, op1=ALU.add),
                     [pb_, rsi, sk, sc], [sc])
            m.op("dve", lambda e: e.max(out=top[:, 0:8], in_=sc[:]), [sc], [top])
            m.op("dve", lambda e: e.match_replace(out=sc2[:], in_to_replace=top[:, 0:8], in_values=sc[:], imm_value=-3e38), [sc, top], [sc2])
            m.op("dve", lambda e: e.max(out=top[:, 8:16], in_=sc2[:]), [sc2], [top])
            m.op("dve", lambda e: e.tensor_scalar(out=sc2[:], in0=sc[:], scalar1=top[:, 15:16], scalar2=None, op0=ALU.is_ge), [sc, top], [sc2])
            m.op("dve", lambda e: e.tensor_scalar(out=sc2[:], in0=sc2[:], scalar1=-1.0, scalar2=-NEG, op0=ALU.add, op1=ALU.mult), [sc2], [sc2])
            ptm = rot.next()
            m.op("pe", lambda e: e.transpose(out=ptm[:, 0:128], in_=sc2[:], identity=ident), [sc2, cst], [ptm])
            m.op("act", lambda e: e.activation(out=MT[:, si * 128:(si + 1) * 128], in_=ptm[:, 0:128], func=AF.Copy), [ptm], [MT])
        po, prs = acc[0], acc[1]
        nkt = 4 * qt + 4
        for kt in range(nkt):
            dl = q0 - 128 * kt
            ps = rot.next()
            need_add = dl <= 128
            m.op("pe", lambda e: e.matmul(ps[:], lhsT=kslcT[:, kt * 128:(kt + 1) * 128], rhs=qn[:, 4, :], start=True, stop=False), [kslcT, qn], [ps])
            m.op("pe", lambda e: e.matmul(ps[:], lhsT=stair[:, kt * 128:(kt + 1) * 128], rhs=MT[:], start=False, stop=not need_add), [stair, MT], [ps])
            if need_add:
                m.op("pe", lambda e: e.matmul(ps[:], lhsT=identb[:], rhs=MS[:, dl + 384:dl + 384 + 512], start=False, stop=True), [identb, MS], [ps])
            pp = Bp.next()
            m.op("act", lambda e: e.activation(out=pp[:], in_=ps[:], func=AF.Exp), [ps], [pp])
            m.op("pe", lambda e: e.matmul(po[:], lhsT=vslc_tm[:, kt, :], rhs=pp[:], start=(kt == 0), stop=(kt == nkt - 1)), [vslc_tm, pp], [po])
            m.op("pe", lambda e: e.matmul(prs[:], lhsT=onesb[:], rhs=pp[:], start=(kt == 0), stop=(kt == nkt - 1)), [onesb, pp], [prs])
        finish_branch(1, po, prs, False)
        po, prs = acc[0], acc[1]
        wt = []
        if prev_kw is not None:
            wt += [(prev_kw, prev_vw, c, q0 - (q0 - 512 + 128 * c)) for c in range(4)]
        wt += [(kw_cur, vw_cur, c, q0 - (q0 + 128 * c)) for c in range(4)]
        for wi, (kw_, vw_, c, dl) in enumerate(wt):
            ps = rot.next()
            m.op("pe", lambda e: e.matmul(ps[:], lhsT=kw_[:, c * 128:(c + 1) * 128], rhs=qn[:, 4, :], start=True, stop=False), [kw_, qn], [ps])
            m.op("pe", lambda e: e.matmul(ps[:], lhsT=identb[:], rhs=MW[:, dl + 384:dl + 384 + 512], start=False, stop=True), [identb, MW], [ps])
            pp = Bp.next()
            m.op("act", lambda e: e.activation(out=pp[:], in_=ps[:], func=AF.Exp), [ps], [pp])
            m.op("pe", lambda e: e.matmul(po[:], lhsT=vw_[:, c, :], rhs=pp[:], start=(wi == 0), stop=(wi == len(wt) - 1)), [vw_, pp], [po])
            m.op("pe", lambda e: e.matmul(prs[:], lhsT=onesb[:], rhs=pp[:], start=(wi == 0), stop=(wi == len(wt) - 1)), [onesb, pp], [prs])
        finish_branch(2, po, prs, False)
        prev_kw, prev_vw = kw_cur, vw_cur
        m.dma(mx.dq.next(), mx.y_out[256:384, q0:q0 + 512], osum[:, 0:512], reads=[osum], writes=[mx.y_out])


def src_t(x):
    return x.t if hasattr(x, "t") else x


_PROGS = {}


def _prog(key):
    if key not in _PROGS:
        if key == "A":
            _PROGS[key] = build_dense(False, True)[0]
        elif key == "CA":
            _PROGS[key] = build_dense(True, True)[0]
        elif key == "C":
            _PROGS[key] = build_dense(True, False)[0]
        elif key == "M":
            _PROGS[key] = build_mixer()[0]
    return _PROGS[key]


def _halo_cols(aT, tb):
    if tb == 0:
        return np.ascontiguousarray(np.concatenate([np.zeros((aT.shape[0], HALO), aT.dtype), aT[:, 0:NTOK]], axis=1))
    return np.ascontiguousarray(aT[:, tb - HALO:tb + NTOK])


def kernel(**inp):
    inp = {k: np.asarray(v) for k, v in inp.items()}
    x = inp["x"]
    cores = list(range(NCORE))
    xT = [np.ascontiguousarray(x[b].T) for b in range(NB)]
    cst = make_consts()
    ncst = make_nsa_consts()
    dps = [pack_dense_params(inp["attn_norm"][l], inp["ffn_norm"][l], inp["ffn_conv_w"][l], inp["ffn_conv_b"][l])
           for l in range(DEPTH)]
    maps = []
    for c in cores:
        b, tb = c // 4, (c % 4) * NTOK
        maps.append({"x_in": np.ascontiguousarray(xT[b][:, tb:tb + NTOK]), "dpa": dps[0], "w_in_a": inp["w_in"][0]})
    res = run_bass_kernel_spmd(_prog("A"), maps, core_ids=cores).results
    proj = [r["proj_out"] for r in res]
    gates = [r["gates_out"] for r in res]
    for l in range(DEPTH):
        projT = [np.concatenate([proj[b * 4 + i] for i in range(4)], axis=1) for b in range(NB)]
        maps = []
        for c in cores:
            b, j = c // 4, c % 4
            d = pack_mixer_inputs(l, b, j, projT[b], inp)
            d["cst"] = cst
            d.update(ncst)
            maps.append(d)
        res = run_bass_kernel_spmd(_prog("M"), maps, core_ids=cores).results
        yT = []
        for b in range(NB):
            y = np.empty((4 * 512, S), np.float32)
            for j in range(4):
                yo = res[b * 4 + j]["y_out"]
                for n in range(4):
                    y[n * 512 + j * 128:n * 512 + (j + 1) * 128] = yo[n * 128:(n + 1) * 128]
            yT.append(y)
        del res, maps
        last = (l == DEPTH - 1)
        maps = []
        for c in cores:
            b, tb = c // 4, (c % 4) * NTOK
            d = {"x_in": _halo_cols(xT[b], tb), "y_in": _halo_cols(yT[b], tb), "g_in": gates[c], "dpc": dps[l],
                 "w_in_c": inp["w_in"][l], "w_branch": inp["w_branch"][l].reshape(4 * 512, D), "w_out": inp["w_out"][l],
                 "ffn_up": inp["ffn_up"][l], "ffn_down": inp["ffn_down"][l]}
            if not last:
                d["dpa"] = dps[l + 1]
                d["w_in_a"] = inp["w_in"][l + 1]
            maps.append(d)
        res = run_bass_kernel_spmd(_prog("C" if last else "CA"), maps, core_ids=cores).results
        for c in cores:
            b, tb = c // 4, (c % 4) * NTOK
            xT[b][:, tb:tb + NTOK] = res[c]["x_out"]
        if not last:
            proj = [r["proj_out"] for r in res]
            gates = [r["gates_out"] for r in res]
        del res, maps
    out = np.stack([xT[b].T for b in range(NB)], axis=0)
    return np.ascontiguousarray(out.astype(np.float32))
```

```python
import numpy as np
import concourse.bass as bass
import concourse.mybir as mybir
from concourse.bass_utils import run_bass_kernel_spmd

F32 = mybir.dt.float32
BF16 = mybir.dt.bfloat16
AF = mybir.ActivationFunctionType
ALU = mybir.AluOpType
AX = mybir.AxisListType

D = 2048
S = 8192
NB = 2
DEPTH = 4
NCORE = 8
NTOK = 2048
HALO = 2
DFF = 5632
NMIX = 5852
NIN = 14044
KC = D // 128
import os as _os
SAME_ENGINE_SYNC = not bool(_os.environ.get("MK_NOSAME"))
RW_NOSAME = not bool(_os.environ.get("RW_SAME"))
RW_NOINC = bool(_os.environ.get("RW_NOINC"))


class T:
    __slots__ = ("h", "lw", "rd", "name")

    def __init__(self, h, name):
        self.h = h
        self.name = name
        self.lw = None
        self.rd = {}

    def __getitem__(self, idx):
        return self.h[idx]


class MK:
    NDMA = 24

    def __init__(self, nc):
        self.nc = nc
        self.same = SAME_ENGINE_SYNC
        self.nosame_now = False
        self.strict = []
        self.eng = {"pe": nc.tensor, "act": nc.scalar, "dve": nc.vector, "pool": nc.gpsimd, "sp": nc.sync}
        self.sem = {k: nc.alloc_semaphore("s_" + k) for k in self.eng}
        self.cnt = {k: 0 for k in self.eng}
        self.waited = {k: {} for k in self.eng}
        self.dsem, self.dval, self.dnext = {}, {}, {}
        for q in ("sp", "act", "pool"):
            self.dsem[q] = [nc.alloc_semaphore("d_%s_%d" % (q, i)) for i in range(self.NDMA)]
            self.dval[q] = [0] * self.NDMA
            self.dnext[q] = 0
        self.ntile = 0
        self.ninst = 0

    def sb(self, shape, dtype=F32, name=None):
        self.ntile += 1
        name = name or ("t%d" % self.ntile)
        return T(self.nc.alloc_sbuf_tensor(name, list(shape), dtype), name)

    def ps(self, shape, dtype=F32, name=None):
        self.ntile += 1
        name = name or ("p%d" % self.ntile)
        return T(self.nc.alloc_psum_tensor(name, list(shape), dtype), name)

    def dram(self, name, shape, dtype=F32, kind="Internal"):
        return T(self.nc.dram_tensor(name, list(shape), dtype, kind=kind), name)

    def _wait(self, e, key, val):
        if val <= 0:
            return
        w = self.waited[e]
        if w.get(key, 0) >= val:
            return
        w[key] = val
        if isinstance(key, str):
            self.eng[e].wait_ge(self.sem[key], val)
        else:
            q, i = key
            self.eng[e].wait_ge(self.dsem[q][i], val)

    def _deps(self, e, reads, writes):
        same = self.same and e != "pe" and not self.nosame_now
        for t in reads:
            if t.lw is not None and (t.lw[0] != e or same or (t in self.strict and e != "pe")):
                self._wait(e, *t.lw)
        for t in writes:
            if t.lw is not None and (t.lw[0] != e or same):
                self._wait(e, *t.lw)
            for k, v in t.rd.items():
                if k != e or same:
                    self._wait(e, k, v)

    def op(self, e, fn, reads=(), writes=(), inc=True):
        self._deps(e, reads, writes)
        inst = fn(self.eng[e])
        if inc:
            self.cnt[e] += 1
            inst.then_inc(self.sem[e], 1)
            c = self.cnt[e]
        else:
            c = self.cnt[e] + 1
        for t in reads:
            t.rd[e] = c
        for t in writes:
            t.lw = (e, c)
            t.rd = {}
        self.ninst += 1
        return inst

    def dma(self, q, out, in_, reads=(), writes=(), **kw):
        i = self.dnext[q]
        self.dnext[q] = (i + 1) % self.NDMA
        key = (q, i)
        self._wait(q, key, self.dval[q][i])
        self._deps(q, reads, writes)
        inst = self.eng[q].dma_start(out=out, in_=in_, **kw)
        self.dval[q][i] += 16
        inst.then_inc(self.dsem[q][i], 16)
        v = self.dval[q][i]
        for t in reads:
            t.rd[key] = v
        for t in writes:
            t.lw = (key, v)
            t.rd = {}
        self.ninst += 1
        return inst

    def barrier(self):
        for e in ("pe", "act", "dve", "pool", "sp"):
            for o in ("pe", "act", "dve", "pool", "sp"):
                if o != e:
                    self._wait(e, o, self.cnt[o])
            for q in ("sp", "act", "pool"):
                for i in range(self.NDMA):
                    self._wait(e, (q, i), self.dval[q][i])

    def finish(self):
        for e in ("pe", "act", "dve", "pool"):
            self._wait("sp", e, self.cnt[e])
        for q in ("sp", "act", "pool"):
            for i in range(self.NDMA):
                self._wait("sp", (q, i), self.dval[q][i])


class Arena:
    def __init__(self, m, nbytes):
        self.m = m
        nc = m.nc
        top = 229344
        self.base = ((top - nc.sbuf_bytes_remaining) + 63) // 64 * 64
        self.slab = nc.alloc_sbuf_tensor("arena_slab", [128, nbytes + 64], mybir.dt.uint8)
        self.size = nbytes
        self.off = 0

    def reset(self):
        self.m.barrier()
        self.off = 0

    def sb(self, shape, dtype=F32, name=None):
        esz = 4 if dtype == F32 else 2
        n = 1
        for d_ in shape[1:]:
            n *= d_
        nb = (n * esz + 63) // 64 * 64
        assert self.off + nb <= self.size, "arena overflow %s %d+%d>%d" % (name, self.off, nb, self.size)
        h = self.m.nc.alloc_sbuf_tensor_at(name or "ar", list(shape), dtype, offset=self.base + self.off)
        self.off += nb
        return T(h, name)


class Rot:
    def __init__(self, items):
        self.items = items
        self.i = 0

    def next(self):
        t = self.items[self.i]
        self.i = (self.i + 1) % len(self.items)
        return t


DP_ATTN = 0
DP_FFN = 16
DP_CW = 32
DP_CB = DP_CW + 264
DP_N = DP_CB + 88


def pack_dense_params(attn_norm, ffn_norm, conv_w, conv_b):
    out = np.zeros((128, DP_N), np.float32)
    out[:, DP_ATTN:DP_ATTN + 16] = attn_norm.reshape(16, 128).T
    out[:, DP_FFN:DP_FFN + 16] = ffn_norm.reshape(16, 128).T
    out[:, DP_CW:DP_CW + 264] = conv_w.reshape(3, 88, 128).transpose(2, 0, 1).reshape(128, 264)
    out[:, DP_CB:DP_CB + 88] = conv_b.reshape(88, 128).T
    return out


class DenseCtx:
    def __init__(self, m):
        self.m = m
        self.banks = Rot([m.ps([128, 512], F32, "bank%d" % i) for i in range(6)])
        self.sbank = m.ps([128, 512], F32, "sbank")
        self.ones = m.sb([128, 128], F32, "ones")
        m.op("pool", lambda e: e.memset(self.ones[:], 1.0), [], [self.ones])
        self.buf1 = m.sb([128, KC, NTOK + HALO], BF16, "buf1")
        self.buf2 = m.sb([128, 44 * 512], BF16, "buf2")
        self.xs = Rot([m.sb([128, 512], F32, "xs%d" % i) for i in range(4)])
        self.sq = Rot([m.sb([128, 512], F32, "sq%d" % i) for i in range(2)])
        self.rstd = m.sb([128, 512], F32, "rstd")
        self.wb = Rot([m.sb([128, 5632], BF16, "wb%d" % i) for i in range(2)])
        self.ev = Rot([m.sb([128, 512], F32, "ev%d" % i) for i in range(3)])
        self.evb = Rot([m.sb([128, 512], BF16, "evb%d" % i) for i in range(4)])
        self.scr = Rot([m.sb([128, 516], F32, "scr%d" % i) for i in range(7)])
        self.dq = Rot(["sp", "act"])


def rmsnorm_T(cx, x_dram, gain, tiles, dst, dst_off=0):
    m = cx.m
    for (t0, n) in tiles:
        for kc in range(KC):
            xs = cx.xs.next()
            m.dma("sp", xs[:, 0:n], x_dram[kc * 128:(kc + 1) * 128, t0:t0 + n], reads=[x_dram], writes=[xs])
            sq = cx.sq.next()
            m.op("act", lambda e: e.activation(out=sq[:, 0:n], in_=xs[:, 0:n], func=AF.Square), [xs], [sq])
            m.op("pe", lambda e: e.matmul(cx.sbank[:, 0:n], lhsT=cx.ones[:], rhs=sq[:, 0:n],
                                          start=(kc == 0), stop=(kc == KC - 1)), [cx.ones, sq], [cx.sbank])
        m.op("act", lambda e: e.activation(out=cx.rstd[:, 0:n], in_=cx.sbank[:, 0:n], func=AF.Sqrt,
                                           scale=1.0 / D, bias=1e-6), [cx.sbank], [cx.rstd])
        m.op("dve", lambda e: e.reciprocal(out=cx.rstd[:, 0:n], in_=cx.rstd[:, 0:n]), [cx.rstd], [cx.rstd])
        for kc in range(KC):
            xs = cx.xs.next()
            m.dma("sp", xs[:, 0:n], x_dram[kc * 128:(kc + 1) * 128, t0:t0 + n], reads=[x_dram], writes=[xs])
            m.op("dve", lambda e: e.scalar_tensor_tensor(
                out=dst[:, kc, dst_off + t0:dst_off + t0 + n], in0=xs[:, 0:n], scalar=gain[:, kc:kc + 1],
                in1=cx.rstd[:, 0:n], op0=ALU.mult, op1=ALU.mult), [xs, gain_t(gain), cx.rstd], [dst_t(dst)])


def gain_t(g):
    return g.t if hasattr(g, "t") else g


def dst_t(d):
    return d.t if hasattr(d, "t") else d


class V:
    def __init__(self, t, ap):
        self.t = t
        self.ap = ap

    def __getitem__(self, idx):
        return self.ap[idx]


def linear_T(cx, w_dram, row0, K, col_groups, src, src_off, tiles, epilogue, wq="pool"):
    m = cx.m
    kcn = K // 128
    for gi, grp in enumerate(col_groups):
        wb = cx.wb.next()
        gw = sum(nc_ for (_, nc_) in grp)
        wv = wb[:, 0:kcn * gw].rearrange("p (k c) -> p k c", k=kcn)
        off = 0
        offs = []
        for (c0, ncols) in grp:
            for k0 in range(0, kcn, 16):
                k1 = min(kcn, k0 + 16)
                src_ap = w_dram[row0 + k0 * 128:row0 + k1 * 128, c0:c0 + ncols].rearrange("(kc p) c -> p kc c", p=128)
                m.dma(wq, wv[:, k0:k1, off:off + ncols], src_ap, reads=[w_dram], writes=[wb])
            offs.append(off)
            off += ncols
        for si, (c0, ncols) in enumerate(grp):
            for ti, (t0, n) in enumerate(tiles):
                ps = cx.banks.next()
                for kc in range(kcn):
                    m.op("pe", lambda e: e.matmul(ps[0:ncols, 0:n], lhsT=wv[:, kc, offs[si]:offs[si] + ncols],
                                                  rhs=src[:, kc, src_off + t0:src_off + t0 + n],
                                                  start=(kc == 0), stop=(kc == kcn - 1)), [wb, dst_t(src)], [ps])
                epilogue(ps, gi, si, c0, ncols, ti, t0, n)


def groups_of(c_start, c_end, gw=256):
    out = []
    c = c_start
    while c < c_end:
        g = []
        ge = min(c + gw, c_end)
        while c < ge:
            n = min(128, ge - c)
            g.append((c, n))
            c += n
        out.append(g)
    return out


def phase_A(cx, x_dram, x_tiles, dp, w_in, proj_out, gates_out):
    m = cx.m
    gain = V(dp, dp[:, DP_ATTN:DP_ATTN + 16])
    tb = x_tiles[0][0]
    rmsnorm_T(cx, x_dram, gain, x_tiles, cx.buf1, dst_off=-tb)
    tiles = [(t0 - tb, n) for (t0, n) in x_tiles]

    def ep_mix(ps, gi, si, c0, ncols, ti, t0, n):
        ev = cx.ev.next()
        if (ti + si) % 2 == 0:
            m.op("act", lambda e: e.activation(out=ev[0:ncols, 0:n], in_=ps[0:ncols, 0:n], func=AF.Copy), [ps], [ev])
        else:
            m.op("dve", lambda e: e.tensor_copy(out=ev[0:ncols, 0:n], in_=ps[0:ncols, 0:n]), [ps], [ev])
        m.dma(cx.dq.next(), proj_out[c0:c0 + ncols, t0:t0 + n], ev[0:ncols, 0:n], reads=[ev], writes=[proj_out])

    def ep_gate(ps, gi, si, c0, ncols, ti, t0, n):
        ev = cx.evb.next()
        m.op("act", lambda e: e.activation(out=ev[0:ncols, 0:n], in_=ps[0:ncols, 0:n], func=AF.Sigmoid), [ps], [ev])
        m.dma(cx.dq.next(), gates_out[c0 - NMIX:c0 - NMIX + ncols, t0:t0 + n], ev[0:ncols, 0:n], reads=[ev],
              writes=[gates_out])

    linear_T(cx, w_in, 0, D, groups_of(0, NMIX), cx.buf1, 0, tiles, ep_mix)
    linear_T(cx, w_in, 0, D, groups_of(NMIX, NIN), cx.buf1, 0, tiles, ep_gate)


def phase_C(cx, x_dram, y_dram, gates_dram, dp, w_in, w_branch, w_out, ffn_up, ffn_down, xmid, x_out):
    m = cx.m
    NT = NTOK + HALO
    tiles = [(0, HALO)] + [(HALO + i * 512, 512) for i in range(NTOK // 512)]
    CSTOP = 9
    for r in range(16):
        m.dma("pool", cx.buf1[:, r, :], y_dram[r * 128:(r + 1) * 128, :], reads=[y_dram], writes=[cx.buf1])
    if CSTOP <= 2:
        return
    mview = V(cx.buf2, cx.buf2[:, 0:KC * 512].rearrange("p (k t) -> p k t", k=KC))
    for ti, (t0, n) in enumerate(tiles):
        for dt_ in range(KC):
            wb = cx.wb.next()
            wv = wb[:, 0:16 * 128].rearrange("p (k c) -> p k c", k=16)
            m.dma("pool", wv, w_branch[:, dt_ * 128:(dt_ + 1) * 128].rearrange("(r p) c -> p r c", p=128),
                  reads=[w_branch], writes=[wb])
            tmps = []
            for nb in range(4):
                ps = cx.banks.next()
                for kc in range(4):
                    r = nb * 4 + kc
                    m.op("pe", lambda e: e.matmul(ps[:, 0:n], lhsT=wv[:, r, :], rhs=cx.buf1[:, r, t0:t0 + n],
                                                  start=(kc == 0), stop=(kc == 3)), [wb, cx.buf1], [ps])
                tp = cx.scr.next()
                g = cx.evb.next()
                m.dma(cx.dq.next(), g[:, 0:n], gates_dram[nb * D + dt_ * 128:nb * D + (dt_ + 1) * 128, t0:t0 + n],
                      reads=[gates_dram], writes=[g])
                m.op("dve", lambda e: e.tensor_tensor(out=tp[:, 0:n], in0=ps[:, 0:n], in1=g[:, 0:n], op=ALU.mult), [ps, g], [tp])
                tmps.append(tp)
            m.op("pool", lambda e: e.tensor_tensor(out=tmps[0][:, 0:n], in0=tmps[0][:, 0:n], in1=tmps[1][:, 0:n], op=ALU.add),
                 [tmps[0], tmps[1]], [tmps[0]])
            m.op("pool", lambda e: e.tensor_tensor(out=tmps[2][:, 0:n], in0=tmps[2][:, 0:n], in1=tmps[3][:, 0:n], op=ALU.add),
                 [tmps[2], tmps[3]], [tmps[2]])
            m.op("pool", lambda e: e.tensor_tensor(out=mview[:, dt_, 0:n], in0=tmps[0][:, 0:n],
                                                   in1=tmps[2][:, 0:n], op=ALU.add), [tmps[0], tmps[2]], [cx.buf2])

        def ep_out(ps, gi, si, c0, ncols, ti_, t0_, n_, t0=t0):
            xs = cx.xs.next()
            m.dma("sp", xs[:, 0:n_], x_dram[c0:c0 + 128, t0:t0 + n_], reads=[x_dram], writes=[xs])
            ev = cx.ev.next()
            m.op("dve", lambda e: e.tensor_tensor(out=ev[:, 0:n_], in0=ps[:, 0:n_], in1=xs[:, 0:n_], op=ALU.add), [ps, xs], [ev])
            m.dma(cx.dq.next(), xmid[c0:c0 + 128, t0:t0 + n_], ev[:, 0:n_], reads=[ev], writes=[xmid])

        linear_T(cx, w_out, 0, D, groups_of(0, D), mview, 0, [(0, n)], ep_out)
    if CSTOP <= 3:
        return
    gain2 = V(dp, dp[:, DP_FFN:DP_FFN + 16])
    rmsnorm_T(cx, xmid, gain2, tiles, cx.buf1)
    if CSTOP <= 4:
        return
    carry = m.sb([128, 88, 2], F32, "carry")
    m.op("dve", lambda e: e.memset(carry[:], 0.0), [], [carry])
    G = V(cx.buf2, cx.buf2[:, 0:44 * 512].rearrange("p (k t) -> p k t", k=44))
    U = cx.scr
    CV = cx.scr
    passes = [[tiles[0], tiles[1]], [tiles[2]], [tiles[3]], [tiles[4]]]
    for pi, ptiles in enumerate(passes):
        conv_out = {}

        def ep_up(ps, gi, si, c0, ncols, ti, t0, n):
            ct = c0 // 128
            u = U.next()
            m.op("act", lambda e: e.activation(out=u[:, 2:2 + n], in_=ps[:, 0:n], func=AF.Copy), [ps], [u])
            m.op("dve", lambda e: e.tensor_copy(out=u[:, 0:2], in_=carry[:, ct, :]), [carry], [u])
            m.op("dve", lambda e: e.tensor_copy(out=carry[:, ct, :], in_=u[:, n:n + 2]), [u], [carry])
            if n == HALO:
                return
            cv = CV.next()
            w = lambda k: dp[:, DP_CW + k * 88 + ct:DP_CW + k * 88 + ct + 1]
            m.op("dve", lambda e: e.tensor_scalar(out=cv[:, 0:n], in0=u[:, 2:2 + n], scalar1=w(2),
                                                  scalar2=dp[:, DP_CB + ct:DP_CB + ct + 1], op0=ALU.mult, op1=ALU.add),
                 [u, dp], [cv])
            m.op("dve", lambda e: e.scalar_tensor_tensor(out=cv[:, 0:n], in0=u[:, 1:1 + n], scalar=w(1), in1=cv[:, 0:n],
                                                         op0=ALU.mult, op1=ALU.add), [u, dp, cv], [cv])
            m.op("dve", lambda e: e.scalar_tensor_tensor(out=cv[:, 0:n], in0=u[:, 0:n], scalar=w(0), in1=cv[:, 0:n],
                                                         op0=ALU.mult, op1=ALU.add), [u, dp, cv], [cv])
            if si == 0:
                sg = CV.next()
                m.op("act", lambda e: e.activation(out=sg[:, 0:n], in_=cv[:, 0:n], func=AF.Silu), [cv], [sg])
                conv_out["g"] = sg
            else:
                sg = conv_out["g"]
                m.op("dve", lambda e: e.tensor_tensor(out=G[:, gi, 0:n], in0=sg[:, 0:n], in1=cv[:, 0:n], op=ALU.mult),
                     [sg, cv], [cx.buf2])

        grps = [[(j * 128, 128), (DFF + j * 128, 128)] for j in range(44)]
        linear_T(cx, ffn_up, 0, D, grps, cx.buf1, 0, ptiles, ep_up)
        (t0r, nr) = ptiles[-1]
        if CSTOP == 5:
            continue

        def ep_down(ps, gi, si, c0, ncols, ti, t0, n):
            xs = cx.xs.next()
            m.dma("sp", xs[:, 0:n], xmid[c0:c0 + 128, t0r:t0r + n], reads=[xmid], writes=[xs])
            ev = cx.ev.next()
            m.op("dve", lambda e: e.tensor_tensor(out=ev[:, 0:n], in0=ps[:, 0:n], in1=xs[:, 0:n], op=ALU.add), [ps, xs], [ev])
            m.dma(cx.dq.next(), x_out[c0:c0 + 128, t0r - HALO:t0r - HALO + n], ev[:, 0:n], reads=[ev], writes=[x_out])

        linear_T(cx, ffn_down, 0, DFF, [[(j * 128, 128)] for j in range(KC)], G, 0, [(0, nr)], ep_down)


def build_dense(do_C, do_A):
    nc = bass.Bass("TRN2", target_bir_lowering=False)
    m = MK(nc)
    cx = DenseCtx(m)
    NT = NTOK + HALO
    ei = lambda name, shape, dt=F32: m.dram(name, shape, dt, kind="ExternalInput")
    eo = lambda name, shape, dt=F32: m.dram(name, shape, dt, kind="ExternalOutput")
    outs = []
    if do_C:
        x_in = ei("x_in", [D, NT])
        y_in = ei("y_in", [4 * 512, NT])
        g_in = ei("g_in", [4 * D, NT], BF16)
        dpc = ei("dpc", [128, DP_N])
        w_branch = ei("w_branch", [4 * 512, D])
        w_out = ei("w_out", [D, D])
        ffn_up = ei("ffn_up", [D, 2 * DFF])
        ffn_down = ei("ffn_down", [DFF, D])
        xmid = m.dram("xmid", [D, NT])
        x_out = eo("x_out", [D, NTOK])
        dpc_s = m.sb([128, DP_N], F32, "dpc_s")
        m.dma("sp", dpc_s[:], dpc[:], reads=[dpc], writes=[dpc_s])
        phase_C(cx, x_in, y_in, g_in, dpc_s, None, w_branch, w_out, ffn_up, ffn_down, xmid, x_out)
        xa, xa_tiles = x_out, [(i * 512, 512) for i in range(NTOK // 512)]
    else:
        xa = ei("x_in", [D, NTOK])
        xa_tiles = [(i * 512, 512) for i in range(NTOK // 512)]
    if do_A:
        dpa = ei("dpa", [128, DP_N])
        w_in_a = ei("w_in_a", [D, NIN])
        proj_out = eo("proj_out", [NMIX, NTOK])
        gates_out = eo("gates_out", [4 * D, NTOK], BF16)
        dpa_s = m.sb([128, DP_N], F32, "dpa_s")
        m.dma("sp", dpa_s[:], dpa[:], reads=[dpa], writes=[dpa_s])
        phase_A(cx, xa, xa_tiles, dpa_s, w_in_a, proj_out, gates_out)
    m.finish()
    return nc, m


R_GQ, R_GK, R_GV, R_GG, R_GLR = 0, 64, 128, 256, 384
R_LX, R_LG = 512, 640
R_NQ, R_NKV, R_NG = 768, 1280, 2048
R_RR, R_RK, R_RV, R_XW, R_XA, R_XG = 2176, 2304, 2432, 2560, 2688, 2816
R_NQO = 3072
MIX_ROWS = 3200
MP_GB, MP_GGAIN = 0, 1
MP_LCW, MP_LCB, MP_LBA, MP_LBI, MP_LLAM = 2, 6, 7, 8, 9
MP_NQG, MP_NKG = 10, 11
MP_MUR, MP_MUK, MP_MUV, MP_MUW, MP_MUA, MP_MUG = 12, 13, 14, 15, 16, 17
MP_W0, MP_A0, MP_KK, MP_KA, MP_RK, MP_LNW, MP_LNB = 19, 20, 21, 22, 23, 24, 25
MP_POS = 26
MP_N = 26 + 32
C_ID, C_BONES, C_CAUS, C_DELTA, C_CMASK = 0, 128, 256, 320, 384
C_REV = 896
C_M8 = 1024
CN = 1024 + 512


def make_consts():
    c = np.zeros((128, CN), np.float32)
    c[:, C_ID:C_ID + 128] = np.eye(128)
    p = np.arange(128)
    c[:, C_BONES:C_BONES + 128] = (p[:, None] // 64 == p[None, :] // 64)
    s = np.arange(64)
    c[0:64, C_CAUS:C_CAUS + 64] = (s[:, None] <= s[None, :])
    c[:, C_DELTA:C_DELTA + 64] = ((p[:, None] % 64) == s[None, :])
    cm = np.ones((128, 512), np.float32)
    cm[:, 0::64] = 0.0
    c[:, C_CMASK:C_CMASK + 512] = cm
    c[:, C_REV:C_REV + 128] = np.eye(128)[::-1]
    m8 = np.ones((128, 512), np.float32)
    m8[:, 0::8] = 0.0
    c[:, C_M8:C_M8 + 512] = m8
    return c


class MixCtx:
    def __init__(self, m, mix_in, y_out, mp, cst):
        self.m = m
        self.mix_in = mix_in
        self.y_out = y_out
        self.banks = Rot([m.ps([128, 512], F32, "mbank%d" % i) for i in range(8)])
        self.mp = m.sb([128, MP_N], F32, "mp_s")
        m.dma("sp", self.mp[:], mp[:], reads=[mp], writes=[self.mp])
        self.cst = m.sb([128, CN], F32, "cst_s")
        m.dma("sp", self.cst[:], cst[:], reads=[cst], writes=[self.cst])
        self.dq = Rot(["sp", "act"])
        self.arena = Arena(m, m.nc.sbuf_bytes_remaining - 1024)

    def col(self, c, rows=128):
        return self.mp[0:rows, c:c + 1]


def bcast_mid(base, reps):
    a = base.ap
    return bass.AP(tensor=base.tensor, offset=base.offset, ap=[[a[0][0], a[0][1]], [a[1][0], a[1][1]], [0, reps]])


def bcast_outer(t_ap, reps):
    a = t_ap.ap
    return bass.AP(tensor=t_ap.tensor, offset=t_ap.offset, ap=[[a[0][0], a[0][1]], [0, reps], [a[1][0], a[1][1]]])


def mixer_lru(mx, wa_d, wi_d, ntiles=S // 512):
    m, mp = mx.m, mx.mp
    ar = mx.arena
    ar.reset()
    c8 = ar.sb([128, 2], F32, "lru_c8")
    m.op("act", lambda e: e.activation(out=c8[:, 0:1], in_=mx.col(MP_LLAM), func=AF.Exp, scale=-1.0), [mp], [c8])
    m.op("act", lambda e: e.activation(out=c8[:, 0:1], in_=c8[:, 0:1], func=AF.Ln, bias=1.0), [c8], [c8])
    m.op("dve", lambda e: e.tensor_scalar(out=c8[:, 1:2], in0=c8[:, 0:1], scalar1=-16.0, scalar2=None, op0=ALU.mult), [c8], [c8])
    m.op("dve", lambda e: e.tensor_scalar(out=c8[:, 0:1], in0=c8[:, 0:1], scalar1=-8.0, scalar2=None, op0=ALU.mult), [c8], [c8])
    wa = ar.sb([128, 128], BF16, "lru_wa_s")
    wi = ar.sb([128, 128], BF16, "lru_wi_s")
    m.dma("pool", wa[:], wa_d[:], reads=[wa_d], writes=[wa])
    m.dma("pool", wi[:], wi_d[:], reads=[wi_d], writes=[wi])
    xt = Rot([ar.sb([128, 515], F32, "lru_xt%d" % i) for i in range(2)])
    F = Rot([ar.sb([128, 512], F32, "lru_f%d" % i) for i in range(10)])
    hb = Rot([ar.sb([128, 512], F32, "lru_h%d" % i) for i in range(2)])
    xcb = ar.sb([128, 512], BF16, "lru_xcb")
    prev_x, prev_h = None, None
    for ti in range(ntiles):
        t0 = ti * 512
        x = xt.next()
        m.dma("sp", x[:, 3:515], mx.mix_in[R_LX:R_LX + 128, t0:t0 + 512], reads=[mx.mix_in], writes=[x])
        if prev_x is None:
            m.op("dve", lambda e: e.memset(x[:, 0:3], 0.0), [], [x])
        else:
            m.op("dve", lambda e: e.tensor_copy(out=x[:, 0:3], in_=prev_x[:, 512:515]), [prev_x], [x])
        prev_x = x
        xc = F.next()
        m.op("dve", lambda e: e.tensor_scalar(out=xc[:], in0=x[:, 3:515], scalar1=mx.col(MP_LCW + 3), scalar2=mx.col(MP_LCB),
                                              op0=ALU.mult, op1=ALU.add), [x, mp], [xc])
        for k in range(3):
            m.op("dve", lambda e: e.scalar_tensor_tensor(out=xc[:], in0=x[:, k:k + 512], scalar=mx.col(MP_LCW + k), in1=xc[:],
                                                         op0=ALU.mult, op1=ALU.add), [x, mp, xc], [xc])
        m.op("act", lambda e: e.activation(out=xcb[:], in_=xc[:], func=AF.Copy), [xc], [xcb])
        pa, pi = mx.banks.next(), mx.banks.next()
        m.op("pe", lambda e: e.matmul(pa[:], lhsT=wa[:], rhs=xcb[:], start=True, stop=True), [wa, xcb], [pa])
        m.op("pe", lambda e: e.matmul(pi[:], lhsT=wi[:], rhs=xcb[:], start=True, stop=True), [wi, xcb], [pi])
        r, ig, a, a2 = F.next(), F.next(), F.next(), F.next()
        m.op("act", lambda e: e.activation(out=r[:], in_=pa[:], func=AF.Sigmoid, bias=mx.col(MP_LBA)), [pa, mp], [r])
        m.op("act", lambda e: e.activation(out=ig[:], in_=pi[:], func=AF.Sigmoid, bias=mx.col(MP_LBI)), [pi, mp], [ig])
        m.op("act", lambda e: e.activation(out=a[:], in_=r[:], func=AF.Exp, scale=c8[:, 0:1]), [r, c8], [a])
        m.op("act", lambda e: e.activation(out=a2[:], in_=r[:], func=AF.Exp, scale=c8[:, 1:2]), [r, c8], [a2])
        m.op("dve", lambda e: e.tensor_scalar(out=a2[:], in0=a2[:], scalar1=-1.0, scalar2=1.0, op0=ALU.mult, op1=ALU.add), [a2], [a2])
        m.op("act", lambda e: e.activation(out=a2[:], in_=a2[:], func=AF.Sqrt), [a2], [a2])
        m.op("dve", lambda e: e.tensor_tensor(out=ig[:], in0=ig[:], in1=xc[:], op=ALU.mult), [ig, xc], [ig])
        m.op("dve", lambda e: e.tensor_tensor(out=ig[:], in0=ig[:], in1=a2[:], op=ALU.mult), [ig, a2], [ig])
        h = hb.next()
        init = 0.0 if prev_h is None else prev_h[:, 511:512]
        m.op("dve", lambda e: e.tensor_tensor_scan(out=h[:], data0=a[:], data1=ig[:], initial=init, op0=ALU.mult, op1=ALU.add),
             [a, ig] + ([prev_h] if prev_h is not None else []), [h])
        prev_h = h
        g = F.next()
        m.dma("sp", g[:], mx.mix_in[R_LG:R_LG + 128, t0:t0 + 512], reads=[mx.mix_in], writes=[g])
        gl = F.next()
        m.op("act", lambda e: e.activation(out=gl[:], in_=g[:], func=AF.Gelu_apprx_tanh), [g], [gl])
        m.op("dve", lambda e: e.tensor_tensor(out=gl[:], in0=gl[:], in1=h[:], op=ALU.mult), [gl, h], [gl])
        m.dma(mx.dq.next(), mx.y_out[128:256, t0:t0 + 512], gl[:], reads=[gl], writes=[mx.y_out])


def mixer_gla(mx, wgk_d, ntiles=S // 512):
    m, mp, cst = mx.m, mx.mp, mx.cst
    ar = mx.arena
    ar.reset()
    wgk = ar.sb([16, 64], F32, "gla_wgk_s")
    m.dma("sp", wgk[:], wgk_d[:], reads=[wgk_d], writes=[wgk])
    nb = ar.sb([64, 1], F32, "gla_nb")
    m.op("dve", lambda e: e.tensor_scalar(out=nb[:], in0=mx.col(MP_GB, 64), scalar1=-1.0, scalar2=None, op0=ALU.mult), [mp], [nb])
    St = ar.sb([64, 128], F32, "gla_S")
    Sb = ar.sb([64, 128], BF16, "gla_Sb")
    m.op("dve", lambda e: e.memset(St[:], 0.0), [], [St])
    m.op("dve", lambda e: e.memset(Sb[:], 0.0), [], [Sb])
    ones = ar.sb([128, 128], F32, "gla_ones")
    m.op("dve", lambda e: e.memset(ones[:], 1.0), [], [ones])
    F64 = Rot([ar.sb([64, 512], F32, "gla_f%d" % i) for i in range(12)])
    F128 = Rot([ar.sb([128, 512], F32, "gla_F%d" % i) for i in range(8)])
    B16 = Rot([ar.sb([64, 512], BF16, "gla_b%d" % i) for i in range(4)])
    lrp = Rot([ar.sb([16, 512], F32, "gla_lr%d" % i) for i in range(2)])
    khT = Rot([ar.sb([64, 64], BF16, "gla_khT%d" % i) for i in range(3)])
    vT = Rot([ar.sb([64, 128], BF16, "gla_vT%d" % i) for i in range(3)])
    Am = Rot([ar.sb([64, 64], BF16, "gla_A%d" % i) for i in range(3)])
    for ti in range(ntiles):
        t0 = ti * 512
        q, k, lr = F64.next(), F64.next(), lrp.next()
        v, g = F128.next(), F128.next()
        m.dma("sp", q[:], mx.mix_in[R_GQ:R_GQ + 64, t0:t0 + 512], reads=[mx.mix_in], writes=[q])
        m.dma("sp", k[:], mx.mix_in[R_GK:R_GK + 64, t0:t0 + 512], reads=[mx.mix_in], writes=[k])
        m.dma("sp", v[:], mx.mix_in[R_GV:R_GV + 128, t0:t0 + 512], reads=[mx.mix_in], writes=[v])
        m.dma("sp", g[:], mx.mix_in[R_GG:R_GG + 128, t0:t0 + 512], reads=[mx.mix_in], writes=[g])
        m.dma("sp", lr[:], mx.mix_in[R_GLR:R_GLR + 16, t0:t0 + 512], reads=[mx.mix_in], writes=[lr])
        pz = mx.banks.next()
        m.op("pe", lambda e: e.matmul(pz[0:64, :], lhsT=wgk[:], rhs=lr[:], start=True, stop=True), [wgk, lr], [pz])
        la, Bc, E, Ei, kh = F64.next(), F64.next(), F64.next(), F64.next(), F64.next()
        m.op("act", lambda e: e.activation(out=la[:], in_=pz[0:64, :], func=AF.Exp, scale=-1.0, bias=nb[:]), [pz, nb], [la])
        m.op("act", lambda e: e.activation(out=la[:], in_=la[:], func=AF.Ln, bias=1.0), [la], [la])
        m.op("dve", lambda e: e.tensor_scalar(out=la[:], in0=la[:], scalar1=-1.0 / 16.0, scalar2=None, op0=ALU.mult), [la], [la])
        m.op("dve", lambda e: e.tensor_tensor_scan(out=Bc[:], data0=cst[0:64, C_CMASK:C_CMASK + 512], data1=la[:], initial=0.0,
                                                   op0=ALU.mult, op1=ALU.add), [cst, la], [Bc])
        m.op("act", lambda e: e.activation(out=E[:], in_=Bc[:], func=AF.Exp), [Bc], [E])
        m.op("act", lambda e: e.activation(out=Ei[:], in_=Bc[:], func=AF.Exp, scale=-1.0), [Bc], [Ei])
        qt, kt = B16.next(), B16.next()
        m.op("dve", lambda e: e.scalar_tensor_tensor(out=qt[:], in0=q[:], scalar=0.125, in1=E[:], op0=ALU.mult, op1=ALU.mult), [q, E], [qt])
        m.op("dve", lambda e: e.tensor_tensor(out=kt[:], in0=k[:], in1=Ei[:], op=ALU.mult), [k, Ei], [kt])
        for c in range(8):
            cs = slice(c * 64, (c + 1) * 64)
            m.op("dve", lambda e: e.scalar_tensor_tensor(out=kh[:, cs], in0=k[:, cs], scalar=E[:, c * 64 + 63:c * 64 + 64], in1=Ei[:, cs],
                                                         op0=ALU.mult, op1=ALU.mult), [k, E, Ei], [kh])
        o = F128.next()
        for c in range(8):
            cs = slice(c * 64, (c + 1) * 64)
            pT = mx.banks.next()
            m.op("pe", lambda e: e.transpose(out=pT[0:64, 0:64], in_=kh[:, cs], identity=cst[0:64, C_ID:C_ID + 64]), [kh, cst], [pT])
            m.op("pe", lambda e: e.transpose(out=pT[0:64, 64:192], in_=v[:, cs], identity=cst[:, C_ID:C_ID + 128]), [v, cst], [pT])
            kT_, vT_ = khT.next(), vT.next()
            m.op("act", lambda e: e.activation(out=kT_[:], in_=pT[0:64, 0:64], func=AF.Copy), [pT], [kT_])
            m.op("act", lambda e: e.activation(out=vT_[:], in_=pT[0:64, 64:192], func=AF.Copy), [pT], [vT_])
            pS = mx.banks.next()
            m.op("pe", lambda e: e.matmul(pS[0:64, 0:64], lhsT=kt[:, cs], rhs=qt[:, cs], start=True, stop=True), [kt, qt], [pS])
            A = Am.next()
            m.op("dve", lambda e: e.tensor_tensor(out=A[:], in0=pS[0:64, 0:64], in1=cst[0:64, C_CAUS:C_CAUS + 64], op=ALU.mult), [pS, cst], [A])
            pO = mx.banks.next()
            m.op("pe", lambda e: e.matmul(pO[:, 0:64], lhsT=vT_[:], rhs=A[:], start=True, stop=False), [vT_, A], [pO])
            m.op("pe", lambda e: e.matmul(pO[:, 0:64], lhsT=Sb[:], rhs=qt[:, cs], start=False, stop=True), [Sb, qt], [pO])
            m.op("act", lambda e: e.activation(out=o[:, cs], in_=pO[:, 0:64], func=AF.Copy), [pO], [o])
            pD = mx.banks.next()
            m.op("pe", lambda e: e.matmul(pD[0:64, 0:128], lhsT=kT_[:], rhs=vT_[:], start=True, stop=True), [kT_, vT_], [pD])
            m.op("dve", lambda e: e.scalar_tensor_tensor(out=St[:], in0=St[:], scalar=E[:, c * 64 + 63:c * 64 + 64], in1=pD[0:64, 0:128],
                                                         op0=ALU.mult, op1=ALU.add), [St, E, pD], [St])
            m.op("act", lambda e: e.activation(out=Sb[:], in_=St[:], func=AF.Copy), [St], [Sb])
        sq = F128.next()
        m.op("act", lambda e: e.activation(out=sq[:], in_=o[:], func=AF.Square), [o], [sq])
        pn = mx.banks.next()
        m.op("pe", lambda e: e.matmul(pn[:], lhsT=ones[:], rhs=sq[:], start=True, stop=True), [ones, sq], [pn])
        m.op("act", lambda e: e.activation(out=sq[:], in_=pn[:], func=AF.Sqrt, scale=1.0 / 128, bias=1e-6), [pn], [sq])
        m.op("dve", lambda e: e.reciprocal(out=sq[:], in_=sq[:]), [sq], [sq])
        m.op("dve", lambda e: e.scalar_tensor_tensor(out=o[:], in0=o[:], scalar=mx.col(MP_GGAIN), in1=sq[:], op0=ALU.mult, op1=ALU.mult),
             [o, mp, sq], [o])
        m.op("act", lambda e: e.activation(out=g[:], in_=g[:], func=AF.Silu), [g], [g])
        m.op("dve", lambda e: e.tensor_tensor(out=o[:], in0=o[:], in1=g[:], op=ALU.mult), [o, g], [o])
        m.dma(mx.dq.next(), mx.y_out[0:128, t0:t0 + 512], o[:], reads=[o], writes=[mx.y_out])


def mixer_rwkv(mx, wl_d, al_d, gl_d, ntiles=S // 512):
    m, mp, cst = mx.m, mx.mp, mx.cst
    ar = mx.arena
    ar.reset()
    wl = ar.sb([96, 128], F32, "rw_wl_s")
    al = ar.sb([96, 128], F32, "rw_al_s")
    gl = ar.sb([128, 2, 128], F32, "rw_gl_s")
    m.dma("sp", wl[:], wl_d[:], reads=[wl_d], writes=[wl])
    m.dma("sp", al[:], al_d[:], reads=[al_d], writes=[al])
    m.dma("sp", gl[:], gl_d[:, :].rearrange("(k p) c -> p k c", p=128), reads=[gl_d], writes=[gl])
    bones = cst[:, C_BONES:C_BONES + 128]
    St = ar.sb([128, 64], F32, "rw_S")
    m.op("dve", lambda e: e.memset(St[:], 0.0), [], [St])
    S2 = bass.AP(tensor=St.h, offset=0, ap=[[64, 128], [0, 2], [1, 64]])
    P2 = ar.sb([128, 2, 64], F32, "rw_P2")
    ft = Rot([ar.sb([128, 513], F32, "rw_ft%d" % i) for i in range(8)])
    prev = {}
    F = Rot([ar.sb([128, 512], F32, "rw_f%d" % i) for i in range(20)])
    XQ = Rot([ar.sb([128, 5, 512], F32, "rw_xq%d" % i) for i in range(2)])
    VM = Rot([ar.sb([128, 512], F32, "rw_vm%d" % i) for i in range(2)])
    SAZ = Rot([ar.sb([128, 512, 2], F32, "rw_saz%d" % i) for i in range(2)])
    Dt = Rot([ar.sb([128, 512], F32, "rw_D%d" % i) for i in range(int(_os.environ.get("RW_ND", "4")))])
    BC = Rot([ar.sb([128, 5, 512], F32, "rw_bc%d" % i) for i in range(int(_os.environ.get("RW_NBC", "2")))])
    delta = cst[:, C_DELTA:C_DELTA + 64]

    def shifted(name, row0, rows, mucol, t0):
        f = ft.next()
        m.dma("sp", f[0:rows, 1:513], mx.mix_in[row0:row0 + rows, t0:t0 + 512], reads=[mx.mix_in], writes=[f])
        if name not in prev:
            m.op("dve", lambda e: e.memset(f[0:rows, 0:1], 0.0), [], [f])
        else:
            p = prev[name]
            m.op("dve", lambda e: e.tensor_copy(out=f[0:rows, 0:1], in_=p[0:rows, 512:513]), [p], [f])
        prev[name] = f
        d = F.next()
        m.op("dve", lambda e: e.tensor_tensor(out=d[0:rows, :], in0=f[0:rows, 0:512], in1=f[0:rows, 1:513], op=ALU.subtract), [f], [d])
        m.op("dve", lambda e: e.scalar_tensor_tensor(out=d[0:rows, :], in0=d[0:rows, :], scalar=mp[0:rows, mucol:mucol + 1],
                                                     in1=f[0:rows, 1:513], op0=ALU.mult, op1=ALU.add), [d, mp, f], [d])
        return d

    def bsum(src, scale=None):
        ps = mx.banks.next()
        m.op("pe", lambda e: e.matmul(ps[:], lhsT=bones, rhs=src[:], start=True, stop=True), [cst, src], [ps])
        return ps

    for ti in range(ntiles):
        t0 = ti * 512
        r = shifted("r", R_RR, 128, MP_MUR, t0)
        k = shifted("k", R_RK, 128, MP_MUK, t0)
        v = shifted("v", R_RV, 128, MP_MUV, t0)
        vm = VM.next()
        m.op("act", lambda e: e.activation(out=vm[:], in_=v[:], func=AF.Copy), [v], [vm])
        xw = shifted("xw", R_XW, 96, MP_MUW, t0)
        xa = shifted("xa", R_XA, 96, MP_MUA, t0)
        xg0 = shifted("xg0", R_XG, 128, MP_MUG, t0)
        xg1 = shifted("xg1", R_XG + 128, 128, MP_MUG + 1, t0)
        xq = XQ.next()
        kk_, wr_, w_, nkka_, kp_ = (xq[:, i, :] for i in range(5))
        m.op("act", lambda e: e.activation(out=xw[0:96, :], in_=xw[0:96, :], func=AF.Tanh), [xw], [xw])
        pw = mx.banks.next()
        m.op("pe", lambda e: e.matmul(pw[:], lhsT=wl[:], rhs=xw[0:96, :], start=True, stop=True), [wl, xw], [pw])
        lw, Lc, wcm, winv = F.next(), F.next(), F.next(), F.next()
        m.op("act", lambda e: e.activation(out=lw[:], in_=pw[:], func=AF.Sigmoid, bias=mx.col(MP_W0)), [pw, mp], [lw])
        m.op("dve", lambda e: e.tensor_scalar(out=lw[:], in0=lw[:], scalar1=-0.6065306597126334, scalar2=None, op0=ALU.mult), [lw], [lw])
        m.op("dve", lambda e: e.tensor_tensor_scan(out=Lc[:], data0=cst[:, C_M8:C_M8 + 512], data1=lw[:], initial=0.0,
                                                   op0=ALU.mult, op1=ALU.add), [cst, lw], [Lc])
        m.op("act", lambda e: e.activation(out=w_, in_=Lc[:], func=AF.Exp), [Lc], [xq])
        m.op("act", lambda e: e.activation(out=winv[:], in_=Lc[:], func=AF.Exp, scale=-1.0), [Lc], [winv])
        m.op("dve", lambda e: e.tensor_tensor(out=wcm[:], in0=Lc[:], in1=lw[:], op=ALU.subtract), [Lc, lw], [wcm])
        m.op("act", lambda e: e.activation(out=wcm[:], in_=wcm[:], func=AF.Exp), [wcm], [wcm])
        pa = mx.banks.next()
        m.op("pe", lambda e: e.matmul(pa[:], lhsT=al[:], rhs=xa[0:96, :], start=True, stop=True), [al, xa], [pa])
        a = F.next()
        m.op("act", lambda e: e.activation(out=a[:], in_=pa[:], func=AF.Sigmoid, bias=mx.col(MP_A0)), [pa, mp], [a])
        m.op("act", lambda e: e.activation(out=xg0[:], in_=xg0[:], func=AF.Sigmoid), [xg0], [xg0])
        m.op("act", lambda e: e.activation(out=xg1[:], in_=xg1[:], func=AF.Sigmoid), [xg1], [xg1])
        pg = mx.banks.next()
        m.op("pe", lambda e: e.matmul(pg[:], lhsT=gl[:, 0, :], rhs=xg0[:], start=True, stop=False), [gl, xg0], [pg])
        m.op("pe", lambda e: e.matmul(pg[:], lhsT=gl[:, 1, :], rhs=xg1[:], start=False, stop=True), [gl, xg1], [pg])
        gg = F.next()
        m.op("act", lambda e: e.activation(out=gg[:], in_=pg[:], func=AF.Copy), [pg], [gg])
        m.op("dve", lambda e: e.tensor_scalar(out=kk_, in0=k[:], scalar1=mx.col(MP_KK), scalar2=None, op0=ALU.mult), [k, mp], [xq])
        sq = F.next()
        m.op("act", lambda e: e.activation(out=sq[:], in_=kk_, func=AF.Square), [xq], [sq])
        pn = bsum(sq)
        m.op("act", lambda e: e.activation(out=sq[:], in_=pn[:], func=AF.Sqrt), [pn], [sq])
        m.op("dve", lambda e: e.tensor_scalar(out=sq[:], in0=sq[:], scalar1=1e-12, scalar2=None, op0=ALU.max), [sq], [sq])
        m.op("dve", lambda e: e.reciprocal(out=sq[:], in_=sq[:]), [sq], [sq])
        m.op("dve", lambda e: e.tensor_tensor(out=kk_, in0=kk_, in1=sq[:], op=ALU.mult), [xq, sq], [xq])
        m.op("dve", lambda e: e.tensor_scalar(out=kp_, in0=a[:], scalar1=-1.0, scalar2=mx.col(MP_KA), op0=ALU.add, op1=ALU.mult), [a, mp], [xq])
        m.op("dve", lambda e: e.scalar_tensor_tensor(out=kp_, in0=kp_, scalar=1.0, in1=k[:], op0=ALU.add, op1=ALU.mult), [xq, k], [xq])
        m.op("dve", lambda e: e.scalar_tensor_tensor(out=nkka_, in0=kk_, scalar=-1.0, in1=a[:], op0=ALU.mult, op1=ALU.mult), [xq, a], [xq])
        m.op("dve", lambda e: e.tensor_tensor(out=wr_, in0=w_, in1=r[:], op=ALU.mult), [xq, r], [xq])
        p1, p2, p3 = F.next(), F.next(), F.next()
        m.op("dve", lambda e: e.tensor_tensor(out=p1[:], in0=nkka_, in1=r[:], op=ALU.mult), [xq, r], [p1])
        m.op("dve", lambda e: e.tensor_tensor(out=p2[:], in0=kp_, in1=r[:], op=ALU.mult), [xq, r], [p2])
        m.op("dve", lambda e: e.tensor_scalar(out=p3[:], in0=p2[:], scalar1=mx.col(MP_RK), scalar2=None, op0=ALU.mult), [p2, mp], [p3])
        c1, c2, c3 = bsum(p1), bsum(p2), bsum(p3)
        m.op("dve", lambda e: e.tensor_tensor(out=nkka_, in0=nkka_, in1=winv[:], op=ALU.mult), [xq, winv], [xq])
        m.op("dve", lambda e: e.tensor_tensor(out=kp_, in0=kp_, in1=winv[:], op=ALU.mult), [xq, winv], [xq])
        m.op("dve", lambda e: e.tensor_tensor(out=kk_, in0=kk_, in1=wcm[:], op=ALU.mult), [xq, wcm], [xq])
        m.op("act", lambda e: e.activation(out=p1[:], in_=c1[:], func=AF.Copy), [c1], [p1])
        m.op("act", lambda e: e.activation(out=p2[:], in_=c2[:], func=AF.Copy), [c2], [p2])
        m.op("act", lambda e: e.activation(out=p3[:], in_=c3[:], func=AF.Copy), [c3], [p3])
        saz = SAZ.next()
        for gi in range(64):
            bc = BC.next()
            for qi in range(5):
                dt_ = Dt.next()
                m.op("pool", lambda e: e.tensor_tensor(out=dt_[:, :].rearrange("p (t j) -> p t j", j=64),
                                                       in0=bcast_mid(xq[:, qi, gi * 8:gi * 8 + 8], 64),
                                                       in1=bcast_outer(delta, 8), op=ALU.mult), [xq, cst], [dt_])
                pb = mx.banks.next()
                m.op("pe", lambda e: e.matmul(pb[:], lhsT=bones, rhs=dt_[:], start=True, stop=True), [cst, dt_], [pb])
                m.op("act", lambda e: e.activation(out=bc[:, qi, :], in_=pb[:], func=AF.Copy), [pb], [bc])
            m.nosame_now = RW_NOSAME
            for tt in range(8):
                t = gi * 8 + tt
                js = slice(tt * 64, (tt + 1) * 64)
                m.op("dve", lambda e: e.tensor_tensor(out=P2[:], in0=S2, in1=bc[:, 0:2, js], op=ALU.mult), [St, bc], [P2])
                m.op("dve", lambda e: e.tensor_reduce(out=saz[:, t, :], in_=P2[:], axis=AX.X, op=ALU.add), [P2], [saz])
                m.op("dve", lambda e: e.scalar_tensor_tensor(out=St[:], in0=bc[:, 4, js], scalar=vm[:, t:t + 1], in1=St[:],
                                                             op0=ALU.mult, op1=ALU.add), [bc, vm, St], [St])
                m.strict = [saz]
                m.op("dve", lambda e: e.scalar_tensor_tensor(out=St[:], in0=bc[:, 3, js], scalar=saz[:, t, 0:1], in1=St[:],
                                                             op0=ALU.mult, op1=ALU.add), [bc, saz, St], [St])
                m.strict = []
                if tt == 7:
                    m.op("dve", lambda e: e.tensor_tensor(out=St[:], in0=St[:], in1=bc[:, 2, js], op=ALU.mult), [St, bc], [St])
            m.nosame_now = False
        y = F.next()
        m.op("pool", lambda e: e.tensor_tensor(out=y[:], in0=saz[:, :, 0], in1=p1[:], op=ALU.mult), [saz, p1], [y])
        m.op("pool", lambda e: e.tensor_tensor(out=y[:], in0=y[:], in1=saz[:, :, 1], op=ALU.add), [y, saz], [y])
        m.op("pool", lambda e: e.tensor_tensor(out=p2[:], in0=p2[:], in1=vm[:], op=ALU.mult), [p2, vm], [p2])
        m.op("pool", lambda e: e.tensor_tensor(out=y[:], in0=y[:], in1=p2[:], op=ALU.add), [y, p2], [y])
        pm_ = bsum(y)
        m.op("pool", lambda e: e.tensor_copy(out=p1[:], in_=y[:]), [y], [p1])
        m.op("dve", lambda e: e.scalar_tensor_tensor(out=y[:], in0=pm_[:], scalar=-1.0 / 64, in1=p1[:], op0=ALU.mult, op1=ALU.add),
             [pm_, p1], [y])
        m.op("act", lambda e: e.activation(out=p1[:], in_=y[:], func=AF.Square), [y], [p1])
        pv = bsum(p1)
        m.op("act", lambda e: e.activation(out=p1[:], in_=pv[:], func=AF.Sqrt, scale=1.0 / 64, bias=64e-5), [pv], [p1])
        m.op("dve", lambda e: e.reciprocal(out=p1[:], in_=p1[:]), [p1], [p1])
        m.op("dve", lambda e: e.tensor_tensor(out=y[:], in0=y[:], in1=p1[:], op=ALU.mult), [y, p1], [y])
        m.op("dve", lambda e: e.tensor_scalar(out=y[:], in0=y[:], scalar1=mx.col(MP_LNW), scalar2=mx.col(MP_LNB), op0=ALU.mult, op1=ALU.add),
             [y, mp], [y])
        m.op("dve", lambda e: e.tensor_tensor(out=p3[:], in0=p3[:], in1=vm[:], op=ALU.mult), [p3, vm], [p3])
        m.op("dve", lambda e: e.tensor_tensor(out=y[:], in0=y[:], in1=p3[:], op=ALU.add), [y, p3], [y])
        m.op("dve", lambda e: e.tensor_tensor(out=y[:], in0=y[:], in1=gg[:], op=ALU.mult), [y, gg], [y])
        m.dma(mx.dq.next(), mx.y_out[384:512, t0:t0 + 512], y[:], reads=[y], writes=[mx.y_out])


def build_mixer(which=("gla", "lru", "nsa", "rwkv"), ntiles=S // 512):
    nc = bass.Bass("TRN2", target_bir_lowering=False)
    m = MK(nc)
    ei = lambda name, shape, dt=F32: m.dram(name, shape, dt, kind="ExternalInput")
    mix_in = ei("mix_in", [MIX_ROWS, S])
    mp = ei("mp", [128, MP_N])
    cst = ei("cst", [128, CN])
    y_out = m.dram("y_out", [512, S], F32, kind="ExternalOutput")
    mx = MixCtx(m, mix_in, y_out, mp, cst)
    if "lru" in which:
        mixer_lru(mx, ei("lru_wa", [128, 128]), ei("lru_wi", [128, 128]), ntiles)
    if "gla" in which:
        mixer_gla(mx, ei("gla_wgk", [16, 64]), ntiles)
    if "nsa" in which:
        d = {"k1": ei("nsa_k1", [4096, 128]), "k2": ei("nsa_k2", [128, 128]), "v1": ei("nsa_v1", [4096, 128]),
             "v2": ei("nsa_v2", [128, 128]), "rb": ei("nsa_rb", [32, 5]), "oh": ei("nsa_oh", [33, LV]),
             "ovl": ei("nsa_ovl", [128, 4 * 129]), "selmask": ei("nsa_selmask", [64 * 128, 128]), "gsel": ei("nsa_gsel", [3, 384])}
        mixer_nsa(mx, d, ntiles)
    if "rwkv" in which:
        mixer_rwkv(mx, ei("rw_wl", [96, 128]), ei("rw_al", [96, 128]), ei("rw_gl", [256, 128]), ntiles)
    m.finish()
    return nc, m


def pack_mixer_inputs(l, b, j, projT, inp):
    mi = np.zeros((MIX_ROWS, S), np.float32)
    mi[R_GQ:R_GQ + 64] = projT[64 * j:64 * j + 64]
    mi[R_GK:R_GK + 64] = projT[256 + 64 * j:256 + 64 * j + 64]
    mi[R_GV:R_GV + 128] = projT[512 + 128 * j:512 + 128 * j + 128]
    mi[R_GG:R_GG + 128] = projT[1024 + 128 * j:1024 + 128 * j + 128]
    mi[R_GLR:R_GLR + 16] = projT[1536:1552]
    mi[R_LX:R_LX + 128] = projT[1552 + 128 * j:1552 + 128 * j + 128]
    mi[R_LG:R_LG + 128] = projT[2064 + 128 * j:2064 + 128 * j + 128]
    mi[R_NQ:R_NQ + 512] = projT[2576:3088]
    mi[R_NKV:R_NKV + 768] = projT[3088:3856]
    mi[R_NQO:R_NQO + 128] = projT[2576 + 128 * j:2576 + 128 * j + 128]
    for gi_ in range(3):
        mi[R_NG + gi_] = projT[3856 + gi_ * 4 + j]
    f0 = 3868
    mi[R_RR:R_RR + 128] = projT[f0 + 128 * j:f0 + 128 * j + 128]
    mi[R_RK:R_RK + 128] = projT[f0 + 512 + 128 * j:f0 + 512 + 128 * j + 128]
    mi[R_RV:R_RV + 128] = projT[f0 + 1024 + 128 * j:f0 + 1024 + 128 * j + 128]
    mi[R_XW:R_XW + 96] = projT[f0 + 1536:f0 + 1632]
    mi[R_XA:R_XA + 96] = projT[f0 + 1632:f0 + 1728]
    mi[R_XG:R_XG + 256] = projT[f0 + 1728:f0 + 1984]
    mp = np.zeros((128, MP_N), np.float32)
    sl = slice(128 * j, 128 * j + 128)
    mp[0:64, MP_GB] = inp["gla_b_gk"][l][64 * j:64 * j + 64]
    mp[:, MP_GGAIN] = inp["gla_out_norm"][l]
    for k in range(4):
        mp[:, MP_LCW + k] = inp["lru_conv_w"][l][k, sl]
    mp[:, MP_LCB] = inp["lru_conv_b"][l][sl]
    mp[:, MP_LBA] = inp["lru_b_a"][l][sl]
    mp[:, MP_LBI] = inp["lru_b_i"][l][sl]
    mp[:, MP_LLAM] = inp["lru_lambda"][l][sl]
    mp[:, MP_NQG] = inp["nsa_q_norm"][l]
    mp[:, MP_NKG] = inp["nsa_k_norm"][l]
    mu = inp["rwkv_mu"][l]
    mp[:, MP_MUR] = mu[128 * j:128 * j + 128]
    mp[:, MP_MUK] = mu[512 + 128 * j:512 + 128 * j + 128]
    mp[:, MP_MUV] = mu[1024 + 128 * j:1024 + 128 * j + 128]
    mp[0:96, MP_MUW] = mu[1536:1632]
    mp[0:96, MP_MUA] = mu[1632:1728]
    mp[:, MP_MUG] = mu[1728:1856]
    mp[:, MP_MUG + 1] = mu[1856:1984]
    mp[:, MP_W0] = inp["rwkv_w0"][l][sl]
    mp[:, MP_A0] = inp["rwkv_a0"][l][sl]
    mp[:, MP_KK] = inp["rwkv_k_k"][l][sl]
    mp[:, MP_KA] = inp["rwkv_k_a"][l][sl]
    mp[:, MP_RK] = inp["rwkv_r_k"][l].reshape(512)[sl]
    mp[:, MP_LNW] = inp["rwkv_ln_w"][l][sl]
    mp[:, MP_LNB] = inp["rwkv_ln_b"][l][sl]
    mp[:, MP_POS:MP_POS + 32] = inp["nsa_cmp_pos"][l].T
    rb = inp["rel_bias"]
    d = {"mix_in": mi, "mp": mp, "nsa_k1": inp["nsa_cmp_k1"][l], "nsa_k2": inp["nsa_cmp_k2"][l], "nsa_v1": inp["nsa_cmp_v1"][l],
         "nsa_v2": inp["nsa_cmp_v2"][l], "nsa_rb": np.ascontiguousarray(np.concatenate([rb, rb[:, j:j + 1]], axis=1)),
         "gla_wgk": np.ascontiguousarray(inp["gla_w_gk"][l][:, 64 * j:64 * j + 64]),
         "lru_wa": np.ascontiguousarray(inp["lru_w_a"][l][j]), "lru_wi": np.ascontiguousarray(inp["lru_w_i"][l][j]),
         "rw_wl": np.ascontiguousarray(inp["rwkv_w_lora"][l][:, sl]), "rw_al": np.ascontiguousarray(inp["rwkv_a_lora"][l][:, sl]),
         "rw_gl": np.ascontiguousarray(inp["rwkv_g_lora"][l][:, sl])}
    return d


NEG = -30000.0
LV_SEL = 4592
LV_WIN = 1536
LV = LV_SEL + LV_WIN
NCMP = 511


def _t5_bucket(n):
    n = np.asarray(n)
    nf = np.maximum(n, 1).astype(np.float32)
    large = 16 + (np.log(nf / np.float32(16)) / np.float32(np.log(128 / 16)) * np.float32(16)).astype(np.int32)
    large = np.minimum(large, 31)
    return np.where(n < 16, n, large)


def make_nsa_consts():
    oh = np.zeros((33, LV), np.float32)
    i = np.arange(LV_SEL)
    dist = i - 2063
    ok = dist >= 0
    b = _t5_bucket(np.maximum(dist, 0))
    oh[b[ok], i[ok]] += 1.0
    oh[31, i[ok]] -= 1.0
    oh[32, i[~ok]] = NEG
    i2 = np.arange(LV_WIN)
    dist = i2 - 511
    ok = (dist >= 0) & (dist < 512)
    b = _t5_bucket(np.maximum(dist, 0))
    oh[b[ok], LV_SEL + i2[ok]] += 1.0
    oh[31, LV_SEL + i2[ok]] -= 1.0
    oh[32, LV_SEL + i2[~ok]] = NEG
    n = np.arange(512)
    j = np.arange(128)
    ov = ((16 * n[:, None] < 64 * j[None, :] + 64) & (16 * n[:, None] + 32 > 64 * j[None, :])).astype(np.float32)
    ov[511] = 0.0
    ovl = np.zeros((128, 4, 129), np.float32)
    ovl[:, :, 0:128] = ov.reshape(4, 128, 128).transpose(1, 0, 2)
    ovl[:, :, 128] = 1.0
    ovl[127, 3, :] = 0.0
    sm = np.zeros((64, 128, 128), np.float32)
    for st in range(64):
        pos = st * 128 + np.arange(128)
        cur = pos // 64
        blk = np.arange(128)[None, :]
        forced = (blk == 0) | (blk == cur[:, None]) | (blk == cur[:, None] - 1)
        fut = blk > cur[:, None]
        sm[st] = np.where(forced, 1e9, np.where(fut, -1e9, 0.0))
    gsel = np.zeros((3, 3, 128), np.float32)
    for g in range(3):
        gsel[g, g, :] = 1.0
    return {"nsa_oh": oh, "nsa_ovl": ovl.reshape(128, 4 * 129), "nsa_selmask": sm.reshape(64 * 128, 128),
            "nsa_gsel": gsel.reshape(3, 384)}


def mixer_nsa(mx, d, nqt=S // 512):
    m, mp, cst = mx.m, mx.mp, mx.cst
    ar = mx.arena
    ar.reset()
    F = Rot([ar.sb([128, 516], F32, "nsaF%d" % i) for i in range(12)])
    Bp = Rot([ar.sb([128, 512], BF16, "nsaB%d" % i) for i in range(6)])
    acc = [mx.banks.items[0], mx.banks.items[1], mx.banks.items[2]]
    rot = Rot(mx.banks.items[3:8])
    ident = cst[:, C_ID:C_ID + 128]
    onesf = ar.sb([128, 128], F32, "nsa_onesf")
    onesb = ar.sb([128, 128], BF16, "nsa_onesb")
    identb = ar.sb([128, 128], BF16, "nsa_identb")
    m.op("dve", lambda e: e.memset(onesf[:], 1.0), [], [onesf])
    m.op("dve", lambda e: e.memset(onesb[:], 1.0), [], [onesb])
    m.op("dve", lambda e: e.tensor_copy(out=identb[:], in_=ident), [cst], [identb])
    gq = ar.sb([128, 1], F32, "nsa_gq")
    m.op("dve", lambda e: e.tensor_scalar(out=gq[:], in0=mx.col(MP_NQG), scalar1=128 ** -0.5, scalar2=None, op0=ALU.mult), [mp], [gq])

    def rms_rows(src, gaincol, dst, n=512):
        sq = F.next()
        m.op("act", lambda e: e.activation(out=sq[:, 0:n], in_=src.ap, func=AF.Square), [src_t(src)], [sq])
        pn = rot.next()
        m.op("pe", lambda e: e.matmul(pn[:, 0:n], lhsT=onesf[:], rhs=sq[:, 0:n], start=True, stop=True), [onesf, sq], [pn])
        m.op("act", lambda e: e.activation(out=sq[:, 0:n], in_=pn[:, 0:n], func=AF.Sqrt, scale=1.0 / 128, bias=1e-6), [pn], [sq])
        m.op("dve", lambda e: e.reciprocal(out=sq[:, 0:n], in_=sq[:, 0:n]), [sq], [sq])
        m.op("dve", lambda e: e.scalar_tensor_tensor(out=dst.ap, in0=src.ap, scalar=gaincol, in1=sq[:, 0:n], op0=ALU.mult, op1=ALU.mult),
             [src_t(src), mp, gq, sq], [src_t(dst)])

    kcT = ar.sb([128, 512], BF16, "nsa_kcT")
    vc_tm = ar.sb([128, 4, 128], BF16, "nsa_vctm")
    m.op("dve", lambda e: e.memset(kcT[:], 0.0), [], [kcT])
    m.op("dve", lambda e: e.memset(vc_tm[:], 0.0), [], [vc_tm])
    w1 = [ar.sb([128, 32, 128], BF16, "nsa_w1%d" % i) for i in range(2)]
    w2 = [ar.sb([128, 128], BF16, "nsa_w2%d" % i) for i in range(2)]
    posb = ar.sb([128, 32], BF16, "nsa_posb")
    m.op("dve", lambda e: e.tensor_copy(out=posb[:], in_=mp[:, MP_POS:MP_POS + 32]), [mp], [posb])
    cb = ar.sb([128, 2], F32, "nsa_cb")
    for i, (k1n, k2n) in enumerate((("k1", "k2"), ("v1", "v2"))):
        for l0 in range(0, 32, 16):
            m.dma("pool", w1[i][:, l0:l0 + 16, :], d[k1n][l0 * 128:(l0 + 16) * 128, :].rearrange("(l p) h -> p l h", p=128),
                  reads=[d[k1n]], writes=[w1[i]])
        m.dma("pool", w2[i][:], d[k2n][:], reads=[d[k2n]], writes=[w2[i]])
        pb = rot.next()
        for l in range(32):
            m.op("pe", lambda e: e.matmul(pb[:, 0:1], lhsT=w1[i][:, l, :], rhs=posb[:, l:l + 1], start=(l == 0), stop=(l == 31)),
                 [w1[i], posb], [pb])
        m.op("act", lambda e: e.activation(out=cb[:, i:i + 1], in_=pb[:, 0:1], func=AF.Copy), [pb], [cb])
    chunk = Rot([ar.sb([128, 2080], BF16, "nsa_chunk%d" % i) for i in range(2)])
    for a in range(4):
        nb_ = 128 if a < 3 else 127
        tok0 = a * 2048
        ntok = 16 * (nb_ - 1) + 32
        for i in range(2):
            ch = chunk.next()
            row0 = R_NKV + (0 if i == 0 else 128)
            m.dma("pool", ch[:, 0:ntok], mx.mix_in[row0:row0 + 128, tok0:tok0 + ntok], reads=[mx.mix_in], writes=[ch])
            ph = rot.next()
            for l in range(32):
                rhs = bass.AP(tensor=ch.h, offset=l, ap=[[2080, 128], [16, nb_]])
                m.op("pe", lambda e: e.matmul(ph[:, 0:nb_], lhsT=w1[i][:, l, :], rhs=rhs, start=(l == 0), stop=(l == 31)), [w1[i], ch], [ph])
            hb_ = Bp.next()
            m.op("act", lambda e: e.activation(out=hb_[:, 0:nb_], in_=ph[:, 0:nb_], func=AF.Gelu_apprx_tanh, bias=cb[:, i:i + 1]), [ph, cb], [hb_])
            po = rot.next()
            m.op("pe", lambda e: e.matmul(po[:, 0:nb_], lhsT=w2[i][:], rhs=hb_[:, 0:nb_], start=True, stop=True), [w2[i], hb_], [po])
            of = F.next()
            m.op("act", lambda e: e.activation(out=of[:, 0:nb_], in_=po[:, 0:nb_], func=AF.Copy), [po], [of])
            if i == 0:
                rms_rows(V(of, of[:, 0:nb_]), mx.col(MP_NKG), V(kcT, kcT[:, a * 128:a * 128 + nb_]), nb_)
            else:
                pt = rot.next()
                m.op("pe", lambda e: e.transpose(out=pt[0:nb_, 0:128], in_=of[:, 0:nb_], identity=ident), [of, cst], [pt])
                m.op("act", lambda e: e.activation(out=vc_tm[0:nb_, a, :], in_=pt[0:nb_, 0:128], func=AF.Copy), [pt], [vc_tm])
    rbx = ar.sb([33, 5], F32, "nsa_rbx")
    m.op("dve", lambda e: e.memset(rbx[32:33, :], 1.0), [], [rbx])
    m.dma("sp", rbx[0:32, :], d["rb"][:], reads=[d["rb"]], writes=[rbx])
    fv_d = m.dram("nsa_fv", [5, LV], F32)
    ohs = Rot([ar.sb([33, 512], F32, "nsa_oh%d" % i) for i in range(2)])
    for c0 in range(0, LV, 512):
        n = min(512, LV - c0)
        o_ = ohs.next()
        m.dma("sp", o_[:, 0:n], d["oh"][:, c0:c0 + n], reads=[d["oh"]], writes=[o_])
        pf = rot.next()
        m.op("pe", lambda e: e.matmul(pf[0:5, 0:n], lhsT=rbx[:], rhs=o_[:, 0:n], start=True, stop=True), [rbx, o_], [pf])
        fs = F.next()
        m.op("act", lambda e: e.activation(out=fs[0:5, 0:n], in_=pf[0:5, 0:n], func=AF.Copy), [pf], [fs])
        m.dma("sp", fv_d[:, c0:c0 + n], fs[0:5, 0:n], reads=[fs], writes=[fv_d])
    MC = ar.sb([128, 5, 2560], BF16, "nsa_MC")
    MS = ar.sb([128, 1024], BF16, "nsa_MS")
    MW = ar.sb([128, 1408], BF16, "nsa_MW")
    jrev = cst[:, C_REV:C_REV + 128]
    tz = Rot([ar.sb([128, 512], F32, "nsa_tz%d" % i) for i in range(2)])

    def toeplitz(dst_fn, head_off, off, pstep, W):
        for c0 in range(0, W, 512):
            n = min(512, W - c0)
            t_ = tz.next()
            src = bass.AP(tensor=fv_d.h, offset=head_off + off + c0, ap=[[pstep, 128], [1, n]])
            m.dma("sp", t_[:, 0:n], src, reads=[fv_d], writes=[t_])
            pz = rot.next()
            m.op("pe", lambda e: e.matmul(pz[:, 0:n], lhsT=jrev, rhs=t_[:, 0:n], start=True, stop=True), [cst, t_], [pz])
            dst, dt_ = dst_fn(c0, n)
            m.op("act", lambda e: e.activation(out=dst, in_=pz[:, 0:n], func=AF.Copy), [pz], [dt_])

    for h in range(5):
        toeplitz(lambda c0, n, h=h: (MC[:, h, c0:c0 + n], MC), h * LV, 0, 16, 2560)
    toeplitz(lambda c0, n: (MS[:, c0:c0 + n], MS), 4 * LV, 1552, 1, 1024)
    toeplitz(lambda c0, n: (MW[:, c0:c0 + n], MW), 4 * LV, LV_SEL, 1, 1408)
    ovl = ar.sb([128, 4, 129], BF16, "nsa_ovl_s")
    m.dma("pool", ovl[:], d["ovl"][:, :].rearrange("p (a c) -> p a c", a=4), reads=[d["ovl"]], writes=[ovl])
    gsel = ar.sb([3, 3, 128], F32, "nsa_gsel_s")
    m.dma("sp", gsel[:], d["gsel"][:, :].rearrange("p (a c) -> p a c", a=3), reads=[d["gsel"]], writes=[gsel])
    stair = ar.sb([128, S], BF16, "nsa_stair")
    m.op("dve", lambda e: e.tensor_copy(out=stair[:, :].rearrange("p (j r) -> p j r", r=64), in_=bcast_mid(identb[:, 0:128], 64)),
         [identb], [stair])
    kslcT = ar.sb([128, S], BF16, "nsa_kslcT")
    vslc_tm = ar.sb([128, S // 128, 128], BF16, "nsa_vslc")
    kwin = Rot([ar.sb([128, 512], BF16, "nsa_kwin%d" % i) for i in range(2)])
    vwin = Rot([ar.sb([128, 4, 128], BF16, "nsa_vwin%d" % i) for i in range(2)])
    qn = ar.sb([128, 5, 512], BF16, "nsa_qn")
    Pc = [[ar.sb([128, 512], BF16, "nsa_pc%d_%d" % (h, a)) for a in range(4)] for h in range(5)]
    MT = ar.sb([128, 512], BF16, "nsa_MT")
    sc = ar.sb([128, 128], F32, "nsa_sc")
    sc2 = ar.sb([128, 128], F32, "nsa_sc2")
    top = ar.sb([128, 16], F32, "nsa_top")
    rsi = ar.sb([128, 4], F32, "nsa_rsi")
    smk = Rot([ar.sb([128, 128], F32, "nsa_smk%d" % i) for i in range(2)])
    gate3 = ar.sb([3, 512], F32, "nsa_gate3")
    prev_kw, prev_vw = None, None
    osum_b = ar.sb([128, 512], F32, "nsa_osum")
    for qt in range(nqt):
        q0 = qt * 512
        for (row, which_) in ((256, "ks"), (512, "kw")):
            kf = F.next()
            m.dma("sp", kf[:, 0:512], mx.mix_in[R_NKV + row:R_NKV + row + 128, q0:q0 + 512], reads=[mx.mix_in], writes=[kf])
            if which_ == "ks":
                rms_rows(V(kf, kf[:, 0:512]), mx.col(MP_NKG), V(kslcT, kslcT[:, q0:q0 + 512]))
            else:
                kw_cur = kwin.next()
                rms_rows(V(kf, kf[:, 0:512]), mx.col(MP_NKG), V(kw_cur, kw_cur[:, :]))
        vw_cur = vwin.next()
        for (row, which_) in ((384, "vs"), (640, "vw")):
            vf = F.next()
            m.dma("sp", vf[:, 0:512], mx.mix_in[R_NKV + row:R_NKV + row + 128, q0:q0 + 512], reads=[mx.mix_in], writes=[vf])
            pt = rot.next()
            for c in range(4):
                m.op("pe", lambda e: e.transpose(out=pt[:, c * 128:(c + 1) * 128], in_=vf[:, c * 128:(c + 1) * 128], identity=ident),
                     [vf, cst], [pt])
            if which_ == "vs":
                m.op("act", lambda e: e.activation(out=vslc_tm[:, qt * 4:qt * 4 + 4, :], in_=pt[:, :].rearrange("p (c v) -> p c v", c=4),
                                                   func=AF.Copy), [pt], [vslc_tm])
            else:
                m.op("act", lambda e: e.activation(out=vw_cur[:], in_=pt[:, :].rearrange("p (c v) -> p c v", c=4), func=AF.Copy),
                     [pt], [vw_cur])
        for h in range(5):
            qf = F.next()
            r0 = R_NQ + h * 128 if h < 4 else R_NQO
            m.dma("sp", qf[:, 0:512], mx.mix_in[r0:r0 + 128, q0:q0 + 512], reads=[mx.mix_in], writes=[qf])
            rms_rows(V(qf, qf[:, 0:512]), gq[:], V(qn, qn[:, h, :]))
        m.dma("sp", gate3[:], mx.mix_in[R_NG:R_NG + 3, q0:q0 + 512], reads=[mx.mix_in], writes=[gate3])
        m.op("act", lambda e: e.activation(out=gate3[:], in_=gate3[:], func=AF.Sigmoid), [gate3], [gate3])
        osum = osum_b

        def finish_branch(g, po, prs, first):
            pg = rot.next()
            m.op("pe", lambda e: e.matmul(pg[:], lhsT=gsel[:, g, :], rhs=gate3[:], start=True, stop=True), [gsel, gate3], [pg])
            wv_ = F.next()
            m.op("dve", lambda e: e.tensor_scalar(out=wv_[:, 0:512], in0=prs[:], scalar1=1e-30, scalar2=None, op0=ALU.max), [prs], [wv_])
            m.op("dve", lambda e: e.reciprocal(out=wv_[:, 0:512], in_=wv_[:, 0:512]), [wv_], [wv_])
            m.op("dve", lambda e: e.tensor_tensor(out=wv_[:, 0:512], in0=wv_[:, 0:512], in1=pg[:], op=ALU.mult), [wv_, pg], [wv_])
            if first:
                m.op("dve", lambda e: e.tensor_tensor(out=osum[:, 0:512], in0=wv_[:, 0:512], in1=po[:], op=ALU.mult), [wv_, po], [osum])
            else:
                m.op("dve", lambda e: e.tensor_tensor(out=wv_[:, 0:512], in0=wv_[:, 0:512], in1=po[:], op=ALU.mult), [wv_, po], [wv_])
                m.op("dve", lambda e: e.tensor_tensor(out=osum[:, 0:512], in0=osum[:, 0:512], in1=wv_[:, 0:512], op=ALU.add), [osum, wv_], [osum])

        na = min(4, qt // 4 + 1)
        for h in range(5):
            for a in range(na):
                kn = 128 if a < 3 else 127
                dc = q0 - 2048 * a
                ps = rot.next()
                need_add = dc <= 2048
                m.op("pe", lambda e: e.matmul(ps[0:kn, :], lhsT=kcT[:, a * 128:a * 128 + kn], rhs=qn[:, h, :], start=True, stop=not need_add),
                     [kcT, qn], [ps])
                if need_add:
                    m.op("pe", lambda e: e.matmul(ps[0:kn, :], lhsT=identb[0:kn, 0:kn], rhs=MC[0:kn, h, dc:dc + 512], start=False, stop=True),
                         [identb, MC], [ps])
                m.op("act", lambda e: e.activation(out=Pc[h][a][0:kn, :], in_=ps[0:kn, :], func=AF.Exp), [ps], [Pc[h][a]])
        po, prs = acc[0], acc[1]
        for a in range(na):
            kn = 128 if a < 3 else 127
            m.op("pe", lambda e: e.matmul(po[:], lhsT=vc_tm[0:kn, a, :], rhs=Pc[4][a][0:kn, :], start=(a == 0), stop=(a == na - 1)),
                 [vc_tm, Pc[4][a]], [po])
        for a in range(na):
            kn = 128 if a < 3 else 127
            m.op("pe", lambda e: e.matmul(prs[:], lhsT=onesb[0:kn, :], rhs=Pc[4][a][0:kn, :], start=(a == 0), stop=(a == na - 1)),
                 [onesb, Pc[4][a]], [prs])
        finish_branch(0, po, prs, True)
        for si in range(4):
            pss = [rot.next(), rot.next()]
            for h in range(4):
                pb_ = pss[h // 2]
                c0 = (h % 2) * 160
                for a in range(na):
                    kn = 128 if a < 3 else 127
                    m.op("pe", lambda e: e.matmul(pb_[:, c0:c0 + 129], lhsT=Pc[h][a][0:kn, si * 128:(si + 1) * 128], rhs=ovl[0:kn, a, :],
                                                  start=(a == 0), stop=(a == na - 1)), [Pc[h][a], ovl], [pb_])
            for h in range(4):
                pb_ = pss[h // 2]
                c0 = (h % 2) * 160
                m.op("dve", lambda e: e.tensor_scalar(out=rsi[:, h:h + 1], in0=pb_[:, c0 + 128:c0 + 129], scalar1=1e-30, scalar2=None, op0=ALU.max),
                     [pb_], [rsi])
            m.op("dve", lambda e: e.reciprocal(out=rsi[:], in_=rsi[:]), [rsi], [rsi])
            sk = smk.next()
            st_ = qt * 4 + si
            m.dma("sp", sk[:], d["selmask"][st_ * 128:(st_ + 1) * 128, :], reads=[d["selmask"]], writes=[sk])
            for h in range(4):
                pb_ = pss[h // 2]
                c0 = (h % 2) * 160
                m.op("dve", lambda e: e.scalar_tensor_tensor(out=sc[:], in0=pb_[:, c0:c0 + 128], scalar=rsi[:, h:h + 1],
                                                             in1=(sk[:] if h == 0 else sc[:]), op0=ALU.mult, op1=ALU.add),
                     [pb_, rsi, sk, sc], [sc])
            m.op("dve", lambda e: e.max(out=top[:, 0:8], in_=sc[:]), [sc], [top])
            m.op("dve", lambda e: e.match_replace(out=sc2[:], in_to_replace=top[:, 0:8], in_values=sc[:], imm_value=-3e38), [sc, top], [sc2])
            m.op("dve", lambda e: e.max(out=top[:, 8:16], in_=sc2[:]), [sc2], [top])
            m.op("dve", lambda e: e.tensor_scalar(out=sc2[:], in0=sc[:], scalar1=top[:, 15:16], scalar2=None, op0=ALU.is_ge), [sc, top], [sc2])
            m.op("dve", lambda e: e.tensor_scalar(out=sc2[:], in0=sc2[:], scalar1=-1.0, scalar2=-NEG, op0=ALU.add, op1=ALU.mult), [sc2], [sc2])
            ptm = rot.next()
            m.op("pe", lambda e: e.transpose(out=ptm[:, 0:128], in_=sc2[:], identity=ident), [sc2, cst], [ptm])
            m.op("act", lambda e: e.activation(out=MT[:, si * 128:(si + 1) * 128], in_=ptm[:, 0:128], func=AF.Copy), [ptm], [MT])
        po, prs = acc[0], acc[1]
        nkt = 4 * qt + 4
        for kt in range(nkt):
            dl = q0 - 128 * kt
            ps = rot.next()
            need_add = dl <= 128
            m.op("pe", lambda e: e.matmul(ps[:], lhsT=kslcT[:, kt * 128:(kt + 1) * 128], rhs=qn[:, 4, :], start=True, stop=False), [kslcT, qn], [ps])
            m.op("pe", lambda e: e.matmul(ps[:], lhsT=stair[:, kt * 128:(kt + 1) * 128], rhs=MT[:], start=False, stop=not need_add), [stair, MT], [ps])
            if need_add:
                m.op("pe", lambda e: e.matmul(ps[:], lhsT=identb[:], rhs=MS[:, dl + 384:dl + 384 + 512], start=False, stop=True), [identb, MS], [ps])
            pp = Bp.next()
            m.op("act", lambda e: e.activation(out=pp[:], in_=ps[:], func=AF.Exp), [ps], [pp])
            m.op("pe", lambda e: e.matmul(po[:], lhsT=vslc_tm[:, kt, :], rhs=pp[:], start=(kt == 0), stop=(kt == nkt - 1)), [vslc_tm, pp], [po])
            m.op("pe", lambda e: e.matmul(prs[:], lhsT=onesb[:], rhs=pp[:], start=(kt == 0), stop=(kt == nkt - 1)), [onesb, pp], [prs])
        finish_branch(1, po, prs, False)
        po, prs = acc[0], acc[1]
        wt = []
        if prev_kw is not None:
            wt += [(prev_kw, prev_vw, c, q0 - (q0 - 512 + 128 * c)) for c in range(4)]
        wt += [(kw_cur, vw_cur, c, q0 - (q0 + 128 * c)) for c in range(4)]
        for wi, (kw_, vw_, c, dl) in enumerate(wt):
            ps = rot.next()
            m.op("pe", lambda e: e.matmul(ps[:], lhsT=kw_[:, c * 128:(c + 1) * 128], rhs=qn[:, 4, :], start=True, stop=False), [kw_, qn], [ps])
            m.op("pe", lambda e: e.matmul(ps[:], lhsT=identb[:], rhs=MW[:, dl + 384:dl + 384 + 512], start=False, stop=True), [identb, MW], [ps])
            pp = Bp.next()
            m.op("act", lambda e: e.activation(out=pp[:], in_=ps[:], func=AF.Exp), [ps], [pp])
            m.op("pe", lambda e: e.matmul(po[:], lhsT=vw_[:, c, :], rhs=pp[:], start=(wi == 0), stop=(wi == len(wt) - 1)), [vw_, pp], [po])
            m.op("pe", lambda e: e.matmul(prs[:], lhsT=onesb[:], rhs=pp[:], start=(wi == 0), stop=(wi == len(wt) - 1)), [onesb, pp], [prs])
        finish_branch(2, po, prs, False)
        prev_kw, prev_vw = kw_cur, vw_cur
        m.dma(mx.dq.next(), mx.y_out[256:384, q0:q0 + 512], osum[:, 0:512], reads=[osum], writes=[mx.y_out])


def src_t(x):
    return x.t if hasattr(x, "t") else x


_PROGS = {}


def _prog(key):
    if key not in _PROGS:
        if key == "A":
            _PROGS[key] = build_dense(False, True)[0]
        elif key == "CA":
            _PROGS[key] = build_dense(True, True)[0]
        elif key == "C":
            _PROGS[key] = build_dense(True, False)[0]
        elif key == "M":
            _PROGS[key] = build_mixer()[0]
    return _PROGS[key]


def _halo_cols(aT, tb):
    if tb == 0:
        return np.ascontiguousarray(np.concatenate([np.zeros((aT.shape[0], HALO), aT.dtype), aT[:, 0:NTOK]], axis=1))
    return np.ascontiguousarray(aT[:, tb - HALO:tb + NTOK])


def kernel(**inp):
    inp = {k: np.asarray(v) for k, v in inp.items()}
    x = inp["x"]
    cores = list(range(NCORE))
    xT = [np.ascontiguousarray(x[b].T) for b in range(NB)]
    cst = make_consts()
    ncst = make_nsa_consts()
    dps = [pack_dense_params(inp["attn_norm"][l], inp["ffn_norm"][l], inp["ffn_conv_w"][l], inp["ffn_conv_b"][l])
           for l in range(DEPTH)]
    maps = []
    for c in cores:
        b, tb = c // 4, (c % 4) * NTOK
        maps.append({"x_in": np.ascontiguousarray(xT[b][:, tb:tb + NTOK]), "dpa": dps[0], "w_in_a": inp["w_in"][0]})
    res = run_bass_kernel_spmd(_prog("A"), maps, core_ids=cores).results
    proj = [r["proj_out"] for r in res]
    gates = [r["gates_out"] for r in res]
    for l in range(DEPTH):
        projT = [np.concatenate([proj[b * 4 + i] for i in range(4)], axis=1) for b in range(NB)]
        maps = []
        for c in cores:
            b, j = c // 4, c % 4
            d = pack_mixer_inputs(l, b, j, projT[b], inp)
            d["cst"] = cst
            d.update(ncst)
            maps.append(d)
        res = run_bass_kernel_spmd(_prog("M"), maps, core_ids=cores).results
        yT = []
        for b in range(NB):
            y = np.empty((4 * 512, S), np.float32)
            for j in range(4):
                yo = res[b * 4 + j]["y_out"]
                for n in range(4):
                    y[n * 512 + j * 128:n * 512 + (j + 1) * 128] = yo[n * 128:(n + 1) * 128]
            yT.append(y)
        del res, maps
        last = (l == DEPTH - 1)
        gT = [np.concatenate([gates[b * 4 + i] for i in range(4)], axis=1) for b in range(NB)]
        maps = []
        for c in cores:
            b, tb = c // 4, (c % 4) * NTOK
            d = {"x_in": _halo_cols(xT[b], tb), "y_in": _halo_cols(yT[b], tb), "g_in": _halo_cols(gT[b], tb), "dpc": dps[l],
                 "w_branch": inp["w_branch"][l].reshape(4 * 512, D), "w_out": inp["w_out"][l],
                 "ffn_up": inp["ffn_up"][l], "ffn_down": inp["ffn_down"][l]}
            if not last:
                d["dpa"] = dps[l + 1]
                d["w_in_a"] = inp["w_in"][l + 1]
            maps.append(d)
        res = run_bass_kernel_spmd(_prog("C" if last else "CA"), maps, core_ids=cores).results
        for c in cores:
            b, tb = c // 4, (c % 4) * NTOK
            xT[b][:, tb:tb + NTOK] = res[c]["x_out"]
        if not last:
            proj = [r["proj_out"] for r in res]
            gates = [r["gates_out"] for r in res]
        del res, maps
    out = np.stack([xT[b].T for b in range(NB)], axis=0)
    return np.ascontiguousarray(out.astype(np.float32))
```
